# Optimizing a Trainium2 kernel written in Bass

```python
import math
import jax, jax.numpy as jnp
from jax import lax
import numpy as np

D_MODEL = 1024
BATCH = 32
SEQ = 2048
DEPTH = 1

MIX_WIDTH = D_MODEL
MOBA_WIDTH = MIX_WIDTH // 2
MOBA_HEAD_DIM = 64
MOBA_HEADS = MOBA_WIDTH // MOBA_HEAD_DIM
MOBA_BLOCK = 256
MOBA_TOPK = 3
QUERY_CHUNK = 128
GDN_WIDTH = MIX_WIDTH - MOBA_WIDTH
GDN_HEAD_DIM = 128
GDN_HEADS = GDN_WIDTH // GDN_HEAD_DIM
GDN_CONV = 4
GDN_CHUNK = 64
IN_SPLITS = (MOBA_WIDTH,) * 4 + (GDN_WIDTH,) * 4 + (GDN_HEADS, GDN_HEADS)
IN_WIDTH = sum(IN_SPLITS)
RMS_EPS = 1e-6
NEG_INF = -1e30

kernel_name = "hymba_moba_gated_deltanet_sandwich"


def rms_norm(x, w):
    xf = x.astype(jnp.float32)
    y = xf * lax.rsqrt(jnp.mean(xf * xf, axis=-1, keepdims=True) + RMS_EPS)
    return (y * w.astype(jnp.float32)).astype(x.dtype)


def alibi_slopes(n_heads):
    start = 2.0 ** (-8.0 / n_heads)
    return jnp.asarray(start ** np.arange(1, n_heads + 1), dtype=jnp.float32)


def moba_attention(q, k, v):
    B, T, H, dh = q.shape
    nb = -(-T // MOBA_BLOCK)
    pad = nb * MOBA_BLOCK - T
    n_qc = T // QUERY_CHUNK
    k_sel = min(MOBA_TOPK, nb - 1)
    scale = dh ** -0.5
    slopes = alibi_slopes(H)
    qh = q.transpose(0, 2, 1, 3)
    pad_cfg = ((0, 0), (0, pad), (0, 0), (0, 0))
    kb = jnp.pad(k, pad_cfg).reshape(B, nb, MOBA_BLOCK, H, dh).transpose(0, 3, 1, 2, 4)
    vb = jnp.pad(v, pad_cfg).reshape(B, nb, MOBA_BLOCK, H, dh).transpose(0, 3, 1, 2, 4)
    kbar = jnp.mean(kb.astype(jnp.float32), axis=3)
    blk_off = jnp.arange(MOBA_BLOCK)
    head_idx = jnp.arange(H)[:, None, None]

    def chunk_attend(step):
        bi = step // n_qc
        q0 = (step % n_qc) * QUERY_CHUNK
        own = q0 // MOBA_BLOCK
        qc = lax.dynamic_slice_in_dim(qh[bi], q0, QUERY_CHUNK, axis=1)
        kb_b, vb_b = kb[bi], vb[bi]
        t_pos = q0 + jnp.arange(QUERY_CHUNK)
        k_own = lax.dynamic_index_in_dim(kb_b, own, axis=1, keepdims=False)
        v_own = lax.dynamic_index_in_dim(vb_b, own, axis=1, keepdims=False)
        dist_own = (t_pos[:, None] - (own * MOBA_BLOCK + blk_off)[None, :]).astype(jnp.float32)
        logit_own = (jnp.einsum('hqd,hkd->hqk', qc, k_own).astype(jnp.float32) * scale
                     - slopes[:, None, None] * dist_own)
        logit_own = jnp.where(dist_own >= 0, logit_own, NEG_INF)
        if k_sel == 0:
            p = jax.nn.softmax(logit_own, axis=-1).astype(v.dtype)
            return jnp.einsum('hqk,hkd->hqd', p, v_own)
        gate = jnp.einsum('hqd,hnd->hqn', qc.astype(jnp.float32), kbar[bi])
        gate = jnp.where(jnp.arange(nb) < own, gate, NEG_INF)
        _, sel = lax.top_k(gate, k_sel)
        k_g = kb_b[head_idx, sel]
        v_g = vb_b[head_idx, sel]
        s_sel = sel[..., None] * MOBA_BLOCK + blk_off
        dist_sel = (t_pos[None, :, None, None] - s_sel).astype(jnp.float32)
        logit_sel = (jnp.einsum('hqd,hqnkd->hqnk', qc, k_g).astype(jnp.float32) * scale
                     - slopes[:, None, None, None] * dist_sel)
        valid = (jnp.arange(k_sel) < own)[None, None, :, None]
        logit_sel = jnp.where(valid, logit_sel, NEG_INF).reshape(H, QUERY_CHUNK, k_sel * MOBA_BLOCK)
        p = jax.nn.softmax(jnp.concatenate([logit_sel, logit_own], axis=-1), axis=-1).astype(v.dtype)
        p_sel = p[..., :k_sel * MOBA_BLOCK].reshape(H, QUERY_CHUNK, k_sel, MOBA_BLOCK)
        p_own = p[..., k_sel * MOBA_BLOCK:]
        return (jnp.einsum('hqnk,hqnkd->hqd', p_sel, v_g)
                + jnp.einsum('hqk,hkd->hqd', p_own, v_own))

    out = lax.map(chunk_attend, jnp.arange(B * n_qc))
    out = out.reshape(B, n_qc, H, QUERY_CHUNK, dh).transpose(0, 1, 3, 2, 4)
    return out.reshape(B, T, H * dh)


def causal_depthwise_conv(x, w):
    K, C = w.shape
    return lax.conv_general_dilated(
        x, w[:, None, :].astype(x.dtype), window_strides=(1,), padding=((K - 1, 0),),
        dimension_numbers=('NWC', 'WIO', 'NWC'), feature_group_count=C)


def l2_normalize(x):
    return x * lax.rsqrt(jnp.sum(x * x, axis=-1, keepdims=True) + RMS_EPS)


def gated_delta_rule(q, k, v, g, beta):
    B, T, H, dk = q.shape
    dv = v.shape[-1]
    C = GDN_CHUNK
    nc = T // C
    q = l2_normalize(q) * (dk ** -0.5)
    k = l2_normalize(k)

    def chunks(t):
        return t.reshape(B, nc, C, H, -1).transpose(1, 0, 3, 2, 4)

    q, k, v = chunks(q), chunks(k), chunks(v)
    beta = beta.reshape(B, nc, C, H).transpose(1, 0, 3, 2)
    gc = jnp.cumsum(g.reshape(B, nc, C, H).transpose(1, 0, 3, 2), axis=-1)
    causal = jnp.tril(jnp.ones((C, C), dtype=bool))
    strict = jnp.tril(jnp.ones((C, C), dtype=bool), -1)
    decay = jnp.exp(jnp.where(causal, gc[..., :, None] - gc[..., None, :], -jnp.inf))
    k_beta = k * beta[..., None]
    a = jnp.where(strict, jnp.einsum('nbhid,nbhjd->nbhij', k_beta, k) * decay, 0.0)
    eye = jnp.eye(C, dtype=jnp.float32)
    t_inv = lax.linalg.triangular_solve(a + eye, jnp.broadcast_to(eye, a.shape),
                                        left_side=True, lower=True, unit_diagonal=True)
    u = t_inv @ (v * beta[..., None])
    w = t_inv @ (k_beta * jnp.exp(gc)[..., None])
    intra = jnp.where(causal, jnp.einsum('nbhid,nbhjd->nbhij', q, k) * decay, 0.0)
    q_dec = q * jnp.exp(gc)[..., None]
    k_dec = k * jnp.exp(gc[..., -1:] - gc)[..., None]
    g_last = jnp.exp(gc[..., -1])

    def step(S, xs):
        q_i, k_i, u_i, w_i, intra_i, gl = xs
        v_new = u_i - w_i @ S
        o = q_i @ S + intra_i @ v_new
        S = S * gl[..., None, None] + jnp.einsum('bhcd,bhce->bhde', k_i, v_new)
        return S, o

    S0 = jnp.zeros((B, H, dk, dv), dtype=jnp.float32)
    _, o = lax.scan(step, S0, (q_dec, k_dec, u, w, intra, g_last))
    return o.transpose(1, 0, 3, 2, 4).reshape(B, T, H, dv)


def hybrid_layer(x, norm_pre_w, w_in, conv_w, a_log, dt_bias, gdn_norm_w, w_out, norm_post_w):
    B, T, _ = x.shape
    h = rms_norm(x, norm_pre_w)
    proj = h @ w_in.astype(x.dtype)
    split_at = list(np.cumsum(IN_SPLITS)[:-1])
    a_q, a_k, a_v, a_z, d_q, d_k, d_v, d_z, d_b, d_a = jnp.split(proj, split_at, axis=-1)

    heads_a = lambda t: t.reshape(B, T, MOBA_HEADS, MOBA_HEAD_DIM)
    moba_out = moba_attention(heads_a(a_q), heads_a(a_k), heads_a(a_v)) * jax.nn.silu(a_z)

    qkv = jax.nn.silu(causal_depthwise_conv(jnp.concatenate([d_q, d_k, d_v], axis=-1), conv_w))
    g_q, g_k, g_v = jnp.split(qkv, 3, axis=-1)
    heads_b = lambda t: t.reshape(B, T, GDN_HEADS, GDN_HEAD_DIM).astype(jnp.float32)
    beta = jax.nn.sigmoid(d_b.astype(jnp.float32))
    g = -jnp.exp(a_log.astype(jnp.float32)) * jax.nn.softplus(
        d_a.astype(jnp.float32) + dt_bias.astype(jnp.float32))
    o = gated_delta_rule(heads_b(g_q), heads_b(g_k), heads_b(g_v), g, beta)
    gdn_out = rms_norm(o, gdn_norm_w).reshape(B, T, GDN_WIDTH).astype(x.dtype) * jax.nn.silu(d_z)

    mixed = jnp.concatenate([moba_out, gdn_out], axis=-1) @ w_out.astype(x.dtype)
    return x + rms_norm(mixed, norm_post_w)


def setup_inputs(seed: int = 0) -> dict:
    key = jax.random.key(seed)
    ks = jax.random.split(key, 9)
    f32 = jnp.float32
    x = jax.random.normal(ks[0], (BATCH, SEQ, D_MODEL), f32)
    norm_pre_w = 1.0 + 0.05 * jax.random.normal(ks[1], (DEPTH, D_MODEL), f32)
    w_in = jax.random.normal(ks[2], (DEPTH, D_MODEL, IN_WIDTH), f32) * D_MODEL ** -0.5
    conv_w = jax.random.normal(ks[3], (DEPTH, GDN_CONV, 3 * GDN_WIDTH), f32) * GDN_CONV ** -0.5
    a_log = jnp.log(jax.random.uniform(ks[4], (DEPTH, GDN_HEADS), f32, minval=1.0, maxval=16.0))
    dt = jnp.exp(jax.random.uniform(ks[5], (DEPTH, GDN_HEADS), f32,
                                    minval=math.log(1e-3), maxval=math.log(1e-1)))
    dt_bias = dt + jnp.log(-jnp.expm1(-dt))
    gdn_norm_w = 1.0 + 0.05 * jax.random.normal(ks[6], (DEPTH, GDN_HEAD_DIM), f32)
    w_out = jax.random.normal(ks[7], (DEPTH, MIX_WIDTH, D_MODEL), f32) * MIX_WIDTH ** -0.5
    norm_post_w = 1.0 + 0.05 * jax.random.normal(ks[8], (DEPTH, D_MODEL), f32)
    return {"x": x, "norm_pre_w": norm_pre_w, "w_in": w_in, "conv_w": conv_w,
            "a_log": a_log, "dt_bias": dt_bias, "gdn_norm_w": gdn_norm_w,
            "w_out": w_out, "norm_post_w": norm_post_w}


def reference(x, norm_pre_w, w_in, conv_w, a_log, dt_bias, gdn_norm_w, w_out, norm_post_w):
    for layer in range(DEPTH):
        x = hybrid_layer(x, norm_pre_w[layer], w_in[layer], conv_w[layer], a_log[layer],
                         dt_bias[layer], gdn_norm_w[layer], w_out[layer], norm_post_w[layer])
    return x
```

```python
import contextlib
import math
import os

import numpy as np

import concourse.bass as bass
import concourse.mybir as mybir
from concourse.bass_utils import run_bass_kernel_spmd

F32 = mybir.dt.float32
BF16 = mybir.dt.bfloat16
AF = mybir.ActivationFunctionType
ALU = mybir.AluOpType

T = 2048
D = 1024
NCOL = 4104
NSEQ = 4
NCORES = 8
EPS = 1e-6
NEGA = -240000.0
NEGD = -30000.0


class Tok:
    __slots__ = ("sem", "val", "eng")

    def __init__(self, sem, val, eng):
        self.sem, self.val, self.eng = sem, val, eng


class Buf:
    __slots__ = ("name", "w", "r", "excl")

    def __init__(self, name, excl=False):
        self.name = name
        self.w = []
        self.r = []
        self.excl = excl


class Eng:
    def __init__(self, e, sem, name, is_pe=False):
        self.e, self.sem, self.name, self.is_pe = e, sem, name, is_pe
        self.count = 0
        self.waited = {}

    def _wait(self, tok):
        if tok.eng is self and self.is_pe:
            return
        k = id(tok.sem)
        if self.waited.get(k, 0) >= tok.val:
            return
        self.e.wait_ge(tok.sem, tok.val)
        self.waited[k] = tok.val

    def deps(self, reads, writes):
        for b in reads:
            for t in b.w:
                self._wait(t)
            if b.excl:
                for t in b.r:
                    self._wait(t)
        for b in writes:
            for t in b.w:
                self._wait(t)
            for t in b.r:
                self._wait(t)

    def commit(self, tok, reads, writes):
        for b in writes:
            b.w = [tok]
            b.r = []
        for b in reads:
            if b in writes:
                continue
            if b.excl:
                b.w = [tok]
                b.r = []
                continue
            b.r = [t for t in b.r if t.sem is not tok.sem] + [tok]

    def op(self, fn, reads=(), writes=()):
        self.deps(reads, writes)
        ins = fn()
        self.count += 1
        ins.then_inc(self.sem, 1)
        tok = Tok(self.sem, self.count, self)
        self.commit(tok, reads, writes)
        return tok


class Chan:
    def __init__(self, sem):
        self.sem = sem
        self.count = 0


def dma(q, chan, out, in_, reads=(), writes=()):
    q.deps(reads, writes)
    chan.count += 16
    q.e.dma_start(out=out, in_=in_).then_inc(chan.sem, 16)
    tok = Tok(chan.sem, chan.count, None)
    q.commit(tok, reads, writes)
    return tok


class _Cols:
    def __init__(self):
        self.n = 0
        self.d = {}

    def add(self, name, w):
        self.d[name] = (self.n, w)
        self.n += w
        return self.d[name]


def _const_layout():
    c = _Cols()
    c.add("IDENT", 128)
    c.add("TRI", 128)
    c.add("ONES", 128)
    c.add("ALIBI", 8 * 16)
    c.add("OBM", 4 * 64)
    c.add("MASK_LS", 128)
    c.add("MASK_US", 128)
    c.add("MASK_UI", 128)
    c.add("CAUS", 128)
    c.add("SEL", 12 * 128)
    c.add("IND", 8 * 128)
    return c


CL = _const_layout()
SEL_A, SEL_AP, SEL_NB = 0, 1, 2


def make_consts():
    c = np.zeros((128, CL.n), np.float32)
    p = np.arange(128)[:, None]
    f = np.arange(128)[None, :]

    def put(name, arr):
        o, w = CL.d[name]
        c[:, o:o + w] = arr

    put("IDENT", (p == f).astype(np.float32))
    put("TRI", (p <= f).astype(np.float32))
    put("ONES", np.ones((128, 128), np.float32))
    put("MASK_LS", np.where(p > f, 0.0, NEGD))
    put("MASK_US", np.where(f > p, 0.0, NEGD))
    put("MASK_UI", np.where(f >= p, 0.0, NEGD))
    put("CAUS", np.where(f >= p, 0.0, NEGA))
    slopes = (2.0 ** (-8.0 / 8)) ** np.arange(1, 9)
    al = np.zeros((128, 8, 16), np.float32)
    for h in range(8):
        for d in range(16):
            al[:, h, d] = -slopes[h] * (128 * d + 127 - np.arange(128))
    put("ALIBI", al.reshape(128, 128))
    sel = np.zeros((128, 12, 128), np.float32)
    for h in range(4):
        for sp in range(3):
            sel[sp * 12 + 0 * 4 + h, SEL_A * 4 + h, :] = 1.0
            sel[sp * 12 + 2 * 4 + h, SEL_AP * 4 + h, :] = 1.0
            sel[sp * 12 + 1 * 4 + h, SEL_NB * 4 + h, :] = -1.0
    put("SEL", sel.reshape(128, 12 * 128))
    ind = np.zeros((128, 8, 128), np.float32)
    for kb in range(8):
        ind[kb, kb, :] = 1.0
        ind[64 + kb, kb, :] = 1.0
    put("IND", ind.reshape(128, 8 * 128))
    obm = np.zeros((128, 4, 8, 8), np.float32)
    for ob in range(4, 8):
        obm[:, ob - 4, :, ob:] = -1e30
    put("OBM", obm.reshape(128, 256))
    return c


PW = 8 + 48 + 4 + 4 + 512 + 1024
LN_QS = math.log(128.0 ** -0.5)
BT = 256
TPB = 2
NB = T // BT
CF32 = 768


class NS:
    pass


def build(nseq=NSEQ, nblk=NB, dbg=None, phases=("gdn", "attn", "out")):
    nc = bass.Bass("TRN2", target_bir_lowering=False)
    dbg = dbg or {}
    x = nc.dram_tensor("x", [nseq, T, D], F32, kind="ExternalInput").ap()
    w_in = nc.dram_tensor("w_in", [D, NCOL], F32, kind="ExternalInput").ap()
    w_out = nc.dram_tensor("w_out", [D, D], F32, kind="ExternalInput").ap()
    consts = nc.dram_tensor("consts", [128, CL.n], F32, kind="ExternalInput").ap()
    params = nc.dram_tensor("params", [128, PW], F32, kind="ExternalInput").ap()
    y = nc.dram_tensor("y", [nseq, T, D], F32, kind="ExternalOutput").ap()
    dbg_aps = {}
    for name, shape in dbg.items():
        dbg_aps[name] = nc.dram_tensor("dbg_" + name, list(shape), F32, kind="ExternalOutput").ap()
    AX = mybir.AxisListType.X

    with contextlib.ExitStack() as es:
        def sb(name, shape, dt=F32, st=None):
            return (st or es).enter_context(nc.sbuf_tensor(name, list(shape), dt))

        def ps(name, shape, dt=F32):
            return es.enter_context(nc.psum_tensor(name, list(shape), dt))

        def sem(name):
            return es.enter_context(nc.semaphore(name))

        PE = Eng(nc.tensor, sem("s_pe"), "pe", is_pe=True)
        ACT = Eng(nc.scalar, sem("s_act"), "act")
        DVE = Eng(nc.vector, sem("s_dve"), "dve")
        POOL = Eng(nc.gpsimd, sem("s_pool"), "pool")
        SP = Eng(nc.sync, sem("s_sp"), "sp")
        V, G, A, TE = nc.vector, nc.gpsimd, nc.scalar, nc.tensor

        def chan(name):
            return Chan(sem(name))

        pA = [ps(f"pA{i}", [128, 512]) for i in range(2)]
        B_pA = [Buf("pA0", True), Buf("pA1", True)]
        pB = [ps(f"pB{i}", [128, 512]) for i in range(4)]
        B_pB = [Buf(f"pB{i}", True) for i in range(4)]
        pT = ps("pT", [128, 1024], BF16)
        B_pT = Buf("pT", True)
        pS = ps("pS", [128, 512])
        B_pS = Buf("pS", True)
        scr = sb("scr", [128, 8])
        scrb = sb("scrb", [128, 8], BF16)
        dbg_chans = []

        B_scr = Buf("scr")

        def barrier():
            b = B_scr
            PE.op(lambda: TE.matmul(pS[0:8, 510:512], lhsT=scrb[0:8, 0:8], rhs=scrb[0:8, 0:2], start=True, stop=True),
                  reads=[b], writes=[B_pS])
            ACT.op(lambda: A.copy(out=scr[0:1, 0:1], in_=scr[0:1, 0:1]), reads=[b, B_pS], writes=[b])
            DVE.op(lambda: V.tensor_copy(out=scr[0:1, 1:2], in_=scr[0:1, 1:2]), reads=[b], writes=[b])
            POOL.op(lambda: G.tensor_copy(out=scr[0:1, 2:3], in_=scr[0:1, 2:3]), reads=[b], writes=[b])
            for e in (PE, ACT, DVE, SP):
                e.deps([b], [])
            for ch in dbg_chans:
                nc.sync.wait_ge(ch.sem, ch.count)

        POOL.op(lambda: G.memset(scrb[:], 0.0), writes=[B_scr])
        POOL.op(lambda: G.memset(scr[:], 0.0), writes=[B_scr])

        cf = sb("cf", [128, CF32])
        cbf = sb("cbf", [128, CL.n], BF16)
        prm = sb("prm", [128, PW])
        w_in_bf = sb("w_in_bf", [128, 8, NCOL], BF16)
        w_out_bf = sb("w_out_bf", [128, 8, D], BF16)
        HALF = NCOL // 2
        B_c = Buf("consts")
        ch_c = chan("c_c")
        dma(SP, ch_c, cf[:], consts[:, 0:CF32], writes=[B_c])
        dma(SP, ch_c, prm[:], params[:, :], writes=[B_c])
        B_c.w = [B_c.w[-1]]
        npw = prm[:, 0:8]
        convw = prm[:, 8:56]
        alog_bc = prm[:, 56:60]
        dtb_bc = prm[:, 60:64]
        gnw_bc = prm[:, 64:576]
        wpost_bc = prm[:, 576:1600]
        with contextlib.ExitStack() as es2:
            stg = [sb(f"stg{i}", [128, HALF], st=es2) for i in range(2)]
            B_stg = [Buf("stg0"), Buf("stg1")]
            ch_stg = [chan("c_stg0"), chan("c_stg1")]
            i = 0
            hh = CL.n // 2
            for hf in range(2):
                sl = i % 2
                dma(SP, ch_stg[sl], stg[sl][:, 0:hh], consts[:, hf * hh:(hf + 1) * hh], writes=[B_stg[sl]])
                DVE.op(lambda sl=sl, hf=hf: V.tensor_copy(out=cbf[:, hf * hh:(hf + 1) * hh], in_=stg[sl][:, 0:hh]),
                       reads=[B_stg[sl]], writes=[Buf("t")])
                i += 1
            for kc in range(8):
                for hf in range(2):
                    sl = i % 2
                    dma(SP, ch_stg[sl], stg[sl][:], w_in[kc * 128:(kc + 1) * 128, hf * HALF:(hf + 1) * HALF],
                        writes=[B_stg[sl]])
                    if sl == 0:
                        ACT.op(lambda sl=sl, kc=kc, hf=hf: A.activation(
                            out=w_in_bf[:, kc, hf * HALF:(hf + 1) * HALF], in_=stg[sl][:], func=AF.Copy,
                            scale=npw[:, kc:kc + 1]), reads=[B_stg[sl], B_c], writes=[Buf("t")])
                    else:
                        DVE.op(lambda sl=sl, kc=kc, hf=hf: V.tensor_scalar(
                            out=w_in_bf[:, kc, hf * HALF:(hf + 1) * HALF], in0=stg[sl][:],
                            scalar1=npw[:, kc:kc + 1], scalar2=None, op0=ALU.mult),
                            reads=[B_stg[sl], B_c], writes=[Buf("t")])
                    i += 1
            for kc in range(8):
                sl = i % 2
                dma(SP, ch_stg[sl], stg[sl][:, 0:D], w_out[kc * 128:(kc + 1) * 128, :], writes=[B_stg[sl]])
                if sl == 0:
                    ACT.op(lambda sl=sl, kc=kc: A.copy(out=w_out_bf[:, kc, :], in_=stg[sl][:, 0:D]),
                           reads=[B_stg[sl]], writes=[Buf("t")])
                else:
                    DVE.op(lambda sl=sl, kc=kc: V.tensor_copy(out=w_out_bf[:, kc, :], in_=stg[sl][:, 0:D]),
                           reads=[B_stg[sl]], writes=[Buf("t")])
                i += 1
            barrier()

        def C(name, bf=False, rows=128, sub=None):
            o, w = CL.d[name]
            t = cbf if bf else cf
            if not bf:
                assert o + w <= CF32
            if sub is not None:
                so, sw = sub
                return t[0:rows, o + so:o + so + sw]
            return t[0:rows, o:o + w]

        IND_O = CL.d["IND"][0]
        ident_bf = C("IDENT", bf=True)
        ident_f = C("IDENT")

        xo = [sb(f"xo{i}", [128, D]) for i in range(2)]
        B_xo = [Buf(f"xo{i}") for i in range(2)]
        ch_xo = [chan(f"c_xo{i}") for i in range(2)]
        ch_st = [chan(f"c_st{i}") for i in range(2)]
        ch_xs = chan("c_xs")
        aqT = sb("aqT", [128, 4, BT], BF16)
        B_aq = [Buf(f"aq{p}") for p in range(4)]
        akT = sb("akT", [128, 4, T], BF16)
        B_ak = [[Buf(f"ak{p}_{b}") for b in range(NB)] for p in range(4)]
        vtm = sb("vtm", [128, 16, 8, 65], BF16)
        B_v = [Buf(f"v{t}") for t in range(16)]
        siluz = sb("siluz", [128, TPB, 512], BF16)
        B_sz = [Buf(f"sz{t}") for t in range(TPB)]
        zw = sb("zw", [128, TPB, 512], BF16)
        B_zw = [Buf(f"zw{t}") for t in range(TPB)]
        sraw = sb("sraw", [128, TPB, 8])
        B_sraw = Buf("sraw")
        dT = [sb(f"dT{k}", [128, 4, BT], BF16) for k in range(3)]
        B_dT = [[Buf(f"dT{k}_{h}") for h in range(4)] for k in range(3)]
        halo = sb("halo", [128, 12, 4], BF16)
        B_halo = [Buf(f"halo{c}") for c in range(12)]
        mixed = sb("mixed", [128, TPB, D], BF16)
        B_mx = [[Buf(f"mx{t}_{i}") for i in range(2)] for t in range(TPB)]
        mixT = sb("mixT", [128, 8, 128], BF16)
        B_mixT = Buf("mixT")
        tmpf = [sb(f"tmpf{i}", [128, 512]) for i in range(2)]
        B_tmpf = [Buf("tmpf0"), Buf("tmpf1")]
        small = sb("small", [128, 64])
        B_small = Buf("small")
        junk = sb("junk", [128, D], BF16)
        B_junk = Buf("junk")
        nea = sb("nea", [128, 4])
        Sst = sb("Sst", [128, 4, 128])
        Sbf = sb("Sbf", [128, 4, 128], BF16)
        B_S, B_Sbf = Buf("S"), Buf("Sbf")
        kbar = sb("kbar", [128, 4, 8])
        kbar_hi = sb("kbar_hi", [128, 4, 8], BF16)
        kbar_lo = sb("kbar_lo", [128, 4, 8], BF16)
        kbar_t = sb("kbar_t", [128, 4, 8])
        B_kbar = Buf("kbar")
        gm = sb("gm", [128, 8, 8])
        top8 = sb("top8", [128, 8, 8])
        selb = sb("selb", [128, 8, 72], BF16)
        B_gm = Buf("gm")
        selT = sb("selT", [72, 8, 128], BF16)
        B_selT = Buf("selT")
        NPT = 8
        PTs = sb("PTs", [128, NPT, 128], BF16)
        B_PT = [Buf(f"PT{i}") for i in range(NPT)]
        B_pSreg = [[Buf(f"pSreg{i}_{j}") for j in range(4)] for i in range(2)]
        att_t = sb("att_t", [128, 4, 64])
        rden = sb("rden", [128, 4])
        B_att = Buf("att_t")

        pBh = [p[:, :].rearrange("p (h d) -> p h d", h=4) for p in pB]
        pTh = pT[:, :].rearrange("p (h d) -> p h d", h=8)

        def bc(ap, shape):
            return ap.to_broadcast(list(shape))

        def merge(dst, srcs):
            for sbuf in srcs:
                dst.w = dst.w + sbuf.w
                dst.r = dst.r + sbuf.r

        POOL.op(lambda: G.memset(vtm[:, :, :, 64:65], 1.0), writes=B_v)
        POOL.op(lambda: G.memset(kbar[:], 0.0), writes=[B_kbar])
        POOL.op(lambda: G.memset(selb[:], 0.0), writes=[B_gm])
        ACT.op(lambda: A.activation(out=nea[:], in_=alog_bc, func=AF.Exp), reads=[B_c], writes=[B_small])
        DVE.op(lambda: V.tensor_scalar(out=nea[:], in0=nea[:], scalar1=-1.0, scalar2=None, op0=ALU.mult),
               reads=[B_small], writes=[B_small])
        barrier()

        dbg_toks = []

        def dump(name, ap, bufs):
            if name not in dbg_aps:
                return
            ch = chan("c_dbg_" + name)
            dbg_chans.append(ch)
            dbg_toks.append(dma(POOL, ch, dbg_aps[name], ap, reads=bufs))

        state = {"xo": 0, "pt": 0, "uid": 0}

        def inproj_block(s, b, ea):
            t0 = b * BT
            uid = state["uid"]
            xs = sb(f"xs_{uid}", [128, D], st=ea)
            xn = sb(f"xn_{uid}", [128, D], BF16, st=ea)
            hT = sb(f"hT_{uid}", [128, 8, BT], BF16, st=ea)
            pre = sb(f"pre_{uid}", [128, 2, BT + 8], BF16, st=ea)
            cdg = sb(f"cdg_{uid}", [128, 2, 4, 128], BF16, st=ea)
            B_xs, B_xn = Buf("xs"), Buf("xn")
            B_hT = [Buf(f"hT{t}") for t in range(TPB)]
            B_pre = [Buf("pre0"), Buf("pre1")]
            B_cdg = [Buf("cdg0"), Buf("cdg1")]
            for tt in range(TPB):
                gt = b * TPB + tt
                dma(SP, ch_xs, xs[:], x[s, gt * 128:(gt + 1) * 128, :], writes=[B_xs])
                ACT.op(lambda: A.activation(out=junk[:], in_=xs[:], func=AF.Square, accum_out=small[:, 0:1]),
                       reads=[B_xs], writes=[B_junk, B_small])
                ACT.op(lambda: A.activation(out=small[:, 1:2], in_=small[:, 0:1], func=AF.Ln,
                                            scale=1.0 / D, bias=EPS), reads=[B_small], writes=[B_small])
                ACT.op(lambda: A.activation(out=small[:, 2:3], in_=small[:, 1:2], func=AF.Exp, scale=-0.5),
                       reads=[B_small], writes=[B_small])
                DVE.op(lambda: V.tensor_scalar(out=xn[:], in0=xs[:], scalar1=small[:, 2:3], scalar2=None,
                                               op0=ALU.mult), reads=[B_xs, B_small], writes=[B_xn])

                if os.environ.get("K_STOP") == "p1a":
                    return

                def tr():
                    for kc in range(8):
                        ins = TE.transpose(out=pT[:, kc * 128:(kc + 1) * 128], in_=xn[:, kc * 128:(kc + 1) * 128],
                                           identity=ident_bf)
                    return ins
                PE.op(tr, reads=[B_xn], writes=[B_pT])
                if os.environ.get("K_STOP") == "p1b":
                    return
                ACT.op(lambda tt=tt: A.copy(out=hT[:, 0:4, tt * 128:(tt + 1) * 128], in_=pTh[:, 0:4, :]),
                       reads=[B_pT], writes=[Buf("t")])
                if os.environ.get("K_STOP") == "p1c":
                    return
                DVE.op(lambda tt=tt: V.tensor_copy(out=hT[:, 4:8, tt * 128:(tt + 1) * 128], in_=pTh[:, 4:8, :]),
                       reads=[B_pT], writes=[B_hT[tt]])
                if os.environ.get("K_STOP") == "p1d":
                    return
            if s == 0 and b == 0:
                dump("hT", hT[:, 0, :], B_hT)

            if os.environ.get("K_STOP") == "p1":
                return
            def fm_chunk(col0, bank):
                def f():
                    for kc in range(8):
                        ins = TE.matmul(pA[bank][:, 0:BT], lhsT=w_in_bf[:, kc, col0:col0 + 128], rhs=hT[:, kc, :],
                                        start=(kc == 0), stop=(kc == 7))
                    return ins
                PE.op(f, reads=B_hT, writes=[B_pA[bank]])

            ci_all = 0
            for p in range(4):
                bank = ci_all % 2
                ci_all += 1
                fm_chunk(0 + p * 128, bank)
                ACT.op(lambda p=p, bank=bank: A.copy(out=aqT[:, p, :], in_=pA[bank][:, 0:BT]),
                       reads=[B_pA[bank]], writes=[B_aq[p]])
            for p in range(4):
                bank = ci_all % 2
                ci_all += 1
                fm_chunk(512 + p * 128, bank)
                DVE.op(lambda p=p, bank=bank: V.tensor_copy(out=akT[:, p, t0:t0 + BT], in_=pA[bank][:, 0:BT]),
                       reads=[B_pA[bank]], writes=[B_ak[p][b]])
            for kind in range(3):
                for hd in range(4):
                    ci = kind * 4 + hd
                    bank = ci_all % 2
                    ci_all += 1
                    fm_chunk(2048 + ci * 128, bank)
                    sl = ci % 2
                    for tap in range(4):
                        POOL.op(lambda sl=sl, tap=tap, ci=ci: G.tensor_scalar(
                            out=cdg[:, sl, tap, :], in0=ident_f, scalar1=convw[:, tap * 12 + ci:tap * 12 + ci + 1],
                            scalar2=None, op0=ALU.mult), writes=[B_cdg[sl]])
                    if b == 0:
                        POOL.op(lambda sl=sl: G.memset(pre[:, sl, 0:4], 0.0), writes=[B_pre[sl]])
                    else:
                        POOL.op(lambda sl=sl, ci=ci: G.tensor_copy(out=pre[:, sl, 0:4], in_=halo[:, ci, :]),
                                reads=[B_halo[ci]], writes=[B_pre[sl]])
                    ACT.op(lambda sl=sl, bank=bank: A.copy(out=pre[:, sl, 4:4 + BT], in_=pA[bank][:, 0:BT]),
                           reads=[B_pA[bank]], writes=[B_pre[sl]])
                    POOL.op(lambda sl=sl, ci=ci: G.tensor_copy(out=halo[:, ci, :], in_=pre[:, sl, BT:BT + 4]),
                            reads=[B_pre[sl]], writes=[B_halo[ci]])
                    cb = ci % 2

                    def cv(sl=sl, cb=cb):
                        for tap in range(4):
                            ins = TE.matmul(pB[cb][:, 0:BT], lhsT=cdg[:, sl, tap, :],
                                            rhs=pre[:, sl, 1 + tap:1 + tap + BT], start=(tap == 0), stop=(tap == 3))
                        return ins
                    PE.op(cv, reads=[B_pre[sl], B_cdg[sl]], writes=[B_pB[cb]])
                    ACT.op(lambda kind=kind, hd=hd, cb=cb: A.activation(out=dT[kind][:, hd, :], in_=pB[cb][:, 0:BT],
                                                                        func=AF.Silu),
                           reads=[B_pB[cb]], writes=[B_dT[kind][hd]])
            if s == 0 and b == 0:
                dump("aqT", aqT[:, 0, :], B_aq)
                dump("dqT", dT[0][:, 0, :], B_dT[0])
                dump("dkT", dT[1][:, 0, :], B_dT[1])

            if os.environ.get("K_STOP") == "p2":
                return
            for tt in range(TPB):
                gt = b * TPB + tt

                def tm(tt=tt):
                    for j, col0 in enumerate((1024, 1536, 3584)):
                        for kc in range(8):
                            ins = TE.matmul(pB[j + 1][:, :], lhsT=hT[:, kc, tt * 128:(tt + 1) * 128],
                                            rhs=w_in_bf[:, kc, col0:col0 + 512], start=(kc == 0), stop=(kc == 7))
                    for kc in range(8):
                        ins = TE.matmul(pS[:, 0:8], lhsT=hT[:, kc, tt * 128:(tt + 1) * 128],
                                        rhs=w_in_bf[:, kc, 4096:4104], start=(kc == 0), stop=(kc == 7))
                    return ins
                PE.op(tm, reads=B_hT, writes=[B_pB[1], B_pB[2], B_pB[3], B_pS])
                ACT.op(lambda gt=gt: A.copy(out=vtm[:, gt, :, 0:64],
                                            in_=pB[1][:, :].rearrange("p (h d) -> p h d", h=8)),
                       reads=[B_pB[1]], writes=[B_v[gt]])
                ACT.op(lambda tt=tt: A.activation(out=siluz[:, tt, :], in_=pB[2][:, :], func=AF.Silu),
                       reads=[B_pB[2]], writes=[B_sz[tt]])
                fsl = tt % 2
                ACT.op(lambda fsl=fsl: A.activation(out=tmpf[fsl][:], in_=pB[3][:, :], func=AF.Silu),
                       reads=[B_pB[3]], writes=[B_tmpf[fsl]])
                POOL.op(lambda tt=tt, fsl=fsl: G.tensor_tensor(out=zw[:, tt, :], in0=tmpf[fsl][:], in1=gnw_bc,
                                                               op=ALU.mult),
                        reads=[B_tmpf[fsl]], writes=[B_zw[tt]])
                DVE.op(lambda tt=tt: V.tensor_copy(out=sraw[:, tt, :], in_=pS[:, 0:8]), reads=[B_pS], writes=[B_sraw])
            if s == 0 and b == 0:
                dump("sraw", sraw[:, 0, :], [B_sraw])
                dump("vtm", vtm[:, 0, 0, :], B_v)
                dump("zw", zw[:, 0, :], B_zw)

        def gdn_block(s, b, eb):
            uid = state["uid"]
            g = NS()

            def gb(name, shape, dt=F32):
                return sb(f"{name}_{uid}", shape, dt, st=eb)
            g.st_lnr = gb("st_lnr", [128, TPB, 8])
            g.st_lnbn = gb("st_lnbn", [128, TPB, 4])
            g.st_g = gb("st_g", [128, TPB, 4])
            g.st_e = gb("st_e", [128, TPB, 8])
            g.ST = gb("ST", [128, TPB, 12])
            g.st_c = gb("st_c", [128, TPB, 4])
            g.ex_a = gb("ex_a", [128, TPB, 4])
            g.ex_c = gb("ex_c", [128, TPB, 4])
            g.ex_b = gb("ex_b", [128, TPB, 4])
            g.ex_gl = gb("ex_gl", [128, TPB, 4])
            g.STs = gb("STs", [128, TPB, 36], BF16)
            g.STr = gb("STr", [128, TPB, 12])
            g.STh = gb("STh", [128, TPB, 12])
            g.STT = gb("STT", [36, TPB, 128], BF16)
            g.sq = [gb(f"sq{i}", [128, 4, BT], BF16) for i in range(2)]
            g.Eb = [gb(f"Eb{i}", [128, 4, 128], BF16) for i in range(2)]
            g.Xb = [gb(f"Xb{i}", [128, 4, 128], BF16) for i in range(2)]
            g.Yb = [gb(f"Yb{i}", [128, 4, 128], BF16) for i in range(2)]
            g.Pb = [gb(f"Pb{i}", [128, 4, 128], BF16) for i in range(2)]
            g.intraT = gb("intraT", [128, 4, 128], BF16)
            g.kbg = gb("kbg", [128, 4, 128], BF16)
            g.kdec = gb("kdec", [128, 4, 128], BF16)
            g.vb = gb("vb", [128, 4, 128], BF16)
            g.qdT = gb("qdT", [128, 4, 128], BF16)
            g.u_sb = gb("u_sb", [128, 4, 128])
            g.wT = gb("wT", [128, 4, 128], BF16)
            g.vnew = gb("vnew", [128, 4, 128], BF16)
            g.osq = gb("osq", [128, 4, 128])
            g.ost = gb("ost", [128, 16])
            g.B_st, g.B_STT = Buf("stats"), Buf("STT")
            g.B_sq = [Buf("sqq"), Buf("sqk")]
            g.B_Eb = [Buf("Eb0"), Buf("Eb1")]
            g.B_X = [Buf("X0"), Buf("X1")]
            g.B_Y = [Buf("Y0"), Buf("Y1")]
            g.B_P = [Buf("P0"), Buf("P1")]
            g.B_iT, g.B_kbg, g.B_kdec, g.B_vb, g.B_qdT = Buf("iT"), Buf("kbg"), Buf("kdec"), Buf("vb"), Buf("qdT")
            g.B_u, g.B_wT, g.B_vn, g.B_osq, g.B_ost = Buf("u"), Buf("wT"), Buf("vn"), Buf("osq"), Buf("ost")
            B_st = g.B_st
            ST, st_lnr, st_lnbn, st_g, st_e, st_c = g.ST, g.st_lnr, g.st_lnbn, g.st_g, g.st_e, g.st_c
            STs, STr, STh, STT = g.STs, g.STr, g.STh, g.STT
            if b == 0:
                DVE.op(lambda: V.memset(Sst[:], 0.0), writes=[B_S])
                POOL.op(lambda: G.memset(Sbf[:], 0.0), writes=[B_Sbf])
            for k in range(2):
                POOL.op(lambda k=k: G.tensor_tensor(out=g.sq[k][:], in0=dT[k][:], in1=dT[k][:], op=ALU.mult),
                        reads=B_dT[k], writes=[g.B_sq[k]])
            ones_col = C("ONES", bf=True, sub=(0, 1))

            def ssq():
                for tt in range(TPB):
                    for k in range(2):
                        for hd in range(4):
                            c0 = 64 + tt * 8 + k * 4 + hd
                            ins = TE.matmul(pS[:, c0:c0 + 1], lhsT=g.sq[k][:, hd, tt * 128:(tt + 1) * 128],
                                            rhs=ones_col, start=True, stop=True)
                return ins
            PE.op(ssq, reads=g.B_sq, writes=[B_pS])
            pS_ssq = pS[:, 64:64 + 8 * TPB].rearrange("p (t k) -> p t k", t=TPB)
            ACT.op(lambda: A.activation(out=st_lnr[:], in_=pS_ssq, func=AF.Ln, bias=EPS),
                   reads=[B_pS], writes=[B_st])
            ACT.op(lambda: A.activation(out=st_e[:, :, 0:4], in_=sraw[:, :, 0:4], func=AF.Exp, scale=-1.0),
                   reads=[B_sraw, B_st], writes=[B_st])
            ACT.op(lambda: A.activation(out=st_lnbn[:], in_=st_e[:, :, 0:4], func=AF.Ln, bias=1.0),
                   reads=[B_st], writes=[B_st])
            DVE.op(lambda: V.tensor_tensor(out=st_e[:, :, 4:8], in0=sraw[:, :, 4:8],
                                           in1=bc(dtb_bc.unsqueeze(1), [128, TPB, 4]), op=ALU.add),
                   reads=[B_sraw, B_st], writes=[B_st])
            ACT.op(lambda: A.activation(out=st_e[:, :, 4:8], in_=st_e[:, :, 4:8], func=AF.Exp),
                   reads=[B_st], writes=[B_st])
            ACT.op(lambda: A.activation(out=st_e[:, :, 4:8], in_=st_e[:, :, 4:8], func=AF.Ln, bias=1.0),
                   reads=[B_st], writes=[B_st])
            DVE.op(lambda: V.tensor_tensor(out=st_g[:], in0=st_e[:, :, 4:8], in1=bc(nea[:].unsqueeze(1), [128, TPB, 4]),
                                           op=ALU.mult), reads=[B_st], writes=[B_st])

            def gcm():
                for tt in range(TPB):
                    TE.matmul(pS[:, 96 + tt * 4:100 + tt * 4], lhsT=C("TRI"), rhs=st_g[:, tt, :], start=True, stop=True)
                    ins = TE.matmul(pS[:, 112 + tt * 4:116 + tt * 4], lhsT=C("ONES"), rhs=st_g[:, tt, :],
                                    start=True, stop=True)
                return ins
            PE.op(gcm, reads=[B_st], writes=[B_pS])
            gc = pS[:, 96:96 + 4 * TPB].rearrange("p (t k) -> p t k", t=TPB)
            gl = pS[:, 112:112 + 4 * TPB].rearrange("p (t k) -> p t k", t=TPB)
            lnrq = st_lnr[:, :, 0:4]
            lnrk = st_lnr[:, :, 4:8]
            DVE.op(lambda: V.scalar_tensor_tensor(out=ST[:, :, 4:8], in0=lnrk, scalar=0.5, op0=ALU.mult, in1=gc,
                                                  op1=ALU.add), reads=[B_st, B_pS], writes=[B_st])
            DVE.op(lambda: V.scalar_tensor_tensor(out=ST[:, :, 0:4], in0=lnrk, scalar=-0.5, op0=ALU.mult, in1=gc,
                                                  op1=ALU.add), reads=[B_st, B_pS], writes=[B_st])
            DVE.op(lambda: V.scalar_tensor_tensor(out=st_c[:], in0=ST[:, :, 4:8], scalar=-1.0, op0=ALU.mult, in1=gl,
                                                  op1=ALU.add), reads=[B_st, B_pS], writes=[B_st])
            DVE.op(lambda: V.tensor_tensor(out=ST[:, :, 0:4], in0=ST[:, :, 0:4], in1=st_lnbn[:], op=ALU.subtract),
                   reads=[B_st], writes=[B_st])
            DVE.op(lambda: V.scalar_tensor_tensor(out=ST[:, :, 8:12], in0=lnrq, scalar=-0.5, op0=ALU.mult, in1=gc,
                                                  op1=ALU.add), reads=[B_st, B_pS], writes=[B_st])
            DVE.op(lambda: V.tensor_scalar(out=ST[:, :, 8:12], in0=ST[:, :, 8:12], scalar1=LN_QS, scalar2=None,
                                           op0=ALU.add), reads=[B_st], writes=[B_st])
            ACT.op(lambda: A.activation(out=g.ex_a[:], in_=ST[:, :, 0:4], func=AF.Exp), reads=[B_st], writes=[B_st])
            ACT.op(lambda: A.activation(out=g.ex_c[:], in_=st_c[:], func=AF.Exp), reads=[B_st], writes=[B_st])
            ACT.op(lambda: A.activation(out=g.ex_b[:], in_=st_lnbn[:], func=AF.Exp, scale=-1.0),
                   reads=[B_st], writes=[B_st])
            ACT.op(lambda: A.activation(out=g.ex_gl[:], in_=gl, func=AF.Exp), reads=[B_st, B_pS], writes=[B_st])
            DVE.op(lambda: V.tensor_copy(out=STs[:, :, 0:12], in_=ST[:]), reads=[B_st], writes=[B_st])
            DVE.op(lambda: V.tensor_copy(out=STh[:], in_=STs[:, :, 0:12]), reads=[B_st], writes=[B_st])
            DVE.op(lambda: V.tensor_tensor(out=STr[:], in0=ST[:], in1=STh[:], op=ALU.subtract),
                   reads=[B_st], writes=[B_st])
            DVE.op(lambda: V.tensor_copy(out=STs[:, :, 12:24], in_=STr[:]), reads=[B_st], writes=[B_st])
            DVE.op(lambda: V.tensor_copy(out=STh[:], in_=STs[:, :, 12:24]), reads=[B_st], writes=[B_st])
            DVE.op(lambda: V.tensor_tensor(out=STr[:], in0=STr[:], in1=STh[:], op=ALU.subtract),
                   reads=[B_st], writes=[B_st])
            DVE.op(lambda: V.tensor_copy(out=STs[:, :, 24:36], in_=STr[:]), reads=[B_st], writes=[B_st])

            def trs():
                for tt in range(TPB):
                    ins = TE.transpose(out=pT[0:36, tt * 128:(tt + 1) * 128], in_=STs[:, tt, :], identity=ident_bf)
                return ins
            PE.op(trs, reads=[B_st], writes=[B_pT])
            ACT.op(lambda: A.copy(out=STT[:], in_=pT[0:36, 0:128 * TPB].rearrange("p (t k) -> p t k", t=TPB)),
                   reads=[B_pT], writes=[g.B_STT])
            if s == 0 and b == 0:
                dump("ST", ST[:, 0, :], [B_st])
                dump("exa", g.ex_a[:, 0, :], [B_st])
                dump("sg", st_g[:, 0, :], [B_st])
            for tt in range(TPB):
                gdn_chunk(s, b, tt, g)

        def gdn_chunk(s, b, tt, g):
            cs = slice(tt * 128, (tt + 1) * 128)
            first = (s == 0 and b == 0 and tt == 0)
            qT, kT, vT = dT[0], dT[1], dT[2]
            STc = g.STT[:, tt, :]
            B_st, B_STT = g.B_st, g.B_STT
            Eb, Xb, Yb, Pb = g.Eb, g.Xb, g.Yb, g.Pb
            B_Eb, B_X, B_Y, B_P = g.B_Eb, g.B_X, g.B_Y, g.B_P

            def sel(which, hd):
                return C("SEL", bf=True, rows=36, sub=((which * 4 + hd) * 128, 128))

            def trkv():
                for hd in range(4):
                    TE.transpose(out=pT[:, hd * 128:(hd + 1) * 128], in_=kT[:, hd, cs], identity=ident_bf)
                for hd in range(4):
                    ins = TE.transpose(out=pT[:, 512 + hd * 128:512 + (hd + 1) * 128], in_=vT[:, hd, cs],
                                       identity=ident_bf)
                return ins
            PE.op(trkv, reads=B_dT[1] + B_dT[2], writes=[B_pT])
            DVE.op(lambda: V.tensor_tensor(out=g.kbg[:], in0=pTh[:, 0:4, :],
                                           in1=bc(g.ex_a[:, tt, :].unsqueeze(2), [128, 4, 128]), op=ALU.mult),
                   reads=[B_pT, B_st], writes=[g.B_kbg])
            DVE.op(lambda: V.tensor_tensor(out=g.kdec[:], in0=pTh[:, 0:4, :],
                                           in1=bc(g.ex_c[:, tt, :].unsqueeze(2), [128, 4, 128]), op=ALU.mult),
                   reads=[B_pT, B_st], writes=[g.B_kdec])
            DVE.op(lambda: V.tensor_tensor(out=g.vb[:], in0=pTh[:, 4:8, :],
                                           in1=bc(g.ex_b[:, tt, :].unsqueeze(2), [128, 4, 128]), op=ALU.mult),
                   reads=[B_pT, B_st], writes=[g.B_vb])

            def kkqk():
                for hd in range(4):
                    TE.matmul(pB[0][:, hd * 128:(hd + 1) * 128], lhsT=kT[:, hd, cs], rhs=kT[:, hd, cs],
                              start=True, stop=True)
                for hd in range(4):
                    ins = TE.matmul(pB[1][:, hd * 128:(hd + 1) * 128], lhsT=kT[:, hd, cs], rhs=qT[:, hd, cs],
                                    start=True, stop=True)
                return ins
            PE.op(kkqk, reads=B_dT[0] + B_dT[1], writes=[B_pB[0], B_pB[1]])

            def dmat(bank, which, mask, lower):
                def f():
                    for hd in range(4):
                        o = pB[bank][:, hd * 128:(hd + 1) * 128]
                        if lower:
                            TE.matmul(o, lhsT=STc, rhs=sel(which, hd), start=True, stop=False)
                            TE.matmul(o, lhsT=sel(SEL_NB, hd), rhs=STc, start=False, stop=False)
                        else:
                            TE.matmul(o, lhsT=sel(which, hd), rhs=STc, start=True, stop=False)
                            TE.matmul(o, lhsT=STc, rhs=sel(SEL_NB, hd), start=False, stop=False)
                        ins = TE.matmul(o, lhsT=ident_bf, rhs=C(mask, bf=True), start=False, stop=True)
                    return ins
                PE.op(f, reads=[B_STT], writes=[B_pB[bank]])

            dmat(2, SEL_A, "MASK_LS", True)
            ACT.op(lambda: A.activation(out=Eb[0][:], in_=pBh[2], func=AF.Exp), reads=[B_pB[2]], writes=[B_Eb[0]])
            DVE.op(lambda: V.scalar_tensor_tensor(out=Xb[0][:], in0=pBh[0], scalar=-1.0, op0=ALU.mult, in1=Eb[0][:],
                                                  op1=ALU.mult), reads=[B_pB[0], B_Eb[0]], writes=[B_X[0]])
            dmat(3, SEL_A, "MASK_US", False)
            ACT.op(lambda: A.activation(out=Eb[1][:], in_=pBh[3], func=AF.Exp), reads=[B_pB[3]], writes=[B_Eb[1]])
            DVE.op(lambda: V.scalar_tensor_tensor(out=Yb[0][:], in0=pBh[0], scalar=-1.0, op0=ALU.mult, in1=Eb[1][:],
                                                  op1=ALU.mult), reads=[B_pB[0], B_Eb[1]], writes=[B_Y[0]])
            dmat(2, SEL_AP, "MASK_UI", False)
            ACT.op(lambda: A.activation(out=Eb[0][:], in_=pBh[2], func=AF.Exp), reads=[B_pB[2]], writes=[B_Eb[0]])
            DVE.op(lambda: V.tensor_tensor(out=g.intraT[:], in0=pBh[1], in1=Eb[0][:], op=ALU.mult),
                   reads=[B_pB[1], B_Eb[0]], writes=[g.B_iT])

            def fb():
                for hd in range(4):
                    ins = TE.matmul(pB[3][:, hd * 128:(hd + 1) * 128], lhsT=sel(SEL_AP, hd), rhs=STc,
                                    start=True, stop=True)
                return ins
            PE.op(fb, reads=[B_STT], writes=[B_pB[3]])
            ACT.op(lambda: A.activation(out=Eb[1][:], in_=pBh[3], func=AF.Exp), reads=[B_pB[3]], writes=[B_Eb[1]])
            POOL.op(lambda: G.tensor_tensor(out=g.qdT[:], in0=qT[:, :, cs], in1=Eb[1][:], op=ALU.mult),
                    reads=B_dT[0] + [B_Eb[1]], writes=[g.B_qdT])
            if first:
                dump("X0", Xb[0][:, 0, :], [B_X[0]])
                dump("Y0", Yb[0][:, 0, :], [B_Y[0]])
                dump("intraT", g.intraT[:, 0, :], [g.B_iT])
                dump("qdT", g.qdT[:, 0, :], [g.B_qdT])
                dump("kbg", g.kbg[:, 0, :], [g.B_kbg])

            POOL.op(lambda: G.tensor_tensor(out=Pb[0][:], in0=Yb[0][:],
                                            in1=bc(ident_bf.unsqueeze(1), [128, 4, 128]), op=ALU.add),
                    reads=[B_Y[0]], writes=[B_P[0]])
            cur = 0
            pc = 0
            for lvl in range(1, 7):
                nxt = 1 - cur

                def sqx(cur=cur):
                    for hd in range(4):
                        ins = TE.matmul(pB[0][:, hd * 128:(hd + 1) * 128], lhsT=Yb[cur][:, hd, :], rhs=Xb[cur][:, hd, :],
                                        start=True, stop=True)
                    return ins
                PE.op(sqx, reads=[B_X[cur], B_Y[cur]], writes=[B_pB[0]])
                if lvl < 6:
                    def sqy(cur=cur):
                        for hd in range(4):
                            ins = TE.matmul(pB[1][:, hd * 128:(hd + 1) * 128], lhsT=Xb[cur][:, hd, :],
                                            rhs=Yb[cur][:, hd, :], start=True, stop=True)
                        return ins
                    PE.op(sqy, reads=[B_X[cur], B_Y[cur]], writes=[B_pB[1]])
                ACT.op(lambda nxt=nxt: A.copy(out=Xb[nxt][:], in_=pBh[0]), reads=[B_pB[0]], writes=[B_X[nxt]])
                if lvl < 6:
                    DVE.op(lambda nxt=nxt: V.tensor_copy(out=Yb[nxt][:], in_=pBh[1]), reads=[B_pB[1]],
                           writes=[B_Y[nxt]])
                pn = 1 - pc

                def pm(nxt=nxt, pc=pc):
                    for hd in range(4):
                        ins = TE.matmul(pB[2][:, hd * 128:(hd + 1) * 128], lhsT=Xb[nxt][:, hd, :], rhs=Pb[pc][:, hd, :],
                                        start=True, stop=True)
                    return ins
                PE.op(pm, reads=[B_X[nxt], B_P[pc]], writes=[B_pB[2]])
                DVE.op(lambda pn=pn, pc=pc: V.tensor_tensor(out=Pb[pn][:], in0=pBh[2], in1=Pb[pc][:], op=ALU.add),
                       reads=[B_pB[2], B_P[pc]], writes=[B_P[pn]])
                cur = nxt
                pc = pn
            TT = Pb[pc]
            B_TT = B_P[pc]
            if first:
                dump("TT", TT[:, 0, :], [B_TT])

            def uw():
                for hd in range(4):
                    TE.matmul(pB[0][:, hd * 128:(hd + 1) * 128], lhsT=TT[:, hd, :], rhs=g.vb[:, hd, :],
                              start=True, stop=True)
                for hd in range(4):
                    ins = TE.matmul(pB[1][:, hd * 128:(hd + 1) * 128], lhsT=g.kbg[:, hd, :], rhs=TT[:, hd, :],
                                    start=True, stop=True)
                return ins
            PE.op(uw, reads=[B_TT, g.B_vb, g.B_kbg], writes=[B_pB[0], B_pB[1]])
            ACT.op(lambda: A.copy(out=g.u_sb[:], in_=pBh[0]), reads=[B_pB[0]], writes=[g.B_u])
            DVE.op(lambda: V.tensor_copy(out=g.wT[:], in_=pBh[1]), reads=[B_pB[1]], writes=[g.B_wT])

            def ws():
                for hd in range(4):
                    ins = TE.matmul(pB[3][:, hd * 128:(hd + 1) * 128], lhsT=g.wT[:, hd, :], rhs=Sbf[:, hd, :],
                                    start=True, stop=True)
                return ins
            PE.op(ws, reads=[g.B_wT, B_Sbf], writes=[B_pB[3]])
            DVE.op(lambda: V.scalar_tensor_tensor(out=g.vnew[:], in0=pBh[3], scalar=-1.0, op0=ALU.mult, in1=g.u_sb[:],
                                                  op1=ALU.add), reads=[B_pB[3], g.B_u], writes=[g.B_vn])

            def oo():
                for hd in range(4):
                    TE.matmul(pB[2][:, hd * 128:(hd + 1) * 128], lhsT=g.qdT[:, hd, :], rhs=Sbf[:, hd, :],
                              start=True, stop=False)
                    TE.matmul(pB[2][:, hd * 128:(hd + 1) * 128], lhsT=g.intraT[:, hd, :], rhs=g.vnew[:, hd, :],
                              start=False, stop=True)
                for hd in range(4):
                    ins = TE.matmul(pB[0][:, hd * 128:(hd + 1) * 128], lhsT=g.kdec[:, hd, :], rhs=g.vnew[:, hd, :],
                                    start=True, stop=True)
                return ins
            PE.op(oo, reads=[g.B_qdT, B_Sbf, g.B_iT, g.B_vn, g.B_kdec], writes=[B_pB[2], B_pB[0]])
            DVE.op(lambda: V.tensor_tensor(out=Sst[:], in0=Sst[:],
                                           in1=bc(g.ex_gl[:, tt, :].unsqueeze(2), [128, 4, 128]), op=ALU.mult),
                   reads=[B_st], writes=[B_S])
            DVE.op(lambda: V.tensor_tensor(out=Sst[:], in0=pBh[0], in1=Sst[:], op=ALU.add),
                   reads=[B_pB[0]], writes=[B_S])
            POOL.op(lambda: G.tensor_copy(out=Sbf[:], in_=Sst[:]), reads=[B_S], writes=[B_Sbf])
            ACT.op(lambda: A.activation(out=g.osq[:], in_=pBh[2], func=AF.Square), reads=[B_pB[2]], writes=[g.B_osq])
            DVE.op(lambda: V.tensor_reduce(out=g.ost[:, 0:4], in_=g.osq[:], op=ALU.add, axis=AX),
                   reads=[g.B_osq], writes=[g.B_ost])
            ACT.op(lambda: A.activation(out=g.ost[:, 4:8], in_=g.ost[:, 0:4], func=AF.Ln, scale=1.0 / 128, bias=EPS),
                   reads=[g.B_ost], writes=[g.B_ost])
            ACT.op(lambda: A.activation(out=g.ost[:, 8:12], in_=g.ost[:, 4:8], func=AF.Exp, scale=-0.5),
                   reads=[g.B_ost], writes=[g.B_ost])
            DVE.op(lambda: V.tensor_tensor(out=g.osq[:], in0=pBh[2],
                                           in1=bc(g.ost[:, 8:12].unsqueeze(2), [128, 4, 128]), op=ALU.mult),
                   reads=[B_pB[2], g.B_ost], writes=[g.B_osq])
            POOL.op(lambda: G.tensor_tensor(out=mixed[:, tt, 512:1024], in0=g.osq[:].rearrange("p h d -> p (h d)"),
                                            in1=zw[:, tt, :], op=ALU.mult),
                    reads=[g.B_osq, B_zw[tt]], writes=[B_mx[tt][1]])
            if first:
                dump("u", g.u_sb[:, 0, :], [g.B_u])
                dump("gdn_out", mixed[:, 0, 512:1024], [B_mx[0][1]])

        def attn_block(s, b):
            t0 = b * BT
            DVE.op(lambda: V.tensor_reduce(out=kbar[:, :, b:b + 1], in_=akT[:, :, t0:t0 + BT].unsqueeze(2),
                                           op=ALU.add, axis=AX),
                   reads=[B_ak[p][b] for p in range(4)], writes=[B_kbar])
            DVE.op(lambda: V.tensor_scalar(out=kbar[:, :, b:b + 1], in0=kbar[:, :, b:b + 1],
                                           scalar1=1.0 / 256, scalar2=None, op0=ALU.mult),
                   reads=[B_kbar], writes=[B_kbar])
            DVE.op(lambda: V.tensor_copy(out=kbar_hi[:], in_=kbar[:]), reads=[B_kbar], writes=[B_kbar])
            DVE.op(lambda: V.tensor_copy(out=kbar_t[:], in_=kbar_hi[:]), reads=[B_kbar], writes=[B_kbar])
            DVE.op(lambda: V.tensor_tensor(out=kbar_t[:], in0=kbar[:], in1=kbar_t[:], op=ALU.subtract),
                   reads=[B_kbar], writes=[B_kbar])
            DVE.op(lambda: V.tensor_copy(out=kbar_lo[:], in_=kbar_t[:]), reads=[B_kbar], writes=[B_kbar])
            for tt in range(TPB):
                attn_tile(s, b, tt)

        def attn_tile(s, b, tt):
            gt = b * TPB + tt
            ob = b
            qs = slice(tt * 128, (tt + 1) * 128)
            use_sel = ob >= 4
            if use_sel:
                def gate():
                    for hd in range(8):
                        p, r0 = hd // 2, 64 * (hd % 2)
                        o = (pS if hd % 2 == 0 else pA[0])[:, 128 + p * 8:136 + p * 8]
                        TE.matmul(o, lhsT=aqT[r0:r0 + 64, p, qs], rhs=kbar_hi[r0:r0 + 64, p, :], start=True, stop=False)
                        ins = TE.matmul(o, lhsT=aqT[r0:r0 + 64, p, qs], rhs=kbar_lo[r0:r0 + 64, p, :], start=False,
                                        stop=True)
                    return ins
                PE.op(gate, reads=B_aq + [B_kbar], writes=[B_pS, B_pA[0]])
                obm = C("OBM", sub=((ob - 4) * 64, 64)).rearrange("p (a two j) -> p a two j", two=2, j=8)
                gmv = gm[:].rearrange("p (a two) j -> p a two j", two=2)
                DVE.op(lambda: V.tensor_tensor(out=gmv[:, :, 0, :],
                                               in0=pS[:, 128:160].rearrange("p (a j) -> p a j", a=4),
                                               in1=obm[:, :, 0, :], op=ALU.add), reads=[B_pS], writes=[B_gm])
                DVE.op(lambda: V.tensor_tensor(out=gmv[:, :, 1, :],
                                               in0=pA[0][:, 128:160].rearrange("p (a j) -> p a j", a=4),
                                               in1=obm[:, :, 1, :], op=ALU.add), reads=[B_pA[0]], writes=[B_gm])
                for hd in range(8):
                    DVE.op(lambda hd=hd: V.max(out=top8[:, hd, :], in_=gm[:, hd, :]), reads=[B_gm], writes=[B_gm])
                DVE.op(lambda: V.tensor_tensor(out=gm[:], in0=gm[:], in1=bc(top8[:, :, 2:3], [128, 8, 8]),
                                               op=ALU.is_ge), reads=[B_gm], writes=[B_gm])
                DVE.op(lambda: V.tensor_scalar(out=selb[:, :, 0:8], in0=gm[:], scalar1=-NEGA, scalar2=NEGA, op0=ALU.mult,
                                               op1=ALU.add), reads=[B_gm], writes=[B_gm])
                DVE.op(lambda: V.tensor_copy(out=selb[:, :, 64:72], in_=selb[:, :, 0:8]), reads=[B_gm], writes=[B_gm])

                def trsel():
                    for hd in range(8):
                        ins = TE.transpose(out=pT[0:72, hd * 128:(hd + 1) * 128], in_=selb[:, hd, :], identity=ident_bf)
                    return ins
                PE.op(trsel, reads=[B_gm], writes=[B_pT])
                DVE.op(lambda: V.tensor_copy(out=selT[:], in_=pT[0:72, :].rearrange("p (h q) -> p h q", h=8)),
                       reads=[B_pT], writes=[B_selT])
                if s == 0 and gt == 8:
                    dump("selb", gm[:].rearrange("p h j -> p (h j)"), [B_gm])

            for half in range(2):
                ob_bank = 2 + half
                for h4 in range(4):
                    hd = half * 4 + h4
                    p, r0 = hd // 2, 64 * (hd % 2)
                    for kt in range(gt + 1):
                        kb = kt // 2
                        d = gt - kt
                        sl = state["pt"] % NPT
                        state["pt"] += 1
                        sb_i = sl % 2
                        sreg = pB[sb_i][:, 0:128]
                        B_s = B_pB[sb_i]
                        kbuf = B_ak[p][kb]
                        diag = (kt == gt)
                        selm = use_sel and kb < ob

                        def qk(p=p, r0=r0, kt=kt, sreg=sreg, diag=diag, selm=selm, kb=kb, hd=hd):
                            last = not (diag or selm)
                            ins = TE.matmul(sreg, lhsT=akT[r0:r0 + 64, p, kt * 128:(kt + 1) * 128],
                                            rhs=aqT[r0:r0 + 64, p, qs], start=True, stop=last)
                            if diag:
                                ins = TE.matmul(sreg, lhsT=ident_bf, rhs=C("CAUS", bf=True), start=False, stop=True)
                            elif selm:
                                ins = TE.matmul(sreg, lhsT=cbf[r0:r0 + 8, IND_O + kb * 128:IND_O + (kb + 1) * 128],
                                                rhs=selT[r0:r0 + 8, hd, :], start=False, stop=True)
                            return ins
                        PE.op(qk, reads=[kbuf, B_aq[p]] + ([B_selT] if selm else []), writes=[B_s])
                        ACT.op(lambda sl=sl, sreg=sreg, hd=hd, d=d: A.activation(
                            out=PTs[:, sl, :], in_=sreg, func=AF.Exp, scale=0.125,
                            bias=C("ALIBI", sub=(hd * 16 + d, 1))), reads=[B_s], writes=[B_PT[sl]])
                        PE.op(lambda sl=sl, kt=kt, hd=hd, h4=h4, ob_bank=ob_bank: TE.matmul(
                            pB[ob_bank][:, h4 * 128:h4 * 128 + 65], lhsT=PTs[:, sl, :], rhs=vtm[:, kt, hd, :],
                            start=(kt == 0), stop=(kt == gt)), reads=[B_PT[sl], B_v[kt]], writes=[B_pB[ob_bank]])
                DVE.op(lambda ob_bank=ob_bank: V.reciprocal(out=rden[:], in_=pBh[ob_bank][:, :, 64]),
                       reads=[B_pB[ob_bank]], writes=[B_att])
                DVE.op(lambda ob_bank=ob_bank: V.tensor_tensor(out=att_t[:], in0=pBh[ob_bank][:, :, 0:64],
                                                               in1=bc(rden[:].unsqueeze(2), [128, 4, 64]), op=ALU.mult),
                       reads=[B_pB[ob_bank], B_att], writes=[B_att])
                POOL.op(lambda half=half: G.tensor_tensor(
                    out=mixed[:, tt, half * 256:(half + 1) * 256], in0=att_t[:].rearrange("p h d -> p (h d)"),
                    in1=siluz[:, tt, half * 256:(half + 1) * 256], op=ALU.mult),
                    reads=[B_att, B_sz[tt]], writes=[B_mx[tt][0]])
            if s == 0 and gt in (0, 8):
                dump(f"attn{gt}", mixed[:, tt, 0:512], [B_mx[tt][0]])

        def out_block(s, b):
            for tt in range(TPB):
                gt = b * TPB + tt
                sl = state["xo"] % 2
                state["xo"] += 1
                dma(SP, ch_xo[sl], xo[sl][:], x[s, gt * 128:(gt + 1) * 128, :], writes=[B_xo[sl]])

                def tr(tt=tt):
                    for kc in range(8):
                        ins = TE.transpose(out=pT[:, kc * 128:(kc + 1) * 128], in_=mixed[:, tt, kc * 128:(kc + 1) * 128],
                                           identity=ident_bf)
                    return ins
                PE.op(tr, reads=B_mx[tt], writes=[B_pT])
                ACT.op(lambda: A.copy(out=mixT[:, 0:4, :], in_=pTh[:, 0:4, :]), reads=[B_pT], writes=[B_mixT])
                DVE.op(lambda: V.tensor_copy(out=mixT[:, 4:8, :], in_=pTh[:, 4:8, :]), reads=[B_pT, B_mixT],
                       writes=[B_mixT])

                def op_():
                    for hf in range(2):
                        for kc in range(8):
                            ins = TE.matmul(pA[hf][:, :], lhsT=mixT[:, kc, :], rhs=w_out_bf[:, kc, hf * 512:(hf + 1) * 512],
                                            start=(kc == 0), stop=(kc == 7))
                    return ins
                PE.op(op_, reads=[B_mixT], writes=B_pA)
                for hf in range(2):
                    ACT.op(lambda hf=hf: A.activation(out=junk[:, 0:512], in_=pA[hf][:, :], func=AF.Square,
                                                      accum_out=small[:, 8 + hf:9 + hf]),
                           reads=[B_pA[hf]], writes=[B_junk, B_small])
                DVE.op(lambda: V.tensor_tensor(out=small[:, 10:11], in0=small[:, 8:9], in1=small[:, 9:10], op=ALU.add),
                       reads=[B_small], writes=[B_small])
                ACT.op(lambda: A.activation(out=small[:, 11:12], in_=small[:, 10:11], func=AF.Ln, scale=1.0 / D,
                                            bias=EPS), reads=[B_small], writes=[B_small])
                ACT.op(lambda: A.activation(out=small[:, 12:13], in_=small[:, 11:12], func=AF.Exp, scale=-0.5),
                       reads=[B_small], writes=[B_small])
                for hf in range(2):
                    DVE.op(lambda hf=hf: V.scalar_tensor_tensor(
                        out=tmpf[hf][:], in0=pA[hf][:, :], scalar=small[:, 12:13], op0=ALU.mult,
                        in1=wpost_bc[:, hf * 512:(hf + 1) * 512], op1=ALU.mult),
                        reads=[B_pA[hf], B_small], writes=[B_tmpf[hf]])
                    POOL.op(lambda hf=hf, sl=sl: G.tensor_tensor(out=xo[sl][:, hf * 512:(hf + 1) * 512],
                                                                 in0=xo[sl][:, hf * 512:(hf + 1) * 512],
                                                                 in1=tmpf[hf][:], op=ALU.add),
                            reads=[B_tmpf[hf], B_xo[sl]], writes=[B_xo[sl]])
                dma(SP, ch_st[sl], y[s, gt * 128:(gt + 1) * 128, :], xo[sl][:], reads=[B_xo[sl]])

        for s in range(nseq):
            for b in range(nblk):
                if os.environ.get("K_STOP") == "w":
                    break
                state["uid"] += 1
                with contextlib.ExitStack() as ea:
                    inproj_block(s, b, ea)
                    barrier()
                if "gdn" in phases:
                    with contextlib.ExitStack() as eb:
                        gdn_block(s, b, eb)
                        barrier()
                if "attn" in phases:
                    attn_block(s, b)
                if "out" in phases:
                    out_block(s, b)
        for ch in ch_st + dbg_chans:
            if ch.count:
                nc.sync.wait_ge(ch.sem, ch.count)
    return nc


def make_params(norm_pre_w, conv_w, a_log, dt_bias, gdn_norm_w, norm_post_w):
    params = np.zeros((128, PW), np.float32)
    params[:, 0:8] = np.asarray(norm_pre_w)[0].reshape(8, 128).T
    cw = np.asarray(conv_w)[0]
    params[:, 8:56] = cw.reshape(4, 12, 128).transpose(2, 0, 1).reshape(128, 48)
    params[:, 56:60] = np.asarray(a_log)[0][None, :]
    params[:, 60:64] = np.asarray(dt_bias)[0][None, :]
    params[:, 64:576] = np.tile(np.asarray(gdn_norm_w)[0], 4)[None, :]
    params[:, 576:1600] = np.asarray(norm_post_w)[0][None, :]
    return params


def kernel(x, norm_pre_w, w_in, conv_w, a_log, dt_bias, gdn_norm_w, w_out, norm_post_w):
    x = np.ascontiguousarray(np.asarray(x, dtype=np.float32))
    consts = make_consts()
    params = make_params(norm_pre_w, conv_w, a_log, dt_bias, gdn_norm_w, norm_post_w)
    w_in0 = np.ascontiguousarray(np.asarray(w_in, dtype=np.float32)[0])
    w_out0 = np.ascontiguousarray(np.asarray(w_out, dtype=np.float32)[0])
    nc = build()
    in_maps = []
    for c in range(NCORES):
        in_maps.append({"x": x[c * NSEQ:(c + 1) * NSEQ], "w_in": w_in0, "w_out": w_out0, "consts": consts,
                        "params": params})
    res = run_bass_kernel_spmd(nc, in_maps, core_ids=list(range(NCORES)))
    return np.concatenate([r["y"] for r in res.results], axis=0)
```

```python
import contextlib
import math
import os

import numpy as np

import concourse.bass as bass
import concourse.mybir as mybir
from concourse.bass_utils import run_bass_kernel_spmd

F32 = mybir.dt.float32
BF16 = mybir.dt.bfloat16
AF = mybir.ActivationFunctionType
ALU = mybir.AluOpType

T = 2048
D = 1024
NCOL = 4104
NSEQ = 4
NCORES = 8
EPS = 1e-6
NEGA = -240000.0
NEGD = -30000.0


class Tok:
    __slots__ = ("sem", "val", "eng")

    def __init__(self, sem, val, eng):
        self.sem, self.val, self.eng = sem, val, eng


class Buf:
    __slots__ = ("name", "w", "r", "excl")

    def __init__(self, name, excl=False):
        self.name = name
        self.w = []
        self.r = []
        self.excl = excl


class Eng:
    def __init__(self, e, sem, name, is_pe=False):
        self.e, self.sem, self.name, self.is_pe = e, sem, name, is_pe
        self.count = 0
        self.waited = {}

    def _wait(self, tok):
        if tok.eng is self and self.is_pe:
            return
        k = id(tok.sem)
        if self.waited.get(k, 0) >= tok.val:
            return
        self.e.wait_ge(tok.sem, tok.val)
        self.waited[k] = tok.val

    def deps(self, reads, writes):
        for b in reads:
            for t in b.w:
                self._wait(t)
            if b.excl:
                for t in b.r:
                    self._wait(t)
        for b in writes:
            for t in b.w:
                self._wait(t)
            for t in b.r:
                self._wait(t)

    def commit(self, tok, reads, writes):
        for b in writes:
            b.w = [tok]
            b.r = []
        for b in reads:
            if b in writes:
                continue
            if b.excl:
                b.w = [tok]
                b.r = []
                continue
            b.r = [t for t in b.r if t.sem is not tok.sem] + [tok]

    def op(self, fn, reads=(), writes=()):
        self.deps(reads, writes)
        ins = fn()
        self.count += 1
        ins.then_inc(self.sem, 1)
        tok = Tok(self.sem, self.count, self)
        self.commit(tok, reads, writes)
        return tok


class Chan:
    def __init__(self, sem):
        self.sem = sem
        self.count = 0


def dma(q, chan, out, in_, reads=(), writes=()):
    q.deps(reads, writes)
    chan.count += 16
    q.e.dma_start(out=out, in_=in_).then_inc(chan.sem, 16)
    tok = Tok(chan.sem, chan.count, None)
    q.commit(tok, reads, writes)
    return tok


class _Cols:
    def __init__(self):
        self.n = 0
        self.d = {}

    def add(self, name, w):
        self.d[name] = (self.n, w)
        self.n += w
        return self.d[name]


def _const_layout():
    c = _Cols()
    c.add("IDENT", 128)
    c.add("TRI", 128)
    c.add("ONES", 128)
    c.add("ALIBI", 8 * 16)
    c.add("OBM", 4 * 64)
    c.add("MASK_LS", 128)
    c.add("MASK_US", 128)
    c.add("MASK_UI", 128)
    c.add("CAUS", 128)
    c.add("SEL", 12 * 128)
    c.add("IND", 8 * 128)
    return c


CL = _const_layout()
SEL_A, SEL_AP, SEL_NB = 0, 1, 2


def make_consts():
    c = np.zeros((128, CL.n), np.float32)
    p = np.arange(128)[:, None]
    f = np.arange(128)[None, :]

    def put(name, arr):
        o, w = CL.d[name]
        c[:, o:o + w] = arr

    put("IDENT", (p == f).astype(np.float32))
    put("TRI", (p <= f).astype(np.float32))
    put("ONES", np.ones((128, 128), np.float32))
    put("MASK_LS", np.where(p > f, 0.0, NEGD))
    put("MASK_US", np.where(f > p, 0.0, NEGD))
    put("MASK_UI", np.where(f >= p, 0.0, NEGD))
    put("CAUS", np.where(f >= p, 0.0, NEGA))
    slopes = (2.0 ** (-8.0 / 8)) ** np.arange(1, 9)
    al = np.zeros((128, 8, 16), np.float32)
    for h in range(8):
        for d in range(16):
            al[:, h, d] = -slopes[h] * (128 * d + 127 - np.arange(128))
    put("ALIBI", al.reshape(128, 128))
    sel = np.zeros((128, 12, 128), np.float32)
    for h in range(4):
        for sp in range(3):
            sel[sp * 12 + 0 * 4 + h, SEL_A * 4 + h, :] = 1.0
            sel[sp * 12 + 2 * 4 + h, SEL_AP * 4 + h, :] = 1.0
            sel[sp * 12 + 1 * 4 + h, SEL_NB * 4 + h, :] = -1.0
    put("SEL", sel.reshape(128, 12 * 128))
    ind = np.zeros((128, 8, 128), np.float32)
    for kb in range(8):
        ind[kb, kb, :] = 1.0
        ind[64 + kb, kb, :] = 1.0
    put("IND", ind.reshape(128, 8 * 128))
    obm = np.zeros((128, 4, 8, 8), np.float32)
    for ob in range(4, 8):
        obm[:, ob - 4, :, ob:] = -1e30
    put("OBM", obm.reshape(128, 256))
    return c


PW = 8 + 48 + 4 + 4 + 512 + 1024
LN_QS = math.log(128.0 ** -0.5)
BT = 256
TPB = 2
NB = T // BT
CF32 = 768


class NS:
    pass


def build(nseq=NSEQ, nblk=NB, dbg=None, phases=("gdn", "attn", "out")):
    nc = bass.Bass("TRN2", target_bir_lowering=False)
    dbg = dbg or {}
    x = nc.dram_tensor("x", [nseq, T, D], F32, kind="ExternalInput").ap()
    w_in = nc.dram_tensor("w_in", [D, NCOL], F32, kind="ExternalInput").ap()
    w_out = nc.dram_tensor("w_out", [D, D], F32, kind="ExternalInput").ap()
    consts = nc.dram_tensor("consts", [128, CL.n], F32, kind="ExternalInput").ap()
    params = nc.dram_tensor("params", [128, PW], F32, kind="ExternalInput").ap()
    y = nc.dram_tensor("y", [nseq, T, D], F32, kind="ExternalOutput").ap()
    dbg_aps = {}
    for name, shape in dbg.items():
        dbg_aps[name] = nc.dram_tensor("dbg_" + name, list(shape), F32, kind="ExternalOutput").ap()
    AX = mybir.AxisListType.X

    with contextlib.ExitStack() as es:
        def sb(name, shape, dt=F32, st=None):
            return (st or es).enter_context(nc.sbuf_tensor(name, list(shape), dt))

        def ps(name, shape, dt=F32):
            return es.enter_context(nc.psum_tensor(name, list(shape), dt))

        def sem(name):
            return es.enter_context(nc.semaphore(name))

        PE = Eng(nc.tensor, sem("s_pe"), "pe", is_pe=True)
        ACT = Eng(nc.scalar, sem("s_act"), "act")
        DVE = Eng(nc.vector, sem("s_dve"), "dve")
        POOL = Eng(nc.gpsimd, sem("s_pool"), "pool")
        SP = Eng(nc.sync, sem("s_sp"), "sp")
        V, G, A, TE = nc.vector, nc.gpsimd, nc.scalar, nc.tensor

        def chan(name):
            return Chan(sem(name))

        pA = [ps(f"pA{i}", [128, 512]) for i in range(2)]
        B_pA = [Buf("pA0", True), Buf("pA1", True)]
        pB = [ps(f"pB{i}", [128, 512]) for i in range(4)]
        B_pB = [Buf(f"pB{i}", True) for i in range(4)]
        pT = ps("pT", [128, 1024], BF16)
        B_pT = Buf("pT", True)
        pS = ps("pS", [128, 512])
        B_pS = Buf("pS", True)
        scr = sb("scr", [128, 8])
        scrb = sb("scrb", [128, 8], BF16)
        dbg_chans = []

        B_scr = Buf("scr")

        def barrier():
            b = B_scr
            PE.op(lambda: TE.matmul(pS[0:8, 510:512], lhsT=scrb[0:8, 0:8], rhs=scrb[0:8, 0:2], start=True, stop=True),
                  reads=[b], writes=[B_pS])
            ACT.op(lambda: A.copy(out=scr[0:1, 0:1], in_=scr[0:1, 0:1]), reads=[b, B_pS], writes=[b])
            DVE.op(lambda: V.tensor_copy(out=scr[0:1, 1:2], in_=scr[0:1, 1:2]), reads=[b], writes=[b])
            POOL.op(lambda: G.tensor_copy(out=scr[0:1, 2:3], in_=scr[0:1, 2:3]), reads=[b], writes=[b])
            for e in (PE, ACT, DVE, SP):
                e.deps([b], [])
            for ch in dbg_chans:
                nc.sync.wait_ge(ch.sem, ch.count)

        POOL.op(lambda: G.memset(scrb[:], 0.0), writes=[B_scr])
        POOL.op(lambda: G.memset(scr[:], 0.0), writes=[B_scr])

        cf = sb("cf", [128, CF32])
        cbf = sb("cbf", [128, CL.n], BF16)
        prm = sb("prm", [128, PW])
        w_in_bf = sb("w_in_bf", [128, 8, NCOL], BF16)
        w_out_bf = sb("w_out_bf", [128, 8, D], BF16)
        HALF = NCOL // 2
        B_c = Buf("consts")
        ch_c = chan("c_c")
        dma(SP, ch_c, cf[:], consts[:, 0:CF32], writes=[B_c])
        dma(SP, ch_c, prm[:], params[:, :], writes=[B_c])
        B_c.w = [B_c.w[-1]]
        npw = prm[:, 0:8]
        convw = prm[:, 8:56]
        alog_bc = prm[:, 56:60]
        dtb_bc = prm[:, 60:64]
        gnw_bc = prm[:, 64:576]
        wpost_bc = prm[:, 576:1600]
        with contextlib.ExitStack() as es2:
            stg = [sb(f"stg{i}", [128, HALF], st=es2) for i in range(2)]
            B_stg = [Buf("stg0"), Buf("stg1")]
            ch_stg = [chan("c_stg0"), chan("c_stg1")]
            i = 0
            hh = CL.n // 2
            for hf in range(2):
                sl = i % 2
                dma(SP, ch_stg[sl], stg[sl][:, 0:hh], consts[:, hf * hh:(hf + 1) * hh], writes=[B_stg[sl]])
                DVE.op(lambda sl=sl, hf=hf: V.tensor_copy(out=cbf[:, hf * hh:(hf + 1) * hh], in_=stg[sl][:, 0:hh]),
                       reads=[B_stg[sl]], writes=[Buf("t")])
                i += 1
            for kc in range(8):
                for hf in range(2):
                    sl = i % 2
                    dma(SP, ch_stg[sl], stg[sl][:], w_in[kc * 128:(kc + 1) * 128, hf * HALF:(hf + 1) * HALF],
                        writes=[B_stg[sl]])
                    if sl == 0:
                        ACT.op(lambda sl=sl, kc=kc, hf=hf: A.activation(
                            out=w_in_bf[:, kc, hf * HALF:(hf + 1) * HALF], in_=stg[sl][:], func=AF.Copy,
                            scale=npw[:, kc:kc + 1]), reads=[B_stg[sl], B_c], writes=[Buf("t")])
                    else:
                        DVE.op(lambda sl=sl, kc=kc, hf=hf: V.tensor_scalar(
                            out=w_in_bf[:, kc, hf * HALF:(hf + 1) * HALF], in0=stg[sl][:],
                            scalar1=npw[:, kc:kc + 1], scalar2=None, op0=ALU.mult),
                            reads=[B_stg[sl], B_c], writes=[Buf("t")])
                    i += 1
            for kc in range(8):
                sl = i % 2
                dma(SP, ch_stg[sl], stg[sl][:, 0:D], w_out[kc * 128:(kc + 1) * 128, :], writes=[B_stg[sl]])
                if sl == 0:
                    ACT.op(lambda sl=sl, kc=kc: A.copy(out=w_out_bf[:, kc, :], in_=stg[sl][:, 0:D]),
                           reads=[B_stg[sl]], writes=[Buf("t")])
                else:
                    DVE.op(lambda sl=sl, kc=kc: V.tensor_copy(out=w_out_bf[:, kc, :], in_=stg[sl][:, 0:D]),
                           reads=[B_stg[sl]], writes=[Buf("t")])
                i += 1
            barrier()

        def C(name, bf=False, rows=128, sub=None):
            o, w = CL.d[name]
            t = cbf if bf else cf
            if not bf:
                assert o + w <= CF32
            if sub is not None:
                so, sw = sub
                return t[0:rows, o + so:o + so + sw]
            return t[0:rows, o:o + w]

        IND_O = CL.d["IND"][0]
        ident_bf = C("IDENT", bf=True)
        ident_f = C("IDENT")

        xo = [sb(f"xo{i}", [128, D]) for i in range(2)]
        B_xo = [Buf(f"xo{i}") for i in range(2)]
        ch_xo = [chan(f"c_xo{i}") for i in range(2)]
        ch_st = [chan(f"c_st{i}") for i in range(2)]
        ch_xs = chan("c_xs")
        aqT = sb("aqT", [128, 4, BT], BF16)
        B_aq = [Buf(f"aq{p}") for p in range(4)]
        akT = sb("akT", [128, 4, T], BF16)
        B_ak = [[Buf(f"ak{p}_{b}") for b in range(NB)] for p in range(4)]
        vtm = sb("vtm", [128, 16, 8, 65], BF16)
        B_v = [Buf(f"v{t}") for t in range(16)]
        siluz = sb("siluz", [128, TPB, 512], BF16)
        B_sz = [Buf(f"sz{t}") for t in range(TPB)]
        zw = sb("zw", [128, TPB, 512], BF16)
        B_zw = [Buf(f"zw{t}") for t in range(TPB)]
        sraw = sb("sraw", [128, TPB, 8])
        B_sraw = Buf("sraw")
        dT = [sb(f"dT{k}", [128, 4, BT], BF16) for k in range(3)]
        B_dT = [[Buf(f"dT{k}_{h}") for h in range(4)] for k in range(3)]
        halo = sb("halo", [128, 12, 4], BF16)
        B_halo = [Buf(f"halo{c}") for c in range(12)]
        mixed = sb("mixed", [128, TPB, D], BF16)
        B_mx = [[Buf(f"mx{t}_{i}") for i in range(2)] for t in range(TPB)]
        mixT = sb("mixT", [128, 8, 128], BF16)
        B_mixT = Buf("mixT")
        tmpf = [sb(f"tmpf{i}", [128, 512]) for i in range(2)]
        B_tmpf = [Buf("tmpf0"), Buf("tmpf1")]
        small = sb("small", [128, 64])
        B_small = Buf("small")
        junk = sb("junk", [128, D], BF16)
        B_junk = Buf("junk")
        nea = sb("nea", [128, 4])
        Sst = sb("Sst", [128, 4, 128])
        Sbf = sb("Sbf", [128, 4, 128], BF16)
        B_S, B_Sbf = Buf("S"), Buf("Sbf")
        kbar = sb("kbar", [128, 4, 8])
        kbar_hi = sb("kbar_hi", [128, 4, 8], BF16)
        kbar_lo = sb("kbar_lo", [128, 4, 8], BF16)
        kbar_t = sb("kbar_t", [128, 4, 8])
        B_kbar = Buf("kbar")
        gm = sb("gm", [128, 8, 8])
        top8 = sb("top8", [128, 8, 8])
        selb = sb("selb", [128, 8, 72], BF16)
        B_gm = Buf("gm")
        selT = sb("selT", [72, 8, BT], BF16)
        B_selT = Buf("selT")
        NPT = 4
        PTs = sb("PTs", [128, NPT, BT], BF16)
        B_PT = [Buf(f"PT{i}") for i in range(NPT)]
        B_pSreg = [[Buf(f"pSreg{i}_{j}") for j in range(4)] for i in range(2)]
        att_t = sb("att_t", [128, 4, 64])
        rden = sb("rden", [128, 4])
        B_att = Buf("att_t")

        pBh = [p[:, :].rearrange("p (h d) -> p h d", h=4) for p in pB]
        pTh = pT[:, :].rearrange("p (h d) -> p h d", h=8)

        def bc(ap, shape):
            return ap.to_broadcast(list(shape))

        def merge(dst, srcs):
            for sbuf in srcs:
                dst.w = dst.w + sbuf.w
                dst.r = dst.r + sbuf.r

        POOL.op(lambda: G.memset(vtm[:, :, :, 64:65], 1.0), writes=B_v)
        POOL.op(lambda: G.memset(kbar[:], 0.0), writes=[B_kbar])
        POOL.op(lambda: G.memset(selb[:], 0.0), writes=[B_gm])
        ACT.op(lambda: A.activation(out=nea[:], in_=alog_bc, func=AF.Exp), reads=[B_c], writes=[B_small])
        DVE.op(lambda: V.tensor_scalar(out=nea[:], in0=nea[:], scalar1=-1.0, scalar2=None, op0=ALU.mult),
               reads=[B_small], writes=[B_small])
        barrier()

        dbg_toks = []

        def dump(name, ap, bufs):
            if name not in dbg_aps:
                return
            ch = chan("c_dbg_" + name)
            dbg_chans.append(ch)
            dbg_toks.append(dma(POOL, ch, dbg_aps[name], ap, reads=bufs))

        state = {"xo": 0, "pt": 0, "uid": 0}

        def inproj_block(s, b, ea):
            t0 = b * BT
            uid = state["uid"]
            xs = sb(f"xs_{uid}", [128, D], st=ea)
            xn = sb(f"xn_{uid}", [128, D], BF16, st=ea)
            hT = sb(f"hT_{uid}", [128, 8, BT], BF16, st=ea)
            pre = sb(f"pre_{uid}", [128, 2, BT + 8], BF16, st=ea)
            cdg = sb(f"cdg_{uid}", [128, 2, 4, 128], BF16, st=ea)
            B_xs, B_xn = Buf("xs"), Buf("xn")
            B_hT = [Buf(f"hT{t}") for t in range(TPB)]
            B_pre = [Buf("pre0"), Buf("pre1")]
            B_cdg = [Buf("cdg0"), Buf("cdg1")]
            for tt in range(TPB):
                gt = b * TPB + tt
                dma(SP, ch_xs, xs[:], x[s, gt * 128:(gt + 1) * 128, :], writes=[B_xs])
                ACT.op(lambda: A.activation(out=junk[:], in_=xs[:], func=AF.Square, accum_out=small[:, 0:1]),
                       reads=[B_xs], writes=[B_junk, B_small])
                ACT.op(lambda: A.activation(out=small[:, 1:2], in_=small[:, 0:1], func=AF.Ln,
                                            scale=1.0 / D, bias=EPS), reads=[B_small], writes=[B_small])
                ACT.op(lambda: A.activation(out=small[:, 2:3], in_=small[:, 1:2], func=AF.Exp, scale=-0.5),
                       reads=[B_small], writes=[B_small])
                DVE.op(lambda: V.tensor_scalar(out=xn[:], in0=xs[:], scalar1=small[:, 2:3], scalar2=None,
                                               op0=ALU.mult), reads=[B_xs, B_small], writes=[B_xn])

                if os.environ.get("K_STOP") == "p1a":
                    return

                def tr():
                    for kc in range(8):
                        ins = TE.transpose(out=pT[:, kc * 128:(kc + 1) * 128], in_=xn[:, kc * 128:(kc + 1) * 128],
                                           identity=ident_bf)
                    return ins
                PE.op(tr, reads=[B_xn], writes=[B_pT])
                if os.environ.get("K_STOP") == "p1b":
                    return
                ACT.op(lambda tt=tt: A.copy(out=hT[:, 0:4, tt * 128:(tt + 1) * 128], in_=pTh[:, 0:4, :]),
                       reads=[B_pT], writes=[Buf("t")])
                if os.environ.get("K_STOP") == "p1c":
                    return
                DVE.op(lambda tt=tt: V.tensor_copy(out=hT[:, 4:8, tt * 128:(tt + 1) * 128], in_=pTh[:, 4:8, :]),
                       reads=[B_pT], writes=[B_hT[tt]])
                if os.environ.get("K_STOP") == "p1d":
                    return
            if s == 0 and b == 0:
                dump("hT", hT[:, 0, :], B_hT)

            if os.environ.get("K_STOP") == "p1":
                return
            def fm_chunk(col0, bank):
                def f():
                    for kc in range(8):
                        ins = TE.matmul(pA[bank][:, 0:BT], lhsT=w_in_bf[:, kc, col0:col0 + 128], rhs=hT[:, kc, :],
                                        start=(kc == 0), stop=(kc == 7))
                    return ins
                PE.op(f, reads=B_hT, writes=[B_pA[bank]])

            ci_all = 0
            for p in range(4):
                bank = ci_all % 2
                ci_all += 1
                fm_chunk(0 + p * 128, bank)
                ACT.op(lambda p=p, bank=bank: A.copy(out=aqT[:, p, :], in_=pA[bank][:, 0:BT]),
                       reads=[B_pA[bank]], writes=[B_aq[p]])
            for p in range(4):
                bank = ci_all % 2
                ci_all += 1
                fm_chunk(512 + p * 128, bank)
                DVE.op(lambda p=p, bank=bank: V.tensor_copy(out=akT[:, p, t0:t0 + BT], in_=pA[bank][:, 0:BT]),
                       reads=[B_pA[bank]], writes=[B_ak[p][b]])
            for kind in range(3):
                for hd in range(4):
                    ci = kind * 4 + hd
                    bank = ci_all % 2
                    ci_all += 1
                    fm_chunk(2048 + ci * 128, bank)
                    sl = ci % 2
                    for tap in range(4):
                        DVE.op(lambda sl=sl, tap=tap, ci=ci: V.tensor_scalar(
                            out=cdg[:, sl, tap, :], in0=ident_f, scalar1=convw[:, tap * 12 + ci:tap * 12 + ci + 1],
                            scalar2=None, op0=ALU.mult), writes=[B_cdg[sl]])
                    if b == 0:
                        POOL.op(lambda sl=sl: G.memset(pre[:, sl, 0:4], 0.0), writes=[B_pre[sl]])
                    else:
                        POOL.op(lambda sl=sl, ci=ci: G.tensor_copy(out=pre[:, sl, 0:4], in_=halo[:, ci, :]),
                                reads=[B_halo[ci]], writes=[B_pre[sl]])
                    ACT.op(lambda sl=sl, bank=bank: A.copy(out=pre[:, sl, 4:4 + BT], in_=pA[bank][:, 0:BT]),
                           reads=[B_pA[bank]], writes=[B_pre[sl]])
                    POOL.op(lambda sl=sl, ci=ci: G.tensor_copy(out=halo[:, ci, :], in_=pre[:, sl, BT:BT + 4]),
                            reads=[B_pre[sl]], writes=[B_halo[ci]])
                    cb = ci % 2

                    def cv(sl=sl, cb=cb):
                        for tap in range(4):
                            ins = TE.matmul(pB[cb][:, 0:BT], lhsT=cdg[:, sl, tap, :],
                                            rhs=pre[:, sl, 1 + tap:1 + tap + BT], start=(tap == 0), stop=(tap == 3))
                        return ins
                    PE.op(cv, reads=[B_pre[sl], B_cdg[sl]], writes=[B_pB[cb]])
                    ACT.op(lambda kind=kind, hd=hd, cb=cb: A.activation(out=dT[kind][:, hd, :], in_=pB[cb][:, 0:BT],
                                                                        func=AF.Silu),
                           reads=[B_pB[cb]], writes=[B_dT[kind][hd]])
            if s == 0 and b == 0:
                dump("aqT", aqT[:, 0, :], B_aq)
                dump("dqT", dT[0][:, 0, :], B_dT[0])
                dump("dkT", dT[1][:, 0, :], B_dT[1])

            if os.environ.get("K_STOP") == "p2":
                return
            for tt in range(TPB):
                gt = b * TPB + tt

                def tm(tt=tt):
                    for j, col0 in enumerate((1024, 1536, 3584)):
                        for kc in range(8):
                            ins = TE.matmul(pB[j + 1][:, :], lhsT=hT[:, kc, tt * 128:(tt + 1) * 128],
                                            rhs=w_in_bf[:, kc, col0:col0 + 512], start=(kc == 0), stop=(kc == 7))
                    for kc in range(8):
                        ins = TE.matmul(pS[:, 0:8], lhsT=hT[:, kc, tt * 128:(tt + 1) * 128],
                                        rhs=w_in_bf[:, kc, 4096:4104], start=(kc == 0), stop=(kc == 7))
                    return ins
                PE.op(tm, reads=B_hT, writes=[B_pB[1], B_pB[2], B_pB[3], B_pS])
                ACT.op(lambda gt=gt: A.copy(out=vtm[:, gt, :, 0:64],
                                            in_=pB[1][:, :].rearrange("p (h d) -> p h d", h=8)),
                       reads=[B_pB[1]], writes=[B_v[gt]])
                ACT.op(lambda tt=tt: A.activation(out=siluz[:, tt, :], in_=pB[2][:, :], func=AF.Silu),
                       reads=[B_pB[2]], writes=[B_sz[tt]])
                fsl = tt % 2
                ACT.op(lambda fsl=fsl: A.activation(out=tmpf[fsl][:], in_=pB[3][:, :], func=AF.Silu),
                       reads=[B_pB[3]], writes=[B_tmpf[fsl]])
                POOL.op(lambda tt=tt, fsl=fsl: G.tensor_tensor(out=zw[:, tt, :], in0=tmpf[fsl][:], in1=gnw_bc,
                                                               op=ALU.mult),
                        reads=[B_tmpf[fsl]], writes=[B_zw[tt]])
                DVE.op(lambda tt=tt: V.tensor_copy(out=sraw[:, tt, :], in_=pS[:, 0:8]), reads=[B_pS], writes=[B_sraw])
            if s == 0 and b == 0:
                dump("sraw", sraw[:, 0, :], [B_sraw])
                dump("vtm", vtm[:, 0, 0, :], B_v)
                dump("zw", zw[:, 0, :], B_zw)

        def gdn_block(s, b, eb):
            uid = state["uid"]
            g = NS()

            def gb(name, shape, dt=F32):
                return sb(f"{name}_{uid}", shape, dt, st=eb)
            g.st_lnr = gb("st_lnr", [128, TPB, 8])
            g.st_lnbn = gb("st_lnbn", [128, TPB, 4])
            g.st_g = gb("st_g", [128, TPB, 4])
            g.st_e = gb("st_e", [128, TPB, 8])
            g.ST = gb("ST", [128, TPB, 12])
            g.st_c = gb("st_c", [128, TPB, 4])
            g.ex_a = gb("ex_a", [128, TPB, 4])
            g.ex_c = gb("ex_c", [128, TPB, 4])
            g.ex_b = gb("ex_b", [128, TPB, 4])
            g.ex_gl = gb("ex_gl", [128, TPB, 4])
            g.STs = gb("STs", [128, TPB, 36], BF16)
            g.STr = gb("STr", [128, TPB, 12])
            g.STh = gb("STh", [128, TPB, 12])
            g.STT = gb("STT", [36, TPB, 128], BF16)
            g.sq = [gb(f"sq{i}", [128, 4, BT], BF16) for i in range(2)]
            g.Eb = [gb(f"Eb{i}", [128, 4, 128], BF16) for i in range(2)]
            g.Xb = [gb(f"Xb{i}", [128, 4, 128], BF16) for i in range(2)]
            g.Yb = [gb(f"Yb{i}", [128, 4, 128], BF16) for i in range(2)]
            g.Pb = [gb(f"Pb{i}", [128, 4, 128], BF16) for i in range(2)]
            g.intraT = gb("intraT", [128, 4, 128], BF16)
            g.kbg = gb("kbg", [128, 4, 128], BF16)
            g.kdec = gb("kdec", [128, 4, 128], BF16)
            g.vb = gb("vb", [128, 4, 128], BF16)
            g.qdT = gb("qdT", [128, 4, 128], BF16)
            g.u_sb = gb("u_sb", [128, 4, 128])
            g.wT = gb("wT", [128, 4, 128], BF16)
            g.vnew = gb("vnew", [128, 4, 128], BF16)
            g.osq = gb("osq", [128, 4, 128])
            g.ost = gb("ost", [128, 16])
            g.B_st, g.B_STT = Buf("stats"), Buf("STT")
            g.B_sq = [Buf("sqq"), Buf("sqk")]
            g.B_Eb = [Buf("Eb0"), Buf("Eb1")]
            g.B_X = [Buf("X0"), Buf("X1")]
            g.B_Y = [Buf("Y0"), Buf("Y1")]
            g.B_P = [Buf("P0"), Buf("P1")]
            g.B_iT, g.B_kbg, g.B_kdec, g.B_vb, g.B_qdT = Buf("iT"), Buf("kbg"), Buf("kdec"), Buf("vb"), Buf("qdT")
            g.B_u, g.B_wT, g.B_vn, g.B_osq, g.B_ost = Buf("u"), Buf("wT"), Buf("vn"), Buf("osq"), Buf("ost")
            B_st = g.B_st
            ST, st_lnr, st_lnbn, st_g, st_e, st_c = g.ST, g.st_lnr, g.st_lnbn, g.st_g, g.st_e, g.st_c
            STs, STr, STh, STT = g.STs, g.STr, g.STh, g.STT
            if b == 0:
                DVE.op(lambda: V.memset(Sst[:], 0.0), writes=[B_S])
                POOL.op(lambda: G.memset(Sbf[:], 0.0), writes=[B_Sbf])
            for k in range(2):
                POOL.op(lambda k=k: G.tensor_tensor(out=g.sq[k][:], in0=dT[k][:], in1=dT[k][:], op=ALU.mult),
                        reads=B_dT[k], writes=[g.B_sq[k]])
            ones_col = C("ONES", bf=True, sub=(0, 1))

            def ssq():
                for tt in range(TPB):
                    for k in range(2):
                        for hd in range(4):
                            c0 = 64 + tt * 8 + k * 4 + hd
                            ins = TE.matmul(pS[:, c0:c0 + 1], lhsT=g.sq[k][:, hd, tt * 128:(tt + 1) * 128],
                                            rhs=ones_col, start=True, stop=True)
                return ins
            PE.op(ssq, reads=g.B_sq, writes=[B_pS])
            pS_ssq = pS[:, 64:64 + 8 * TPB].rearrange("p (t k) -> p t k", t=TPB)
            ACT.op(lambda: A.activation(out=st_lnr[:], in_=pS_ssq, func=AF.Ln, bias=EPS),
                   reads=[B_pS], writes=[B_st])
            ACT.op(lambda: A.activation(out=st_e[:, :, 0:4], in_=sraw[:, :, 0:4], func=AF.Exp, scale=-1.0),
                   reads=[B_sraw, B_st], writes=[B_st])
            ACT.op(lambda: A.activation(out=st_lnbn[:], in_=st_e[:, :, 0:4], func=AF.Ln, bias=1.0),
                   reads=[B_st], writes=[B_st])
            DVE.op(lambda: V.tensor_tensor(out=st_e[:, :, 4:8], in0=sraw[:, :, 4:8],
                                           in1=bc(dtb_bc.unsqueeze(1), [128, TPB, 4]), op=ALU.add),
                   reads=[B_sraw, B_st], writes=[B_st])
            ACT.op(lambda: A.activation(out=st_e[:, :, 4:8], in_=st_e[:, :, 4:8], func=AF.Exp),
                   reads=[B_st], writes=[B_st])
            ACT.op(lambda: A.activation(out=st_e[:, :, 4:8], in_=st_e[:, :, 4:8], func=AF.Ln, bias=1.0),
                   reads=[B_st], writes=[B_st])
            DVE.op(lambda: V.tensor_tensor(out=st_g[:], in0=st_e[:, :, 4:8], in1=bc(nea[:].unsqueeze(1), [128, TPB, 4]),
                                           op=ALU.mult), reads=[B_st], writes=[B_st])

            def gcm():
                for tt in range(TPB):
                    TE.matmul(pS[:, 96 + tt * 4:100 + tt * 4], lhsT=C("TRI"), rhs=st_g[:, tt, :], start=True, stop=True)
                    ins = TE.matmul(pS[:, 112 + tt * 4:116 + tt * 4], lhsT=C("ONES"), rhs=st_g[:, tt, :],
                                    start=True, stop=True)
                return ins
            PE.op(gcm, reads=[B_st], writes=[B_pS])
            gc = pS[:, 96:96 + 4 * TPB].rearrange("p (t k) -> p t k", t=TPB)
            gl = pS[:, 112:112 + 4 * TPB].rearrange("p (t k) -> p t k", t=TPB)
            lnrq = st_lnr[:, :, 0:4]
            lnrk = st_lnr[:, :, 4:8]
            DVE.op(lambda: V.scalar_tensor_tensor(out=ST[:, :, 4:8], in0=lnrk, scalar=0.5, op0=ALU.mult, in1=gc,
                                                  op1=ALU.add), reads=[B_st, B_pS], writes=[B_st])
            DVE.op(lambda: V.scalar_tensor_tensor(out=ST[:, :, 0:4], in0=lnrk, scalar=-0.5, op0=ALU.mult, in1=gc,
                                                  op1=ALU.add), reads=[B_st, B_pS], writes=[B_st])
            DVE.op(lambda: V.scalar_tensor_tensor(out=st_c[:], in0=ST[:, :, 4:8], scalar=-1.0, op0=ALU.mult, in1=gl,
                                                  op1=ALU.add), reads=[B_st, B_pS], writes=[B_st])
            DVE.op(lambda: V.tensor_tensor(out=ST[:, :, 0:4], in0=ST[:, :, 0:4], in1=st_lnbn[:], op=ALU.subtract),
                   reads=[B_st], writes=[B_st])
            DVE.op(lambda: V.scalar_tensor_tensor(out=ST[:, :, 8:12], in0=lnrq, scalar=-0.5, op0=ALU.mult, in1=gc,
                                                  op1=ALU.add), reads=[B_st, B_pS], writes=[B_st])
            DVE.op(lambda: V.tensor_scalar(out=ST[:, :, 8:12], in0=ST[:, :, 8:12], scalar1=LN_QS, scalar2=None,
                                           op0=ALU.add), reads=[B_st], writes=[B_st])
            ACT.op(lambda: A.activation(out=g.ex_a[:], in_=ST[:, :, 0:4], func=AF.Exp), reads=[B_st], writes=[B_st])
            ACT.op(lambda: A.activation(out=g.ex_c[:], in_=st_c[:], func=AF.Exp), reads=[B_st], writes=[B_st])
            ACT.op(lambda: A.activation(out=g.ex_b[:], in_=st_lnbn[:], func=AF.Exp, scale=-1.0),
                   reads=[B_st], writes=[B_st])
            ACT.op(lambda: A.activation(out=g.ex_gl[:], in_=gl, func=AF.Exp), reads=[B_st, B_pS], writes=[B_st])
            DVE.op(lambda: V.tensor_copy(out=STs[:, :, 0:12], in_=ST[:]), reads=[B_st], writes=[B_st])
            DVE.op(lambda: V.tensor_copy(out=STh[:], in_=STs[:, :, 0:12]), reads=[B_st], writes=[B_st])
            DVE.op(lambda: V.tensor_tensor(out=STr[:], in0=ST[:], in1=STh[:], op=ALU.subtract),
                   reads=[B_st], writes=[B_st])
            DVE.op(lambda: V.tensor_copy(out=STs[:, :, 12:24], in_=STr[:]), reads=[B_st], writes=[B_st])
            DVE.op(lambda: V.tensor_copy(out=STh[:], in_=STs[:, :, 12:24]), reads=[B_st], writes=[B_st])
            DVE.op(lambda: V.tensor_tensor(out=STr[:], in0=STr[:], in1=STh[:], op=ALU.subtract),
                   reads=[B_st], writes=[B_st])
            DVE.op(lambda: V.tensor_copy(out=STs[:, :, 24:36], in_=STr[:]), reads=[B_st], writes=[B_st])

            def trs():
                for tt in range(TPB):
                    ins = TE.transpose(out=pT[0:36, tt * 128:(tt + 1) * 128], in_=STs[:, tt, :], identity=ident_bf)
                return ins
            PE.op(trs, reads=[B_st], writes=[B_pT])
            ACT.op(lambda: A.copy(out=STT[:], in_=pT[0:36, 0:128 * TPB].rearrange("p (t k) -> p t k", t=TPB)),
                   reads=[B_pT], writes=[g.B_STT])
            if s == 0 and b == 0:
                dump("ST", ST[:, 0, :], [B_st])
                dump("exa", g.ex_a[:, 0, :], [B_st])
                dump("sg", st_g[:, 0, :], [B_st])
            for tt in range(TPB):
                gdn_chunk(s, b, tt, g)

        def gdn_chunk(s, b, tt, g):
            cs = slice(tt * 128, (tt + 1) * 128)
            first = (s == 0 and b == 0 and tt == 0)
            qT, kT, vT = dT[0], dT[1], dT[2]
            STc = g.STT[:, tt, :]
            B_st, B_STT = g.B_st, g.B_STT
            Eb, Xb, Yb, Pb = g.Eb, g.Xb, g.Yb, g.Pb
            B_Eb, B_X, B_Y, B_P = g.B_Eb, g.B_X, g.B_Y, g.B_P

            def sel(which, hd):
                return C("SEL", bf=True, rows=36, sub=((which * 4 + hd) * 128, 128))

            def trkv():
                for hd in range(4):
                    TE.transpose(out=pT[:, hd * 128:(hd + 1) * 128], in_=kT[:, hd, cs], identity=ident_bf)
                for hd in range(4):
                    ins = TE.transpose(out=pT[:, 512 + hd * 128:512 + (hd + 1) * 128], in_=vT[:, hd, cs],
                                       identity=ident_bf)
                return ins
            PE.op(trkv, reads=B_dT[1] + B_dT[2], writes=[B_pT])
            DVE.op(lambda: V.tensor_tensor(out=g.kbg[:], in0=pTh[:, 0:4, :],
                                           in1=bc(g.ex_a[:, tt, :].unsqueeze(2), [128, 4, 128]), op=ALU.mult),
                   reads=[B_pT, B_st], writes=[g.B_kbg])
            DVE.op(lambda: V.tensor_tensor(out=g.kdec[:], in0=pTh[:, 0:4, :],
                                           in1=bc(g.ex_c[:, tt, :].unsqueeze(2), [128, 4, 128]), op=ALU.mult),
                   reads=[B_pT, B_st], writes=[g.B_kdec])
            DVE.op(lambda: V.tensor_tensor(out=g.vb[:], in0=pTh[:, 4:8, :],
                                           in1=bc(g.ex_b[:, tt, :].unsqueeze(2), [128, 4, 128]), op=ALU.mult),
                   reads=[B_pT, B_st], writes=[g.B_vb])

            def kkqk():
                for hd in range(4):
                    TE.matmul(pB[0][:, hd * 128:(hd + 1) * 128], lhsT=kT[:, hd, cs], rhs=kT[:, hd, cs],
                              start=True, stop=True)
                for hd in range(4):
                    ins = TE.matmul(pB[1][:, hd * 128:(hd + 1) * 128], lhsT=kT[:, hd, cs], rhs=qT[:, hd, cs],
                                    start=True, stop=True)
                return ins
            PE.op(kkqk, reads=B_dT[0] + B_dT[1], writes=[B_pB[0], B_pB[1]])

            def dmat(bank, which, mask, lower):
                def f():
                    for hd in range(4):
                        o = pB[bank][:, hd * 128:(hd + 1) * 128]
                        if lower:
                            TE.matmul(o, lhsT=STc, rhs=sel(which, hd), start=True, stop=False)
                            TE.matmul(o, lhsT=sel(SEL_NB, hd), rhs=STc, start=False, stop=False)
                        else:
                            TE.matmul(o, lhsT=sel(which, hd), rhs=STc, start=True, stop=False)
                            TE.matmul(o, lhsT=STc, rhs=sel(SEL_NB, hd), start=False, stop=False)
                        ins = TE.matmul(o, lhsT=ident_bf, rhs=C(mask, bf=True), start=False, stop=True)
                    return ins
                PE.op(f, reads=[B_STT], writes=[B_pB[bank]])

            dmat(2, SEL_A, "MASK_LS", True)
            ACT.op(lambda: A.activation(out=Eb[0][:], in_=pBh[2], func=AF.Exp), reads=[B_pB[2]], writes=[B_Eb[0]])
            DVE.op(lambda: V.scalar_tensor_tensor(out=Xb[0][:], in0=pBh[0], scalar=-1.0, op0=ALU.mult, in1=Eb[0][:],
                                                  op1=ALU.mult), reads=[B_pB[0], B_Eb[0]], writes=[B_X[0]])
            dmat(3, SEL_A, "MASK_US", False)
            ACT.op(lambda: A.activation(out=Eb[1][:], in_=pBh[3], func=AF.Exp), reads=[B_pB[3]], writes=[B_Eb[1]])
            DVE.op(lambda: V.scalar_tensor_tensor(out=Yb[0][:], in0=pBh[0], scalar=-1.0, op0=ALU.mult, in1=Eb[1][:],
                                                  op1=ALU.mult), reads=[B_pB[0], B_Eb[1]], writes=[B_Y[0]])
            dmat(2, SEL_AP, "MASK_UI", False)
            ACT.op(lambda: A.activation(out=Eb[0][:], in_=pBh[2], func=AF.Exp), reads=[B_pB[2]], writes=[B_Eb[0]])
            DVE.op(lambda: V.tensor_tensor(out=g.intraT[:], in0=pBh[1], in1=Eb[0][:], op=ALU.mult),
                   reads=[B_pB[1], B_Eb[0]], writes=[g.B_iT])

            def fb():
                for hd in range(4):
                    ins = TE.matmul(pB[3][:, hd * 128:(hd + 1) * 128], lhsT=sel(SEL_AP, hd), rhs=STc,
                                    start=True, stop=True)
                return ins
            PE.op(fb, reads=[B_STT], writes=[B_pB[3]])
            ACT.op(lambda: A.activation(out=Eb[1][:], in_=pBh[3], func=AF.Exp), reads=[B_pB[3]], writes=[B_Eb[1]])
            POOL.op(lambda: G.tensor_tensor(out=g.qdT[:], in0=qT[:, :, cs], in1=Eb[1][:], op=ALU.mult),
                    reads=B_dT[0] + [B_Eb[1]], writes=[g.B_qdT])
            if first:
                dump("X0", Xb[0][:, 0, :], [B_X[0]])
                dump("Y0", Yb[0][:, 0, :], [B_Y[0]])
                dump("intraT", g.intraT[:, 0, :], [g.B_iT])
                dump("qdT", g.qdT[:, 0, :], [g.B_qdT])
                dump("kbg", g.kbg[:, 0, :], [g.B_kbg])

            POOL.op(lambda: G.tensor_tensor(out=Pb[0][:], in0=Yb[0][:],
                                            in1=bc(ident_bf.unsqueeze(1), [128, 4, 128]), op=ALU.add),
                    reads=[B_Y[0]], writes=[B_P[0]])
            cur = 0
            pc = 0
            for lvl in range(1, 7):
                nxt = 1 - cur

                def sqx(cur=cur):
                    for hd in range(4):
                        ins = TE.matmul(pB[0][:, hd * 128:(hd + 1) * 128], lhsT=Yb[cur][:, hd, :], rhs=Xb[cur][:, hd, :],
                                        start=True, stop=True)
                    return ins
                PE.op(sqx, reads=[B_X[cur], B_Y[cur]], writes=[B_pB[0]])
                if lvl < 6:
                    def sqy(cur=cur):
                        for hd in range(4):
                            ins = TE.matmul(pB[1][:, hd * 128:(hd + 1) * 128], lhsT=Xb[cur][:, hd, :],
                                            rhs=Yb[cur][:, hd, :], start=True, stop=True)
                        return ins
                    PE.op(sqy, reads=[B_X[cur], B_Y[cur]], writes=[B_pB[1]])
                ACT.op(lambda nxt=nxt: A.copy(out=Xb[nxt][:], in_=pBh[0]), reads=[B_pB[0]], writes=[B_X[nxt]])
                if lvl < 6:
                    DVE.op(lambda nxt=nxt: V.tensor_copy(out=Yb[nxt][:], in_=pBh[1]), reads=[B_pB[1]],
                           writes=[B_Y[nxt]])
                pn = 1 - pc

                def pm(nxt=nxt, pc=pc):
                    for hd in range(4):
                        ins = TE.matmul(pB[2][:, hd * 128:(hd + 1) * 128], lhsT=Xb[nxt][:, hd, :], rhs=Pb[pc][:, hd, :],
                                        start=True, stop=True)
                    return ins
                PE.op(pm, reads=[B_X[nxt], B_P[pc]], writes=[B_pB[2]])
                DVE.op(lambda pn=pn, pc=pc: V.tensor_tensor(out=Pb[pn][:], in0=pBh[2], in1=Pb[pc][:], op=ALU.add),
                       reads=[B_pB[2], B_P[pc]], writes=[B_P[pn]])
                cur = nxt
                pc = pn
            TT = Pb[pc]
            B_TT = B_P[pc]
            if first:
                dump("TT", TT[:, 0, :], [B_TT])

            def uw():
                for hd in range(4):
                    TE.matmul(pB[0][:, hd * 128:(hd + 1) * 128], lhsT=TT[:, hd, :], rhs=g.vb[:, hd, :],
                              start=True, stop=True)
                for hd in range(4):
                    ins = TE.matmul(pB[1][:, hd * 128:(hd + 1) * 128], lhsT=g.kbg[:, hd, :], rhs=TT[:, hd, :],
                                    start=True, stop=True)
                return ins
            PE.op(uw, reads=[B_TT, g.B_vb, g.B_kbg], writes=[B_pB[0], B_pB[1]])
            ACT.op(lambda: A.copy(out=g.u_sb[:], in_=pBh[0]), reads=[B_pB[0]], writes=[g.B_u])
            DVE.op(lambda: V.tensor_copy(out=g.wT[:], in_=pBh[1]), reads=[B_pB[1]], writes=[g.B_wT])

            def ws():
                for hd in range(4):
                    ins = TE.matmul(pB[3][:, hd * 128:(hd + 1) * 128], lhsT=g.wT[:, hd, :], rhs=Sbf[:, hd, :],
                                    start=True, stop=True)
                return ins
            PE.op(ws, reads=[g.B_wT, B_Sbf], writes=[B_pB[3]])
            DVE.op(lambda: V.scalar_tensor_tensor(out=g.vnew[:], in0=pBh[3], scalar=-1.0, op0=ALU.mult, in1=g.u_sb[:],
                                                  op1=ALU.add), reads=[B_pB[3], g.B_u], writes=[g.B_vn])

            def oo():
                for hd in range(4):
                    TE.matmul(pB[2][:, hd * 128:(hd + 1) * 128], lhsT=g.qdT[:, hd, :], rhs=Sbf[:, hd, :],
                              start=True, stop=False)
                    TE.matmul(pB[2][:, hd * 128:(hd + 1) * 128], lhsT=g.intraT[:, hd, :], rhs=g.vnew[:, hd, :],
                              start=False, stop=True)
                for hd in range(4):
                    ins = TE.matmul(pB[0][:, hd * 128:(hd + 1) * 128], lhsT=g.kdec[:, hd, :], rhs=g.vnew[:, hd, :],
                                    start=True, stop=True)
                return ins
            PE.op(oo, reads=[g.B_qdT, B_Sbf, g.B_iT, g.B_vn, g.B_kdec], writes=[B_pB[2], B_pB[0]])
            DVE.op(lambda: V.tensor_tensor(out=Sst[:], in0=Sst[:],
                                           in1=bc(g.ex_gl[:, tt, :].unsqueeze(2), [128, 4, 128]), op=ALU.mult),
                   reads=[B_st], writes=[B_S])
            DVE.op(lambda: V.tensor_tensor(out=Sst[:], in0=pBh[0], in1=Sst[:], op=ALU.add),
                   reads=[B_pB[0]], writes=[B_S])
            POOL.op(lambda: G.tensor_copy(out=Sbf[:], in_=Sst[:]), reads=[B_S], writes=[B_Sbf])
            ACT.op(lambda: A.activation(out=g.osq[:], in_=pBh[2], func=AF.Square), reads=[B_pB[2]], writes=[g.B_osq])
            DVE.op(lambda: V.tensor_reduce(out=g.ost[:, 0:4], in_=g.osq[:], op=ALU.add, axis=AX),
                   reads=[g.B_osq], writes=[g.B_ost])
            ACT.op(lambda: A.activation(out=g.ost[:, 4:8], in_=g.ost[:, 0:4], func=AF.Ln, scale=1.0 / 128, bias=EPS),
                   reads=[g.B_ost], writes=[g.B_ost])
            ACT.op(lambda: A.activation(out=g.ost[:, 8:12], in_=g.ost[:, 4:8], func=AF.Exp, scale=-0.5),
                   reads=[g.B_ost], writes=[g.B_ost])
            DVE.op(lambda: V.tensor_tensor(out=g.osq[:], in0=pBh[2],
                                           in1=bc(g.ost[:, 8:12].unsqueeze(2), [128, 4, 128]), op=ALU.mult),
                   reads=[B_pB[2], g.B_ost], writes=[g.B_osq])
            POOL.op(lambda: G.tensor_tensor(out=mixed[:, tt, 512:1024], in0=g.osq[:].rearrange("p h d -> p (h d)"),
                                            in1=zw[:, tt, :], op=ALU.mult),
                    reads=[g.B_osq, B_zw[tt]], writes=[B_mx[tt][1]])
            if first:
                dump("u", g.u_sb[:, 0, :], [g.B_u])
                dump("gdn_out", mixed[:, 0, 512:1024], [B_mx[0][1]])

        def attn_block(s, b):
            t0 = b * BT
            ob = b
            use_sel = ob >= 4
            DVE.op(lambda: V.tensor_reduce(out=kbar[:, :, b:b + 1], in_=akT[:, :, t0:t0 + BT].unsqueeze(2),
                                           op=ALU.add, axis=AX),
                   reads=[B_ak[p][b] for p in range(4)], writes=[B_kbar])
            DVE.op(lambda: V.tensor_scalar(out=kbar[:, :, b:b + 1], in0=kbar[:, :, b:b + 1],
                                           scalar1=1.0 / 256, scalar2=None, op0=ALU.mult),
                   reads=[B_kbar], writes=[B_kbar])
            DVE.op(lambda: V.tensor_copy(out=kbar_hi[:], in_=kbar[:]), reads=[B_kbar], writes=[B_kbar])
            DVE.op(lambda: V.tensor_copy(out=kbar_t[:], in_=kbar_hi[:]), reads=[B_kbar], writes=[B_kbar])
            DVE.op(lambda: V.tensor_tensor(out=kbar_t[:], in0=kbar[:], in1=kbar_t[:], op=ALU.subtract),
                   reads=[B_kbar], writes=[B_kbar])
            DVE.op(lambda: V.tensor_copy(out=kbar_lo[:], in_=kbar_t[:]), reads=[B_kbar], writes=[B_kbar])
            if use_sel:
                for tt in range(TPB):
                    attn_select(s, b, tt)
            nkt = 2 * b + 2
            for half in range(2):
                tiles = [(h4, kt) for h4 in range(4) for kt in range(nkt)]

                def emit_qk(h4, kt):
                    hd = half * 4 + h4
                    p, r0 = hd // 2, 64 * (hd % 2)
                    kb = kt // 2
                    sl = state["pt"] % NPT
                    state["pt"] += 1
                    sb_i = sl % 2
                    B_s = B_pB[sb_i]
                    lastk = (kt == nkt - 1)
                    q0 = 128 if lastk else 0
                    sreg = pB[sb_i][:, q0:256]
                    selm = use_sel and kb < ob

                    def qk():
                        ins = TE.matmul(sreg, lhsT=akT[r0:r0 + 64, p, kt * 128:(kt + 1) * 128],
                                        rhs=aqT[r0:r0 + 64, p, q0:256], start=True, stop=not (selm or kb == ob))
                        if kb == ob:
                            ins = TE.matmul(pB[sb_i][:, q0:q0 + 128], lhsT=ident_bf, rhs=C("CAUS", bf=True),
                                            start=False, stop=True)
                        elif selm:
                            ins = TE.matmul(sreg, lhsT=cbf[r0:r0 + 8, IND_O + kb * 128:IND_O + (kb + 1) * 128],
                                            rhs=selT[r0:r0 + 8, hd, :], start=False, stop=True)
                        return ins
                    PE.op(qk, reads=[B_ak[p][kb], B_aq[p]] + ([B_selT] if selm else []), writes=[B_s])
                    dref = (2 * b + 1) - kt
                    if hd == 0 and not lastk:
                        ACT.op(lambda: A.activation(out=PTs[:, sl, 0:128], in_=pB[sb_i][:, 0:128], func=AF.Exp, scale=0.125,
                                                    bias=C("ALIBI", sub=(hd * 16 + dref - 1, 1))),
                               reads=[B_s], writes=[B_PT[sl]])
                        ACT.op(lambda: A.activation(out=PTs[:, sl, 128:256], in_=pB[sb_i][:, 128:256], func=AF.Exp,
                                                    scale=0.125, bias=C("ALIBI", sub=(hd * 16 + dref, 1))),
                               reads=[B_s], writes=[B_PT[sl]])
                    else:
                        ACT.op(lambda: A.activation(out=PTs[:, sl, q0:256], in_=sreg, func=AF.Exp, scale=0.125,
                                                    bias=C("ALIBI", sub=(hd * 16 + dref, 1))),
                               reads=[B_s], writes=[B_PT[sl]])
                    return sl

                def emit_pv(h4, kt, sl):
                    hd = half * 4 + h4

                    def pv():
                        ins = None
                        for tq in range(TPB):
                            gt = 2 * b + tq
                            if kt > gt:
                                continue
                            ins = TE.matmul(pB[2 + tq][:, h4 * 128:h4 * 128 + 65], lhsT=PTs[:, sl, tq * 128:(tq + 1) * 128],
                                            rhs=vtm[:, kt, hd, :], start=(kt == 0), stop=(kt == gt))
                        return ins
                    PE.op(pv, reads=[B_PT[sl], B_v[kt]], writes=[B_pB[2], B_pB[3]])

                pend = []
                for (h4, kt) in tiles:
                    sl = emit_qk(h4, kt)
                    pend.append((h4, kt, sl))
                    if len(pend) > 1:
                        emit_pv(*pend.pop(0))
                while pend:
                    emit_pv(*pend.pop(0))
                for tq in range(TPB):
                    ob_bank = 2 + tq
                    DVE.op(lambda ob_bank=ob_bank: V.reciprocal(out=rden[:], in_=pBh[ob_bank][:, :, 64]),
                           reads=[B_pB[ob_bank]], writes=[B_att])
                    DVE.op(lambda ob_bank=ob_bank: V.tensor_tensor(out=att_t[:], in0=pBh[ob_bank][:, :, 0:64],
                                                                   in1=bc(rden[:].unsqueeze(2), [128, 4, 64]),
                                                                   op=ALU.mult),
                           reads=[B_pB[ob_bank], B_att], writes=[B_att])
                    POOL.op(lambda half=half, tq=tq: G.tensor_tensor(
                        out=mixed[:, tq, half * 256:(half + 1) * 256], in0=att_t[:].rearrange("p h d -> p (h d)"),
                        in1=siluz[:, tq, half * 256:(half + 1) * 256], op=ALU.mult),
                        reads=[B_att, B_sz[tq]], writes=[B_mx[tq][0]])
            if s == 0 and b in (0, 4):
                dump(f"attn{2 * b}", mixed[:, 0, 0:512], [B_mx[0][0]])

        def attn_select(s, b, tt):
            ob = b
            qs = slice(tt * 128, (tt + 1) * 128)

            def gate():
                for hd in range(8):
                    p, r0 = hd // 2, 64 * (hd % 2)
                    o = (pS if hd % 2 == 0 else pA[0])[:, 128 + p * 8:136 + p * 8]
                    TE.matmul(o, lhsT=aqT[r0:r0 + 64, p, qs], rhs=kbar_hi[r0:r0 + 64, p, :], start=True, stop=False)
                    ins = TE.matmul(o, lhsT=aqT[r0:r0 + 64, p, qs], rhs=kbar_lo[r0:r0 + 64, p, :], start=False,
                                    stop=True)
                return ins
            PE.op(gate, reads=B_aq + [B_kbar], writes=[B_pS, B_pA[0]])
            obm = C("OBM", sub=((ob - 4) * 64, 64)).rearrange("p (a two j) -> p a two j", two=2, j=8)
            gmv = gm[:].rearrange("p (a two) j -> p a two j", two=2)
            DVE.op(lambda: V.tensor_tensor(out=gmv[:, :, 0, :],
                                           in0=pS[:, 128:160].rearrange("p (a j) -> p a j", a=4),
                                           in1=obm[:, :, 0, :], op=ALU.add), reads=[B_pS], writes=[B_gm])
            DVE.op(lambda: V.tensor_tensor(out=gmv[:, :, 1, :],
                                           in0=pA[0][:, 128:160].rearrange("p (a j) -> p a j", a=4),
                                           in1=obm[:, :, 1, :], op=ALU.add), reads=[B_pA[0]], writes=[B_gm])
            for hd in range(8):
                DVE.op(lambda hd=hd: V.max(out=top8[:, hd, :], in_=gm[:, hd, :]), reads=[B_gm], writes=[B_gm])
            DVE.op(lambda: V.tensor_tensor(out=gm[:], in0=gm[:], in1=bc(top8[:, :, 2:3], [128, 8, 8]),
                                           op=ALU.is_ge), reads=[B_gm], writes=[B_gm])
            DVE.op(lambda: V.tensor_scalar(out=selb[:, :, 0:8], in0=gm[:], scalar1=-NEGA, scalar2=NEGA, op0=ALU.mult,
                                           op1=ALU.add), reads=[B_gm], writes=[B_gm])
            DVE.op(lambda: V.tensor_copy(out=selb[:, :, 64:72], in_=selb[:, :, 0:8]), reads=[B_gm], writes=[B_gm])

            def trsel():
                for hd in range(8):
                    ins = TE.transpose(out=pT[0:72, hd * 128:(hd + 1) * 128], in_=selb[:, hd, :], identity=ident_bf)
                return ins
            PE.op(trsel, reads=[B_gm], writes=[B_pT])
            DVE.op(lambda: V.tensor_copy(out=selT[:, :, qs], in_=pT[0:72, :].rearrange("p (h q) -> p h q", h=8)),
                   reads=[B_pT], writes=[B_selT])
            if s == 0 and b == 4 and tt == 0:
                dump("selb", gm[:].rearrange("p h j -> p (h j)"), [B_gm])

        def out_block(s, b):
            for tt in range(TPB):
                gt = b * TPB + tt
                sl = state["xo"] % 2
                state["xo"] += 1
                dma(SP, ch_xo[sl], xo[sl][:], x[s, gt * 128:(gt + 1) * 128, :], writes=[B_xo[sl]])

                def tr(tt=tt):
                    for kc in range(8):
                        ins = TE.transpose(out=pT[:, kc * 128:(kc + 1) * 128], in_=mixed[:, tt, kc * 128:(kc + 1) * 128],
                                           identity=ident_bf)
                    return ins
                PE.op(tr, reads=B_mx[tt], writes=[B_pT])
                ACT.op(lambda: A.copy(out=mixT[:, 0:4, :], in_=pTh[:, 0:4, :]), reads=[B_pT], writes=[B_mixT])
                DVE.op(lambda: V.tensor_copy(out=mixT[:, 4:8, :], in_=pTh[:, 4:8, :]), reads=[B_pT, B_mixT],
                       writes=[B_mixT])

                def op_():
                    for hf in range(2):
                        for kc in range(8):
                            ins = TE.matmul(pA[hf][:, :], lhsT=mixT[:, kc, :], rhs=w_out_bf[:, kc, hf * 512:(hf + 1) * 512],
                                            start=(kc == 0), stop=(kc == 7))
                    return ins
                PE.op(op_, reads=[B_mixT], writes=B_pA)
                for hf in range(2):
                    ACT.op(lambda hf=hf: A.activation(out=junk[:, 0:512], in_=pA[hf][:, :], func=AF.Square,
                                                      accum_out=small[:, 8 + hf:9 + hf]),
                           reads=[B_pA[hf]], writes=[B_junk, B_small])
                DVE.op(lambda: V.tensor_tensor(out=small[:, 10:11], in0=small[:, 8:9], in1=small[:, 9:10], op=ALU.add),
                       reads=[B_small], writes=[B_small])
                ACT.op(lambda: A.activation(out=small[:, 11:12], in_=small[:, 10:11], func=AF.Ln, scale=1.0 / D,
                                            bias=EPS), reads=[B_small], writes=[B_small])
                ACT.op(lambda: A.activation(out=small[:, 12:13], in_=small[:, 11:12], func=AF.Exp, scale=-0.5),
                       reads=[B_small], writes=[B_small])
                for hf in range(2):
                    DVE.op(lambda hf=hf: V.scalar_tensor_tensor(
                        out=tmpf[hf][:], in0=pA[hf][:, :], scalar=small[:, 12:13], op0=ALU.mult,
                        in1=wpost_bc[:, hf * 512:(hf + 1) * 512], op1=ALU.mult),
                        reads=[B_pA[hf], B_small], writes=[B_tmpf[hf]])
                    POOL.op(lambda hf=hf, sl=sl: G.tensor_tensor(out=xo[sl][:, hf * 512:(hf + 1) * 512],
                                                                 in0=xo[sl][:, hf * 512:(hf + 1) * 512],
                                                                 in1=tmpf[hf][:], op=ALU.add),
                            reads=[B_tmpf[hf], B_xo[sl]], writes=[B_xo[sl]])
                dma(SP, ch_st[sl], y[s, gt * 128:(gt + 1) * 128, :], xo[sl][:], reads=[B_xo[sl]])

        for s in range(nseq):
            for b in range(nblk):
                if os.environ.get("K_STOP") == "w":
                    break
                state["uid"] += 1
                with contextlib.ExitStack() as ea:
                    inproj_block(s, b, ea)
                    barrier()
                if "gdn" in phases:
                    with contextlib.ExitStack() as eb:
                        gdn_block(s, b, eb)
                        barrier()
                if "attn" in phases:
                    attn_block(s, b)
                if "out" in phases:
                    out_block(s, b)
        for ch in ch_st + dbg_chans:
            if ch.count:
                nc.sync.wait_ge(ch.sem, ch.count)
    return nc


def make_params(norm_pre_w, conv_w, a_log, dt_bias, gdn_norm_w, norm_post_w):
    params = np.zeros((128, PW), np.float32)
    params[:, 0:8] = np.asarray(norm_pre_w)[0].reshape(8, 128).T
    cw = np.asarray(conv_w)[0]
    params[:, 8:56] = cw.reshape(4, 12, 128).transpose(2, 0, 1).reshape(128, 48)
    params[:, 56:60] = np.asarray(a_log)[0][None, :]
    params[:, 60:64] = np.asarray(dt_bias)[0][None, :]
    params[:, 64:576] = np.tile(np.asarray(gdn_norm_w)[0], 4)[None, :]
    params[:, 576:1600] = np.asarray(norm_post_w)[0][None, :]
    return params


def kernel(x, norm_pre_w, w_in, conv_w, a_log, dt_bias, gdn_norm_w, w_out, norm_post_w):
    x = np.ascontiguousarray(np.asarray(x, dtype=np.float32))
    consts = make_consts()
    params = make_params(norm_pre_w, conv_w, a_log, dt_bias, gdn_norm_w, norm_post_w)
    w_in0 = np.ascontiguousarray(np.asarray(w_in, dtype=np.float32)[0])
    w_out0 = np.ascontiguousarray(np.asarray(w_out, dtype=np.float32)[0])
    nc = build()
    in_maps = []
    for c in range(NCORES):
        in_maps.append({"x": x[c * NSEQ:(c + 1) * NSEQ], "w_in": w_in0, "w_out": w_out0, "consts": consts,
                        "params": params})
    res = run_bass_kernel_spmd(nc, in_maps, core_ids=list(range(NCORES)))
    return np.concatenate([r["y"] for r in res.results], axis=0)
```

```python
import contextlib
import math
import os

import numpy as np

import concourse.bass as bass
import concourse.mybir as mybir
from concourse.bass_utils import run_bass_kernel_spmd

F32 = mybir.dt.float32
BF16 = mybir.dt.bfloat16
AF = mybir.ActivationFunctionType
ALU = mybir.AluOpType

T = 2048
D = 1024
NCOL = 4104
NSEQ = 4
NCORES = 8
EPS = 1e-6
NEGA = -240000.0
NEGD = -30000.0


class Tok:
    __slots__ = ("sem", "val", "eng")

    def __init__(self, sem, val, eng):
        self.sem, self.val, self.eng = sem, val, eng


class Buf:
    __slots__ = ("name", "w", "r", "excl")

    def __init__(self, name, excl=False):
        self.name = name
        self.w = []
        self.r = []
        self.excl = excl


class Eng:
    def __init__(self, e, sem, name, is_pe=False):
        self.e, self.sem, self.name, self.is_pe = e, sem, name, is_pe
        self.count = 0
        self.waited = {}

    def _wait(self, tok):
        if tok.eng is self and self.is_pe:
            return
        k = id(tok.sem)
        if self.waited.get(k, 0) >= tok.val:
            return
        self.e.wait_ge(tok.sem, tok.val)
        self.waited[k] = tok.val

    def deps(self, reads, writes):
        for b in reads:
            for t in b.w:
                self._wait(t)
            if b.excl:
                for t in b.r:
                    self._wait(t)
        for b in writes:
            for t in b.w:
                self._wait(t)
            for t in b.r:
                self._wait(t)

    def commit(self, tok, reads, writes):
        for b in writes:
            b.w = [tok]
            b.r = []
        for b in reads:
            if b in writes:
                continue
            if b.excl:
                b.w = [tok]
                b.r = []
                continue
            b.r = [t for t in b.r if t.sem is not tok.sem] + [tok]

    def op(self, fn, reads=(), writes=()):
        self.deps(reads, writes)
        ins = fn()
        self.count += 1
        ins.then_inc(self.sem, 1)
        tok = Tok(self.sem, self.count, self)
        self.commit(tok, reads, writes)
        return tok


class Chan:
    def __init__(self, sem):
        self.sem = sem
        self.count = 0


def dma(q, chan, out, in_, reads=(), writes=()):
    q.deps(reads, writes)
    chan.count += 16
    q.e.dma_start(out=out, in_=in_).then_inc(chan.sem, 16)
    tok = Tok(chan.sem, chan.count, None)
    q.commit(tok, reads, writes)
    return tok


class _Cols:
    def __init__(self):
        self.n = 0
        self.d = {}

    def add(self, name, w):
        self.d[name] = (self.n, w)
        self.n += w
        return self.d[name]


def _const_layout():
    c = _Cols()
    c.add("IDENT", 128)
    c.add("TRI", 128)
    c.add("ONES", 128)
    c.add("ALIBI", 8 * 16)
    c.add("OBM", 4 * 64)
    c.add("MASK_LS", 128)
    c.add("MASK_US", 128)
    c.add("MASK_UI", 128)
    c.add("CAUS", 128)
    c.add("SEL", 12 * 128)
    c.add("IND", 8 * 128)
    return c


CL = _const_layout()
SEL_A, SEL_AP, SEL_NB = 0, 1, 2


def make_consts():
    c = np.zeros((128, CL.n), np.float32)
    p = np.arange(128)[:, None]
    f = np.arange(128)[None, :]

    def put(name, arr):
        o, w = CL.d[name]
        c[:, o:o + w] = arr

    put("IDENT", (p == f).astype(np.float32))
    put("TRI", (p <= f).astype(np.float32))
    put("ONES", np.ones((128, 128), np.float32))
    put("MASK_LS", np.where(p > f, 0.0, NEGD))
    put("MASK_US", np.where(f > p, 0.0, NEGD))
    put("MASK_UI", np.where(f >= p, 0.0, NEGD))
    put("CAUS", np.where(f >= p, 0.0, NEGA))
    slopes = (2.0 ** (-8.0 / 8)) ** np.arange(1, 9)
    al = np.zeros((128, 8, 16), np.float32)
    for h in range(8):
        for d in range(16):
            al[:, h, d] = -slopes[h] * (128 * d + 127 - np.arange(128))
    put("ALIBI", al.reshape(128, 128))
    sel = np.zeros((128, 12, 128), np.float32)
    for h in range(4):
        for sp in range(3):
            sel[sp * 12 + 0 * 4 + h, SEL_A * 4 + h, :] = 1.0
            sel[sp * 12 + 2 * 4 + h, SEL_AP * 4 + h, :] = 1.0
            sel[sp * 12 + 1 * 4 + h, SEL_NB * 4 + h, :] = -1.0
    put("SEL", sel.reshape(128, 12 * 128))
    ind = np.zeros((128, 8, 128), np.float32)
    for kb in range(8):
        ind[kb, kb, :] = 1.0
        ind[64 + kb, kb, :] = 1.0
    put("IND", ind.reshape(128, 8 * 128))
    obm = np.zeros((128, 4, 8, 8), np.float32)
    for ob in range(4, 8):
        obm[:, ob - 4, :, ob:] = -1e30
    put("OBM", obm.reshape(128, 256))
    return c


PW = 8 + 48 + 4 + 4 + 512 + 1024
LN_QS = math.log(128.0 ** -0.5)
BT = 256
TPB = 2
NB = T // BT
CF32 = 768


class NS:
    pass


def build(nseq=NSEQ, nblk=NB, dbg=None, phases=("gdn", "attn", "out")):
    nc = bass.Bass("TRN2", target_bir_lowering=False)
    dbg = dbg or {}
    x = nc.dram_tensor("x", [nseq, T, D], F32, kind="ExternalInput").ap()
    w_in = nc.dram_tensor("w_in", [D, NCOL], F32, kind="ExternalInput").ap()
    w_out = nc.dram_tensor("w_out", [D, D], F32, kind="ExternalInput").ap()
    consts = nc.dram_tensor("consts", [128, CL.n], F32, kind="ExternalInput").ap()
    params = nc.dram_tensor("params", [128, PW], F32, kind="ExternalInput").ap()
    y = nc.dram_tensor("y", [nseq, T, D], F32, kind="ExternalOutput").ap()
    dbg_aps = {}
    for name, shape in dbg.items():
        dbg_aps[name] = nc.dram_tensor("dbg_" + name, list(shape), F32, kind="ExternalOutput").ap()
    AX = mybir.AxisListType.X

    with contextlib.ExitStack() as es:
        def sb(name, shape, dt=F32, st=None):
            return (st or es).enter_context(nc.sbuf_tensor(name, list(shape), dt))

        def ps(name, shape, dt=F32):
            return es.enter_context(nc.psum_tensor(name, list(shape), dt))

        def sem(name):
            return es.enter_context(nc.semaphore(name))

        PE = Eng(nc.tensor, sem("s_pe"), "pe", is_pe=True)
        ACT = Eng(nc.scalar, sem("s_act"), "act")
        DVE = Eng(nc.vector, sem("s_dve"), "dve")
        POOL = Eng(nc.gpsimd, sem("s_pool"), "pool")
        SP = Eng(nc.sync, sem("s_sp"), "sp")
        V, G, A, TE = nc.vector, nc.gpsimd, nc.scalar, nc.tensor

        def chan(name):
            return Chan(sem(name))

        pA = [ps(f"pA{i}", [128, 512]) for i in range(2)]
        B_pA = [Buf("pA0", True), Buf("pA1", True)]
        pB = [ps(f"pB{i}", [128, 512]) for i in range(4)]
        B_pB = [Buf(f"pB{i}", True) for i in range(4)]
        pT = ps("pT", [128, 1024], BF16)
        B_pT = Buf("pT", True)
        pS = ps("pS", [128, 512])
        B_pS = Buf("pS", True)
        scr = sb("scr", [128, 8])
        scrb = sb("scrb", [128, 8], BF16)
        dbg_chans = []

        B_scr = Buf("scr")

        def barrier():
            b = B_scr
            PE.op(lambda: TE.matmul(pS[0:8, 510:512], lhsT=scrb[0:8, 0:8], rhs=scrb[0:8, 0:2], start=True, stop=True),
                  reads=[b], writes=[B_pS])
            ACT.op(lambda: A.copy(out=scr[0:1, 0:1], in_=scr[0:1, 0:1]), reads=[b, B_pS], writes=[b])
            DVE.op(lambda: V.tensor_copy(out=scr[0:1, 1:2], in_=scr[0:1, 1:2]), reads=[b], writes=[b])
            POOL.op(lambda: G.tensor_copy(out=scr[0:1, 2:3], in_=scr[0:1, 2:3]), reads=[b], writes=[b])
            for e in (PE, ACT, DVE, SP):
                e.deps([b], [])
            for ch in dbg_chans:
                nc.sync.wait_ge(ch.sem, ch.count)

        POOL.op(lambda: G.memset(scrb[:], 0.0), writes=[B_scr])
        POOL.op(lambda: G.memset(scr[:], 0.0), writes=[B_scr])

        cf = sb("cf", [128, CF32])
        cbf = sb("cbf", [128, CL.n], BF16)
        prm = sb("prm", [128, PW])
        w_in_bf = sb("w_in_bf", [128, 8, NCOL], BF16)
        w_out_bf = sb("w_out_bf", [128, 8, D], BF16)
        HALF = NCOL // 2
        B_c = Buf("consts")
        ch_c = chan("c_c")
        dma(SP, ch_c, cf[:], consts[:, 0:CF32], writes=[B_c])
        dma(SP, ch_c, prm[:], params[:, :], writes=[B_c])
        B_c.w = [B_c.w[-1]]
        npw = prm[:, 0:8]
        convw = prm[:, 8:56]
        alog_bc = prm[:, 56:60]
        dtb_bc = prm[:, 60:64]
        gnw_bc = prm[:, 64:576]
        wpost_bc = prm[:, 576:1600]
        with contextlib.ExitStack() as es2:
            stg = [sb(f"stg{i}", [128, HALF], st=es2) for i in range(2)]
            B_stg = [Buf("stg0"), Buf("stg1")]
            ch_stg = [chan("c_stg0"), chan("c_stg1")]
            i = 0
            hh = CL.n // 2
            for hf in range(2):
                sl = i % 2
                dma(SP, ch_stg[sl], stg[sl][:, 0:hh], consts[:, hf * hh:(hf + 1) * hh], writes=[B_stg[sl]])
                DVE.op(lambda sl=sl, hf=hf: V.tensor_copy(out=cbf[:, hf * hh:(hf + 1) * hh], in_=stg[sl][:, 0:hh]),
                       reads=[B_stg[sl]], writes=[Buf("t")])
                i += 1
            for kc in range(8):
                for hf in range(2):
                    sl = i % 2
                    dma(SP, ch_stg[sl], stg[sl][:], w_in[kc * 128:(kc + 1) * 128, hf * HALF:(hf + 1) * HALF],
                        writes=[B_stg[sl]])
                    if sl == 0:
                        ACT.op(lambda sl=sl, kc=kc, hf=hf: A.activation(
                            out=w_in_bf[:, kc, hf * HALF:(hf + 1) * HALF], in_=stg[sl][:], func=AF.Copy,
                            scale=npw[:, kc:kc + 1]), reads=[B_stg[sl], B_c], writes=[Buf("t")])
                    else:
                        DVE.op(lambda sl=sl, kc=kc, hf=hf: V.tensor_scalar(
                            out=w_in_bf[:, kc, hf * HALF:(hf + 1) * HALF], in0=stg[sl][:],
                            scalar1=npw[:, kc:kc + 1], scalar2=None, op0=ALU.mult),
                            reads=[B_stg[sl], B_c], writes=[Buf("t")])
                    i += 1
            for kc in range(8):
                sl = i % 2
                dma(SP, ch_stg[sl], stg[sl][:, 0:D], w_out[kc * 128:(kc + 1) * 128, :], writes=[B_stg[sl]])
                if sl == 0:
                    ACT.op(lambda sl=sl, kc=kc: A.copy(out=w_out_bf[:, kc, :], in_=stg[sl][:, 0:D]),
                           reads=[B_stg[sl]], writes=[Buf("t")])
                else:
                    DVE.op(lambda sl=sl, kc=kc: V.tensor_copy(out=w_out_bf[:, kc, :], in_=stg[sl][:, 0:D]),
                           reads=[B_stg[sl]], writes=[Buf("t")])
                i += 1
            barrier()

        def C(name, bf=False, rows=128, sub=None):
            o, w = CL.d[name]
            t = cbf if bf else cf
            if not bf:
                assert o + w <= CF32
            if sub is not None:
                so, sw = sub
                return t[0:rows, o + so:o + so + sw]
            return t[0:rows, o:o + w]

        IND_O = CL.d["IND"][0]
        ident_bf = C("IDENT", bf=True)
        ident_f = C("IDENT")

        xo = [sb(f"xo{i}", [128, D]) for i in range(2)]
        B_xo = [Buf(f"xo{i}") for i in range(2)]
        ch_xo = [chan(f"c_xo{i}") for i in range(2)]
        ch_st = [chan(f"c_st{i}") for i in range(2)]
        ch_xs = chan("c_xs")
        aqT = sb("aqT", [128, 4, BT], BF16)
        B_aq = [Buf(f"aq{p}") for p in range(4)]
        akT = sb("akT", [128, 4, T], BF16)
        B_ak = [[Buf(f"ak{p}_{b}") for b in range(NB)] for p in range(4)]
        vtm = sb("vtm", [128, 16, 8, 65], BF16)
        B_v = [Buf(f"v{t}") for t in range(16)]
        siluz = sb("siluz", [128, TPB, 512], BF16)
        B_sz = [Buf(f"sz{t}") for t in range(TPB)]
        zw = sb("zw", [128, TPB, 512], BF16)
        B_zw = [Buf(f"zw{t}") for t in range(TPB)]
        sraw = sb("sraw", [128, TPB, 8])
        B_sraw = Buf("sraw")
        dT = [sb(f"dT{k}", [128, 4, BT], BF16) for k in range(3)]
        B_dT = [[Buf(f"dT{k}_{h}") for h in range(4)] for k in range(3)]
        halo = sb("halo", [128, 12, 4])
        B_halo = [Buf(f"halo{c}") for c in range(12)]
        mixed = sb("mixed", [128, TPB, D], BF16)
        B_mx = [[Buf(f"mx{t}_{i}") for i in range(2)] for t in range(TPB)]
        mixT = sb("mixT", [128, 8, 128], BF16)
        B_mixT = Buf("mixT")
        tmpf = [sb(f"tmpf{i}", [128, 512]) for i in range(2)]
        B_tmpf = [Buf("tmpf0"), Buf("tmpf1")]
        small = sb("small", [128, 64])
        B_small = Buf("small")
        junk = sb("junk", [128, D], BF16)
        B_junk = Buf("junk")
        nea = sb("nea", [128, 4])
        Sst = sb("Sst", [128, 4, 128])
        Sbf = sb("Sbf", [128, 4, 128], BF16)
        B_S, B_Sbf = Buf("S"), Buf("Sbf")
        kbar = sb("kbar", [128, 4, 8])
        kbar_hi = sb("kbar_hi", [128, 4, 8], BF16)
        kbar_lo = sb("kbar_lo", [128, 4, 8], BF16)
        kbar_t = sb("kbar_t", [128, 4, 8])
        B_kbar = Buf("kbar")
        gm = sb("gm", [128, 8, 8])
        top8 = sb("top8", [128, 8, 8])
        selb = sb("selb", [128, 8, 72], BF16)
        B_gm = Buf("gm")
        selT = sb("selT", [72, 8, BT], BF16)
        B_selT = Buf("selT")
        NPT = 4
        PTs = sb("PTs", [128, NPT, BT], BF16)
        B_PT = [Buf(f"PT{i}") for i in range(NPT)]
        B_pSreg = [[Buf(f"pSreg{i}_{j}") for j in range(4)] for i in range(2)]
        att_t = sb("att_t", [128, 4, 64])
        rden = sb("rden", [128, 4])
        B_att = Buf("att_t")

        pBh = [p[:, :].rearrange("p (h d) -> p h d", h=4) for p in pB]
        pO = [pB[3], pS]
        B_pO = [B_pB[3], B_pS]
        pOh = [p[:, :].rearrange("p (h d) -> p h d", h=4) for p in pO]
        pTh = pT[:, :].rearrange("p (h d) -> p h d", h=8)

        def bc(ap, shape):
            return ap.to_broadcast(list(shape))

        def merge(dst, srcs):
            for sbuf in srcs:
                dst.w = dst.w + sbuf.w
                dst.r = dst.r + sbuf.r

        POOL.op(lambda: G.memset(vtm[:, :, :, 64:65], 1.0), writes=B_v)
        POOL.op(lambda: G.memset(kbar[:], 0.0), writes=[B_kbar])
        POOL.op(lambda: G.memset(selb[:], 0.0), writes=[B_gm])
        ACT.op(lambda: A.activation(out=nea[:], in_=alog_bc, func=AF.Exp), reads=[B_c], writes=[B_small])
        DVE.op(lambda: V.tensor_scalar(out=nea[:], in0=nea[:], scalar1=-1.0, scalar2=None, op0=ALU.mult),
               reads=[B_small], writes=[B_small])
        barrier()

        dbg_toks = []

        def dump(name, ap, bufs):
            if name not in dbg_aps:
                return
            ch = chan("c_dbg_" + name)
            dbg_chans.append(ch)
            dbg_toks.append(dma(POOL, ch, dbg_aps[name], ap, reads=bufs))

        state = {"xo": 0, "pt": 0, "uid": 0}

        def inproj_block(s, b, ea):
            t0 = b * BT
            uid = state["uid"]
            xs = sb(f"xs_{uid}", [128, D], st=ea)
            xn = sb(f"xn_{uid}", [128, D], BF16, st=ea)
            hT = sb(f"hT_{uid}", [128, 8, BT], BF16, st=ea)
            pre = sb(f"pre_{uid}", [128, 2, BT + 8], st=ea)
            cacc = sb(f"cacc_{uid}", [128, 2, BT], st=ea)
            B_xs, B_xn = Buf("xs"), Buf("xn")
            B_hT = [Buf(f"hT{t}") for t in range(TPB)]
            B_pre = [Buf("pre0"), Buf("pre1")]
            B_cacc = [Buf("cacc0"), Buf("cacc1")]
            for tt in range(TPB):
                gt = b * TPB + tt
                dma(SP, ch_xs, xs[:], x[s, gt * 128:(gt + 1) * 128, :], writes=[B_xs])
                ACT.op(lambda: A.activation(out=junk[:], in_=xs[:], func=AF.Square, accum_out=small[:, 0:1]),
                       reads=[B_xs], writes=[B_junk, B_small])
                ACT.op(lambda: A.activation(out=small[:, 1:2], in_=small[:, 0:1], func=AF.Ln,
                                            scale=1.0 / D, bias=EPS), reads=[B_small], writes=[B_small])
                ACT.op(lambda: A.activation(out=small[:, 2:3], in_=small[:, 1:2], func=AF.Exp, scale=-0.5),
                       reads=[B_small], writes=[B_small])
                DVE.op(lambda: V.tensor_scalar(out=xn[:], in0=xs[:], scalar1=small[:, 2:3], scalar2=None,
                                               op0=ALU.mult), reads=[B_xs, B_small], writes=[B_xn])

                if os.environ.get("K_STOP") == "p1a":
                    return

                def tr():
                    for kc in range(8):
                        ins = TE.transpose(out=pT[:, kc * 128:(kc + 1) * 128], in_=xn[:, kc * 128:(kc + 1) * 128],
                                           identity=ident_bf)
                    return ins
                PE.op(tr, reads=[B_xn], writes=[B_pT])
                if os.environ.get("K_STOP") == "p1b":
                    return
                ACT.op(lambda tt=tt: A.copy(out=hT[:, 0:4, tt * 128:(tt + 1) * 128], in_=pTh[:, 0:4, :]),
                       reads=[B_pT], writes=[Buf("t")])
                if os.environ.get("K_STOP") == "p1c":
                    return
                DVE.op(lambda tt=tt: V.tensor_copy(out=hT[:, 4:8, tt * 128:(tt + 1) * 128], in_=pTh[:, 4:8, :]),
                       reads=[B_pT], writes=[B_hT[tt]])
                if os.environ.get("K_STOP") == "p1d":
                    return
            if s == 0 and b == 0:
                dump("hT", hT[:, 0, :], B_hT)

            if os.environ.get("K_STOP") == "p1":
                return
            def fm_chunk(col0, bank):
                def f():
                    for kc in range(8):
                        ins = TE.matmul(pA[bank][:, 0:BT], lhsT=w_in_bf[:, kc, col0:col0 + 128], rhs=hT[:, kc, :],
                                        start=(kc == 0), stop=(kc == 7))
                    return ins
                PE.op(f, reads=B_hT, writes=[B_pA[bank]])

            ci_all = 0
            for p in range(4):
                bank = ci_all % 2
                ci_all += 1
                fm_chunk(0 + p * 128, bank)
                ACT.op(lambda p=p, bank=bank: A.copy(out=aqT[:, p, :], in_=pA[bank][:, 0:BT]),
                       reads=[B_pA[bank]], writes=[B_aq[p]])
            for p in range(4):
                bank = ci_all % 2
                ci_all += 1
                fm_chunk(512 + p * 128, bank)
                DVE.op(lambda p=p, bank=bank: V.tensor_copy(out=akT[:, p, t0:t0 + BT], in_=pA[bank][:, 0:BT]),
                       reads=[B_pA[bank]], writes=[B_ak[p][b]])
            for kind in range(3):
                for hd in range(4):
                    ci = kind * 4 + hd
                    bank = ci_all % 2
                    ci_all += 1
                    fm_chunk(2048 + ci * 128, bank)
                    sl = ci % 2
                    if b == 0:
                        POOL.op(lambda sl=sl: G.memset(pre[:, sl, 0:4], 0.0), writes=[B_pre[sl]])
                    else:
                        POOL.op(lambda sl=sl, ci=ci: G.tensor_copy(out=pre[:, sl, 0:4], in_=halo[:, ci, :]),
                                reads=[B_halo[ci]], writes=[B_pre[sl]])
                    ACT.op(lambda sl=sl, bank=bank: A.copy(out=pre[:, sl, 4:4 + BT], in_=pA[bank][:, 0:BT]),
                           reads=[B_pA[bank]], writes=[B_pre[sl]])
                    POOL.op(lambda sl=sl, ci=ci: G.tensor_copy(out=halo[:, ci, :], in_=pre[:, sl, BT:BT + 4]),
                            reads=[B_pre[sl]], writes=[B_halo[ci]])
                    DVE.op(lambda sl=sl, ci=ci: V.tensor_scalar(out=cacc[:, sl, :], in0=pre[:, sl, 1:1 + BT],
                                                                scalar1=convw[:, ci:ci + 1], scalar2=None, op0=ALU.mult),
                           reads=[B_pre[sl]], writes=[B_cacc[sl]])
                    for tap in range(1, 4):
                        DVE.op(lambda sl=sl, ci=ci, tap=tap: V.scalar_tensor_tensor(
                            out=cacc[:, sl, :], in0=pre[:, sl, 1 + tap:1 + tap + BT],
                            scalar=convw[:, tap * 12 + ci:tap * 12 + ci + 1], op0=ALU.mult, in1=cacc[:, sl, :],
                            op1=ALU.add), reads=[B_pre[sl]], writes=[B_cacc[sl]])
                    ACT.op(lambda kind=kind, hd=hd, sl=sl: A.activation(out=dT[kind][:, hd, :], in_=cacc[:, sl, :],
                                                                        func=AF.Silu),
                           reads=[B_cacc[sl]], writes=[B_dT[kind][hd]])
            if s == 0 and b == 0:
                dump("aqT", aqT[:, 0, :], B_aq)
                dump("dqT", dT[0][:, 0, :], B_dT[0])
                dump("dkT", dT[1][:, 0, :], B_dT[1])

            if os.environ.get("K_STOP") == "p2":
                return
            for tt in range(TPB):
                gt = b * TPB + tt

                tmb = [(pB[1], B_pB[1]), (pB[2], B_pB[2]), (pB[3], B_pB[3])] if tt % 2 == 0 else \
                      [(pA[0], B_pA[0]), (pA[1], B_pA[1]), (pB[0], B_pB[0])]

                def tm(tt=tt, tmb=tmb):
                    for j, col0 in enumerate((1024, 1536, 3584)):
                        for kc in range(8):
                            ins = TE.matmul(tmb[j][0][:, :], lhsT=hT[:, kc, tt * 128:(tt + 1) * 128],
                                            rhs=w_in_bf[:, kc, col0:col0 + 512], start=(kc == 0), stop=(kc == 7))
                    for kc in range(8):
                        ins = TE.matmul(pS[:, 0:8], lhsT=hT[:, kc, tt * 128:(tt + 1) * 128],
                                        rhs=w_in_bf[:, kc, 4096:4104], start=(kc == 0), stop=(kc == 7))
                    return ins
                PE.op(tm, reads=B_hT, writes=[tmb[0][1], tmb[1][1], tmb[2][1], B_pS])
                ACT.op(lambda gt=gt, tmb=tmb: A.copy(out=vtm[:, gt, :, 0:64],
                                                     in_=tmb[0][0][:, :].rearrange("p (h d) -> p h d", h=8)),
                       reads=[tmb[0][1]], writes=[B_v[gt]])
                ACT.op(lambda tt=tt, tmb=tmb: A.activation(out=siluz[:, tt, :], in_=tmb[1][0][:, :], func=AF.Silu),
                       reads=[tmb[1][1]], writes=[B_sz[tt]])
                fsl = tt % 2
                ACT.op(lambda fsl=fsl, tmb=tmb: A.activation(out=tmpf[fsl][:], in_=tmb[2][0][:, :], func=AF.Silu),
                       reads=[tmb[2][1]], writes=[B_tmpf[fsl]])
                POOL.op(lambda tt=tt, fsl=fsl: G.tensor_tensor(out=zw[:, tt, :], in0=tmpf[fsl][:], in1=gnw_bc,
                                                               op=ALU.mult),
                        reads=[B_tmpf[fsl]], writes=[B_zw[tt]])
                DVE.op(lambda tt=tt: V.tensor_copy(out=sraw[:, tt, :], in_=pS[:, 0:8]), reads=[B_pS], writes=[B_sraw])
            if s == 0 and b == 0:
                dump("sraw", sraw[:, 0, :], [B_sraw])
                dump("vtm", vtm[:, 0, 0, :], B_v)
                dump("zw", zw[:, 0, :], B_zw)

        def gdn_block(s, b, eb):
            uid = state["uid"]
            g = NS()

            def gb(name, shape, dt=F32):
                return sb(f"{name}_{uid}", shape, dt, st=eb)
            g.st_lnr = gb("st_lnr", [128, TPB, 8])
            g.st_lnbn = gb("st_lnbn", [128, TPB, 4])
            g.st_g = gb("st_g", [128, TPB, 4])
            g.st_e = gb("st_e", [128, TPB, 8])
            g.ST = gb("ST", [128, TPB, 12])
            g.st_c = gb("st_c", [128, TPB, 4])
            g.ex_a = gb("ex_a", [128, TPB, 4])
            g.ex_c = gb("ex_c", [128, TPB, 4])
            g.ex_b = gb("ex_b", [128, TPB, 4])
            g.ex_gl = gb("ex_gl", [128, TPB, 4])
            g.STs = gb("STs", [128, TPB, 36], BF16)
            g.STr = gb("STr", [128, TPB, 12])
            g.STh = gb("STh", [128, TPB, 12])
            g.STT = gb("STT", [36, TPB, 128], BF16)
            g.sq = [gb(f"sq{i}", [128, 4, BT], BF16) for i in range(2)]
            g.Eb = [gb(f"Eb{i}", [128, 4, 128], BF16) for i in range(2)]
            g.Xb = [gb(f"Xb{i}", [128, 4, 128], BF16) for i in range(2)]
            g.Yb = [gb(f"Yb{i}", [128, 4, 128], BF16) for i in range(2)]
            g.Pb = [gb(f"Pb{i}", [128, 4, 128], BF16) for i in range(2)]
            g.intraT = gb("intraT", [128, 4, 128], BF16)
            g.kbg = gb("kbg", [128, 4, 128], BF16)
            g.kdec = gb("kdec", [128, 4, 128], BF16)
            g.vb = gb("vb", [128, 4, 128], BF16)
            g.qdT = gb("qdT", [128, 4, 128], BF16)
            g.u_sb = gb("u_sb", [128, 4, 128])
            g.wT = gb("wT", [128, 4, 128], BF16)
            g.vnew = gb("vnew", [128, 4, 128], BF16)
            g.osq = gb("osq", [128, 4, 128])
            g.ost = gb("ost", [128, 16])
            g.B_st, g.B_STT = Buf("stats"), Buf("STT")
            g.B_sq = [Buf("sqq"), Buf("sqk")]
            g.B_Eb = [Buf("Eb0"), Buf("Eb1")]
            g.B_X = [Buf("X0"), Buf("X1")]
            g.B_Y = [Buf("Y0"), Buf("Y1")]
            g.B_P = [Buf("P0"), Buf("P1")]
            g.B_iT, g.B_kbg, g.B_kdec, g.B_vb, g.B_qdT = Buf("iT"), Buf("kbg"), Buf("kdec"), Buf("vb"), Buf("qdT")
            g.B_u, g.B_wT, g.B_vn, g.B_osq, g.B_ost = Buf("u"), Buf("wT"), Buf("vn"), Buf("osq"), Buf("ost")
            B_st = g.B_st
            ST, st_lnr, st_lnbn, st_g, st_e, st_c = g.ST, g.st_lnr, g.st_lnbn, g.st_g, g.st_e, g.st_c
            STs, STr, STh, STT = g.STs, g.STr, g.STh, g.STT
            if b == 0:
                DVE.op(lambda: V.memset(Sst[:], 0.0), writes=[B_S])
                POOL.op(lambda: G.memset(Sbf[:], 0.0), writes=[B_Sbf])
            for k in range(2):
                POOL.op(lambda k=k: G.tensor_tensor(out=g.sq[k][:], in0=dT[k][:], in1=dT[k][:], op=ALU.mult),
                        reads=B_dT[k], writes=[g.B_sq[k]])
            ones_col = C("ONES", bf=True, sub=(0, 1))

            def ssq():
                for tt in range(TPB):
                    for k in range(2):
                        for hd in range(4):
                            c0 = 64 + tt * 8 + k * 4 + hd
                            ins = TE.matmul(pB[0][:, c0:c0 + 1], lhsT=g.sq[k][:, hd, tt * 128:(tt + 1) * 128],
                                            rhs=ones_col, start=True, stop=True)
                return ins
            yield
            PE.op(ssq, reads=g.B_sq, writes=[B_pB[0]])
            pS_ssq = pB[0][:, 64:64 + 8 * TPB].rearrange("p (t k) -> p t k", t=TPB)
            ACT.op(lambda: A.activation(out=st_lnr[:], in_=pS_ssq, func=AF.Ln, bias=EPS),
                   reads=[B_pB[0]], writes=[B_st])
            ACT.op(lambda: A.activation(out=st_e[:, :, 0:4], in_=sraw[:, :, 0:4], func=AF.Exp, scale=-1.0),
                   reads=[B_sraw, B_st], writes=[B_st])
            ACT.op(lambda: A.activation(out=st_lnbn[:], in_=st_e[:, :, 0:4], func=AF.Ln, bias=1.0),
                   reads=[B_st], writes=[B_st])
            DVE.op(lambda: V.tensor_tensor(out=st_e[:, :, 4:8], in0=sraw[:, :, 4:8],
                                           in1=bc(dtb_bc.unsqueeze(1), [128, TPB, 4]), op=ALU.add),
                   reads=[B_sraw, B_st], writes=[B_st])
            ACT.op(lambda: A.activation(out=st_e[:, :, 4:8], in_=st_e[:, :, 4:8], func=AF.Exp),
                   reads=[B_st], writes=[B_st])
            ACT.op(lambda: A.activation(out=st_e[:, :, 4:8], in_=st_e[:, :, 4:8], func=AF.Ln, bias=1.0),
                   reads=[B_st], writes=[B_st])
            DVE.op(lambda: V.tensor_tensor(out=st_g[:], in0=st_e[:, :, 4:8], in1=bc(nea[:].unsqueeze(1), [128, TPB, 4]),
                                           op=ALU.mult), reads=[B_st], writes=[B_st])

            def gcm():
                for tt in range(TPB):
                    TE.matmul(pB[0][:, 96 + tt * 4:100 + tt * 4], lhsT=C("TRI"), rhs=st_g[:, tt, :], start=True, stop=True)
                    ins = TE.matmul(pB[0][:, 112 + tt * 4:116 + tt * 4], lhsT=C("ONES"), rhs=st_g[:, tt, :],
                                    start=True, stop=True)
                return ins
            yield
            PE.op(gcm, reads=[B_st], writes=[B_pB[0]])
            gc = pB[0][:, 96:96 + 4 * TPB].rearrange("p (t k) -> p t k", t=TPB)
            gl = pB[0][:, 112:112 + 4 * TPB].rearrange("p (t k) -> p t k", t=TPB)
            lnrq = st_lnr[:, :, 0:4]
            lnrk = st_lnr[:, :, 4:8]
            DVE.op(lambda: V.scalar_tensor_tensor(out=ST[:, :, 4:8], in0=lnrk, scalar=0.5, op0=ALU.mult, in1=gc,
                                                  op1=ALU.add), reads=[B_st, B_pB[0]], writes=[B_st])
            DVE.op(lambda: V.scalar_tensor_tensor(out=ST[:, :, 0:4], in0=lnrk, scalar=-0.5, op0=ALU.mult, in1=gc,
                                                  op1=ALU.add), reads=[B_st, B_pB[0]], writes=[B_st])
            DVE.op(lambda: V.scalar_tensor_tensor(out=st_c[:], in0=ST[:, :, 4:8], scalar=-1.0, op0=ALU.mult, in1=gl,
                                                  op1=ALU.add), reads=[B_st, B_pB[0]], writes=[B_st])
            DVE.op(lambda: V.tensor_tensor(out=ST[:, :, 0:4], in0=ST[:, :, 0:4], in1=st_lnbn[:], op=ALU.subtract),
                   reads=[B_st], writes=[B_st])
            DVE.op(lambda: V.scalar_tensor_tensor(out=ST[:, :, 8:12], in0=lnrq, scalar=-0.5, op0=ALU.mult, in1=gc,
                                                  op1=ALU.add), reads=[B_st, B_pB[0]], writes=[B_st])
            DVE.op(lambda: V.tensor_scalar(out=ST[:, :, 8:12], in0=ST[:, :, 8:12], scalar1=LN_QS, scalar2=None,
                                           op0=ALU.add), reads=[B_st], writes=[B_st])
            ACT.op(lambda: A.activation(out=g.ex_a[:], in_=ST[:, :, 0:4], func=AF.Exp), reads=[B_st], writes=[B_st])
            ACT.op(lambda: A.activation(out=g.ex_c[:], in_=st_c[:], func=AF.Exp), reads=[B_st], writes=[B_st])
            ACT.op(lambda: A.activation(out=g.ex_b[:], in_=st_lnbn[:], func=AF.Exp, scale=-1.0),
                   reads=[B_st], writes=[B_st])
            ACT.op(lambda: A.activation(out=g.ex_gl[:], in_=gl, func=AF.Exp), reads=[B_st, B_pB[0]], writes=[B_st])
            DVE.op(lambda: V.tensor_copy(out=STs[:, :, 0:12], in_=ST[:]), reads=[B_st], writes=[B_st])
            DVE.op(lambda: V.tensor_copy(out=STh[:], in_=STs[:, :, 0:12]), reads=[B_st], writes=[B_st])
            DVE.op(lambda: V.tensor_tensor(out=STr[:], in0=ST[:], in1=STh[:], op=ALU.subtract),
                   reads=[B_st], writes=[B_st])
            DVE.op(lambda: V.tensor_copy(out=STs[:, :, 12:24], in_=STr[:]), reads=[B_st], writes=[B_st])
            DVE.op(lambda: V.tensor_copy(out=STh[:], in_=STs[:, :, 12:24]), reads=[B_st], writes=[B_st])
            DVE.op(lambda: V.tensor_tensor(out=STr[:], in0=STr[:], in1=STh[:], op=ALU.subtract),
                   reads=[B_st], writes=[B_st])
            DVE.op(lambda: V.tensor_copy(out=STs[:, :, 24:36], in_=STr[:]), reads=[B_st], writes=[B_st])

            def trs():
                for tt in range(TPB):
                    ins = TE.transpose(out=pT[0:36, tt * 128:(tt + 1) * 128], in_=STs[:, tt, :], identity=ident_bf)
                return ins
            yield
            PE.op(trs, reads=[B_st], writes=[B_pT])
            ACT.op(lambda: A.copy(out=STT[:], in_=pT[0:36, 0:128 * TPB].rearrange("p (t k) -> p t k", t=TPB)),
                   reads=[B_pT], writes=[g.B_STT])
            if s == 0 and b == 0:
                dump("ST", ST[:, 0, :], [B_st])
                dump("exa", g.ex_a[:, 0, :], [B_st])
                dump("sg", st_g[:, 0, :], [B_st])
            for tt in range(TPB):
                yield from gdn_chunk(s, b, tt, g)

        def gdn_chunk(s, b, tt, g):
            cs = slice(tt * 128, (tt + 1) * 128)
            first = (s == 0 and b == 0 and tt == 0)
            qT, kT, vT = dT[0], dT[1], dT[2]
            STc = g.STT[:, tt, :]
            B_st, B_STT = g.B_st, g.B_STT
            Eb, Xb, Yb, Pb = g.Eb, g.Xb, g.Yb, g.Pb
            B_Eb, B_X, B_Y, B_P = g.B_Eb, g.B_X, g.B_Y, g.B_P

            def sel(which, hd):
                return C("SEL", bf=True, rows=36, sub=((which * 4 + hd) * 128, 128))

            def trkv():
                for hd in range(4):
                    TE.transpose(out=pT[:, hd * 128:(hd + 1) * 128], in_=kT[:, hd, cs], identity=ident_bf)
                for hd in range(4):
                    ins = TE.transpose(out=pT[:, 512 + hd * 128:512 + (hd + 1) * 128], in_=vT[:, hd, cs],
                                       identity=ident_bf)
                return ins
            yield
            PE.op(trkv, reads=B_dT[1] + B_dT[2], writes=[B_pT])
            DVE.op(lambda: V.tensor_tensor(out=g.kbg[:], in0=pTh[:, 0:4, :],
                                           in1=bc(g.ex_a[:, tt, :].unsqueeze(2), [128, 4, 128]), op=ALU.mult),
                   reads=[B_pT, B_st], writes=[g.B_kbg])
            DVE.op(lambda: V.tensor_tensor(out=g.kdec[:], in0=pTh[:, 0:4, :],
                                           in1=bc(g.ex_c[:, tt, :].unsqueeze(2), [128, 4, 128]), op=ALU.mult),
                   reads=[B_pT, B_st], writes=[g.B_kdec])
            DVE.op(lambda: V.tensor_tensor(out=g.vb[:], in0=pTh[:, 4:8, :],
                                           in1=bc(g.ex_b[:, tt, :].unsqueeze(2), [128, 4, 128]), op=ALU.mult),
                   reads=[B_pT, B_st], writes=[g.B_vb])

            def kkqk():
                for hd in range(4):
                    TE.matmul(pB[0][:, hd * 128:(hd + 1) * 128], lhsT=kT[:, hd, cs], rhs=kT[:, hd, cs],
                              start=True, stop=True)
                for hd in range(4):
                    ins = TE.matmul(pB[1][:, hd * 128:(hd + 1) * 128], lhsT=kT[:, hd, cs], rhs=qT[:, hd, cs],
                                    start=True, stop=True)
                return ins
            yield
            PE.op(kkqk, reads=B_dT[0] + B_dT[1], writes=[B_pB[0], B_pB[1]])

            def dmat(bank, which, mask, lower):
                def f():
                    for hd in range(4):
                        o = pB[bank][:, hd * 128:(hd + 1) * 128]
                        if lower:
                            TE.matmul(o, lhsT=STc, rhs=sel(which, hd), start=True, stop=False)
                            TE.matmul(o, lhsT=sel(SEL_NB, hd), rhs=STc, start=False, stop=False)
                        else:
                            TE.matmul(o, lhsT=sel(which, hd), rhs=STc, start=True, stop=False)
                            TE.matmul(o, lhsT=STc, rhs=sel(SEL_NB, hd), start=False, stop=False)
                        ins = TE.matmul(o, lhsT=ident_bf, rhs=C(mask, bf=True), start=False, stop=True)
                    return ins
                PE.op(f, reads=[B_STT], writes=[B_pB[bank]])

            yield
            dmat(2, SEL_A, "MASK_LS", True)
            ACT.op(lambda: A.activation(out=Eb[0][:], in_=pBh[2], func=AF.Exp), reads=[B_pB[2]], writes=[B_Eb[0]])
            DVE.op(lambda: V.scalar_tensor_tensor(out=Xb[0][:], in0=pBh[0], scalar=-1.0, op0=ALU.mult, in1=Eb[0][:],
                                                  op1=ALU.mult), reads=[B_pB[0], B_Eb[0]], writes=[B_X[0]])
            yield
            dmat(2, SEL_A, "MASK_US", False)
            ACT.op(lambda: A.activation(out=Eb[1][:], in_=pBh[2], func=AF.Exp), reads=[B_pB[2]], writes=[B_Eb[1]])
            DVE.op(lambda: V.scalar_tensor_tensor(out=Yb[0][:], in0=pBh[0], scalar=-1.0, op0=ALU.mult, in1=Eb[1][:],
                                                  op1=ALU.mult), reads=[B_pB[0], B_Eb[1]], writes=[B_Y[0]])
            yield
            dmat(2, SEL_AP, "MASK_UI", False)
            ACT.op(lambda: A.activation(out=Eb[0][:], in_=pBh[2], func=AF.Exp), reads=[B_pB[2]], writes=[B_Eb[0]])
            DVE.op(lambda: V.tensor_tensor(out=g.intraT[:], in0=pBh[1], in1=Eb[0][:], op=ALU.mult),
                   reads=[B_pB[1], B_Eb[0]], writes=[g.B_iT])

            def fb():
                for hd in range(4):
                    ins = TE.matmul(pB[2][:, hd * 128:(hd + 1) * 128], lhsT=sel(SEL_AP, hd), rhs=STc,
                                    start=True, stop=True)
                return ins
            yield
            PE.op(fb, reads=[B_STT], writes=[B_pB[2]])
            ACT.op(lambda: A.activation(out=Eb[1][:], in_=pBh[2], func=AF.Exp), reads=[B_pB[2]], writes=[B_Eb[1]])
            POOL.op(lambda: G.tensor_tensor(out=g.qdT[:], in0=qT[:, :, cs], in1=Eb[1][:], op=ALU.mult),
                    reads=B_dT[0] + [B_Eb[1]], writes=[g.B_qdT])
            if first:
                dump("X0", Xb[0][:, 0, :], [B_X[0]])
                dump("Y0", Yb[0][:, 0, :], [B_Y[0]])
                dump("intraT", g.intraT[:, 0, :], [g.B_iT])
                dump("qdT", g.qdT[:, 0, :], [g.B_qdT])
                dump("kbg", g.kbg[:, 0, :], [g.B_kbg])

            POOL.op(lambda: G.tensor_tensor(out=Pb[0][:], in0=Yb[0][:],
                                            in1=bc(ident_bf.unsqueeze(1), [128, 4, 128]), op=ALU.add),
                    reads=[B_Y[0]], writes=[B_P[0]])
            cur = 0
            pc = 0
            for lvl in range(1, 7):
                nxt = 1 - cur

                def sqx(cur=cur):
                    for hd in range(4):
                        ins = TE.matmul(pB[0][:, hd * 128:(hd + 1) * 128], lhsT=Yb[cur][:, hd, :], rhs=Xb[cur][:, hd, :],
                                        start=True, stop=True)
                    return ins
                yield
                PE.op(sqx, reads=[B_X[cur], B_Y[cur]], writes=[B_pB[0]])
                if lvl < 6:
                    def sqy(cur=cur):
                        for hd in range(4):
                            ins = TE.matmul(pB[1][:, hd * 128:(hd + 1) * 128], lhsT=Xb[cur][:, hd, :],
                                            rhs=Yb[cur][:, hd, :], start=True, stop=True)
                        return ins
                    PE.op(sqy, reads=[B_X[cur], B_Y[cur]], writes=[B_pB[1]])
                ACT.op(lambda nxt=nxt: A.copy(out=Xb[nxt][:], in_=pBh[0]), reads=[B_pB[0]], writes=[B_X[nxt]])
                if lvl < 6:
                    DVE.op(lambda nxt=nxt: V.tensor_copy(out=Yb[nxt][:], in_=pBh[1]), reads=[B_pB[1]],
                           writes=[B_Y[nxt]])
                pn = 1 - pc

                def pm(nxt=nxt, pc=pc):
                    for hd in range(4):
                        ins = TE.matmul(pB[2][:, hd * 128:(hd + 1) * 128], lhsT=Xb[nxt][:, hd, :], rhs=Pb[pc][:, hd, :],
                                        start=True, stop=True)
                    return ins
                yield
                PE.op(pm, reads=[B_X[nxt], B_P[pc]], writes=[B_pB[2]])
                DVE.op(lambda pn=pn, pc=pc: V.tensor_tensor(out=Pb[pn][:], in0=pBh[2], in1=Pb[pc][:], op=ALU.add),
                       reads=[B_pB[2], B_P[pc]], writes=[B_P[pn]])
                cur = nxt
                pc = pn
            TT = Pb[pc]
            B_TT = B_P[pc]
            if first:
                dump("TT", TT[:, 0, :], [B_TT])

            def uw():
                for hd in range(4):
                    TE.matmul(pB[0][:, hd * 128:(hd + 1) * 128], lhsT=TT[:, hd, :], rhs=g.vb[:, hd, :],
                              start=True, stop=True)
                for hd in range(4):
                    ins = TE.matmul(pB[1][:, hd * 128:(hd + 1) * 128], lhsT=g.kbg[:, hd, :], rhs=TT[:, hd, :],
                                    start=True, stop=True)
                return ins
            yield
            PE.op(uw, reads=[B_TT, g.B_vb, g.B_kbg], writes=[B_pB[0], B_pB[1]])
            ACT.op(lambda: A.copy(out=g.u_sb[:], in_=pBh[0]), reads=[B_pB[0]], writes=[g.B_u])
            DVE.op(lambda: V.tensor_copy(out=g.wT[:], in_=pBh[1]), reads=[B_pB[1]], writes=[g.B_wT])

            def ws():
                for hd in range(4):
                    ins = TE.matmul(pB[2][:, hd * 128:(hd + 1) * 128], lhsT=g.wT[:, hd, :], rhs=Sbf[:, hd, :],
                                    start=True, stop=True)
                return ins
            yield
            PE.op(ws, reads=[g.B_wT, B_Sbf], writes=[B_pB[2]])
            DVE.op(lambda: V.scalar_tensor_tensor(out=g.vnew[:], in0=pBh[2], scalar=-1.0, op0=ALU.mult, in1=g.u_sb[:],
                                                  op1=ALU.add), reads=[B_pB[2], g.B_u], writes=[g.B_vn])

            def oo():
                for hd in range(4):
                    TE.matmul(pB[2][:, hd * 128:(hd + 1) * 128], lhsT=g.qdT[:, hd, :], rhs=Sbf[:, hd, :],
                              start=True, stop=False)
                    TE.matmul(pB[2][:, hd * 128:(hd + 1) * 128], lhsT=g.intraT[:, hd, :], rhs=g.vnew[:, hd, :],
                              start=False, stop=True)
                for hd in range(4):
                    ins = TE.matmul(pB[0][:, hd * 128:(hd + 1) * 128], lhsT=g.kdec[:, hd, :], rhs=g.vnew[:, hd, :],
                                    start=True, stop=True)
                return ins
            yield
            PE.op(oo, reads=[g.B_qdT, B_Sbf, g.B_iT, g.B_vn, g.B_kdec], writes=[B_pB[2], B_pB[0]])
            DVE.op(lambda: V.tensor_tensor(out=Sst[:], in0=Sst[:],
                                           in1=bc(g.ex_gl[:, tt, :].unsqueeze(2), [128, 4, 128]), op=ALU.mult),
                   reads=[B_st], writes=[B_S])
            DVE.op(lambda: V.tensor_tensor(out=Sst[:], in0=pBh[0], in1=Sst[:], op=ALU.add),
                   reads=[B_pB[0]], writes=[B_S])
            POOL.op(lambda: G.tensor_copy(out=Sbf[:], in_=Sst[:]), reads=[B_S], writes=[B_Sbf])
            ACT.op(lambda: A.activation(out=g.osq[:], in_=pBh[2], func=AF.Square), reads=[B_pB[2]], writes=[g.B_osq])
            DVE.op(lambda: V.tensor_reduce(out=g.ost[:, 0:4], in_=g.osq[:], op=ALU.add, axis=AX),
                   reads=[g.B_osq], writes=[g.B_ost])
            ACT.op(lambda: A.activation(out=g.ost[:, 4:8], in_=g.ost[:, 0:4], func=AF.Ln, scale=1.0 / 128, bias=EPS),
                   reads=[g.B_ost], writes=[g.B_ost])
            ACT.op(lambda: A.activation(out=g.ost[:, 8:12], in_=g.ost[:, 4:8], func=AF.Exp, scale=-0.5),
                   reads=[g.B_ost], writes=[g.B_ost])
            DVE.op(lambda: V.tensor_tensor(out=g.osq[:], in0=pBh[2],
                                           in1=bc(g.ost[:, 8:12].unsqueeze(2), [128, 4, 128]), op=ALU.mult),
                   reads=[B_pB[2], g.B_ost], writes=[g.B_osq])
            POOL.op(lambda: G.tensor_tensor(out=mixed[:, tt, 512:1024], in0=g.osq[:].rearrange("p h d -> p (h d)"),
                                            in1=zw[:, tt, :], op=ALU.mult),
                    reads=[g.B_osq, B_zw[tt]], writes=[B_mx[tt][1]])
            if first:
                dump("u", g.u_sb[:, 0, :], [g.B_u])
                dump("gdn_out", mixed[:, 0, 512:1024], [B_mx[0][1]])

        def attn_block(s, b):
            t0 = b * BT
            ob = b
            use_sel = ob >= 4
            DVE.op(lambda: V.tensor_reduce(out=kbar[:, :, b:b + 1], in_=akT[:, :, t0:t0 + BT].unsqueeze(2),
                                           op=ALU.add, axis=AX),
                   reads=[B_ak[p][b] for p in range(4)], writes=[B_kbar])
            DVE.op(lambda: V.tensor_scalar(out=kbar[:, :, b:b + 1], in0=kbar[:, :, b:b + 1],
                                           scalar1=1.0 / 256, scalar2=None, op0=ALU.mult),
                   reads=[B_kbar], writes=[B_kbar])
            DVE.op(lambda: V.tensor_copy(out=kbar_hi[:], in_=kbar[:]), reads=[B_kbar], writes=[B_kbar])
            DVE.op(lambda: V.tensor_copy(out=kbar_t[:], in_=kbar_hi[:]), reads=[B_kbar], writes=[B_kbar])
            DVE.op(lambda: V.tensor_tensor(out=kbar_t[:], in0=kbar[:], in1=kbar_t[:], op=ALU.subtract),
                   reads=[B_kbar], writes=[B_kbar])
            DVE.op(lambda: V.tensor_copy(out=kbar_lo[:], in_=kbar_t[:]), reads=[B_kbar], writes=[B_kbar])
            if use_sel:
                for tt in range(TPB):
                    yield from attn_select(s, b, tt)
            nkt = 2 * b + 2
            for half in range(2):
                tiles = [(h4, kt) for h4 in range(4) for kt in range(nkt)]

                def emit_qk(h4, kt):
                    hd = half * 4 + h4
                    p, r0 = hd // 2, 64 * (hd % 2)
                    kb = kt // 2
                    sl = state["pt"] % NPT
                    state["pt"] += 1
                    sb_i = sl % 2
                    B_s = B_pA[sb_i]
                    lastk = (kt == nkt - 1)
                    q0 = 128 if lastk else 0
                    sreg = pA[sb_i][:, q0:256]
                    selm = use_sel and kb < ob

                    def qk():
                        ins = TE.matmul(sreg, lhsT=akT[r0:r0 + 64, p, kt * 128:(kt + 1) * 128],
                                        rhs=aqT[r0:r0 + 64, p, q0:256], start=True, stop=not (selm or kb == ob))
                        if kb == ob:
                            ins = TE.matmul(pA[sb_i][:, q0:q0 + 128], lhsT=ident_bf, rhs=C("CAUS", bf=True),
                                            start=False, stop=True)
                        elif selm:
                            ins = TE.matmul(sreg, lhsT=cbf[r0:r0 + 8, IND_O + kb * 128:IND_O + (kb + 1) * 128],
                                            rhs=selT[r0:r0 + 8, hd, :], start=False, stop=True)
                        return ins
                    PE.op(qk, reads=[B_ak[p][kb], B_aq[p]] + ([B_selT] if selm else []), writes=[B_s])
                    dref = (2 * b + 1) - kt
                    if hd == 0 and not lastk:
                        ACT.op(lambda: A.activation(out=PTs[:, sl, 0:128], in_=pA[sb_i][:, 0:128], func=AF.Exp, scale=0.125,
                                                    bias=C("ALIBI", sub=(hd * 16 + dref - 1, 1))),
                               reads=[B_s], writes=[B_PT[sl]])
                        ACT.op(lambda: A.activation(out=PTs[:, sl, 128:256], in_=pA[sb_i][:, 128:256], func=AF.Exp,
                                                    scale=0.125, bias=C("ALIBI", sub=(hd * 16 + dref, 1))),
                               reads=[B_s], writes=[B_PT[sl]])
                    else:
                        ACT.op(lambda: A.activation(out=PTs[:, sl, q0:256], in_=sreg, func=AF.Exp, scale=0.125,
                                                    bias=C("ALIBI", sub=(hd * 16 + dref, 1))),
                               reads=[B_s], writes=[B_PT[sl]])
                    return sl

                def emit_pv(h4, kt, sl):
                    hd = half * 4 + h4

                    def pv():
                        ins = None
                        for tq in range(TPB):
                            gt = 2 * b + tq
                            if kt > gt:
                                continue
                            ins = TE.matmul(pO[tq][:, h4 * 128:h4 * 128 + 65], lhsT=PTs[:, sl, tq * 128:(tq + 1) * 128],
                                            rhs=vtm[:, kt, hd, :], start=(kt == 0), stop=(kt == gt))
                        return ins
                    PE.op(pv, reads=[B_PT[sl], B_v[kt]], writes=B_pO)

                pend = []
                for (h4, kt) in tiles:
                    yield
                    sl = emit_qk(h4, kt)
                    pend.append((h4, kt, sl))
                    if len(pend) > 1:
                        emit_pv(*pend.pop(0))
                while pend:
                    emit_pv(*pend.pop(0))
                for tq in range(TPB):
                    DVE.op(lambda tq=tq: V.reciprocal(out=rden[:], in_=pOh[tq][:, :, 64]),
                           reads=[B_pO[tq]], writes=[B_att])
                    DVE.op(lambda tq=tq: V.tensor_tensor(out=att_t[:], in0=pOh[tq][:, :, 0:64],
                                                         in1=bc(rden[:].unsqueeze(2), [128, 4, 64]),
                                                         op=ALU.mult),
                           reads=[B_pO[tq], B_att], writes=[B_att])
                    POOL.op(lambda half=half, tq=tq: G.tensor_tensor(
                        out=mixed[:, tq, half * 256:(half + 1) * 256], in0=att_t[:].rearrange("p h d -> p (h d)"),
                        in1=siluz[:, tq, half * 256:(half + 1) * 256], op=ALU.mult),
                        reads=[B_att, B_sz[tq]], writes=[B_mx[tq][0]])
            if s == 0 and b in (0, 4):
                dump(f"attn{2 * b}", mixed[:, 0, 0:512], [B_mx[0][0]])

        def attn_select(s, b, tt):
            ob = b
            qs = slice(tt * 128, (tt + 1) * 128)

            def gate():
                for hd in range(8):
                    p, r0 = hd // 2, 64 * (hd % 2)
                    o = pA[hd % 2][:, 256 + p * 8:264 + p * 8]
                    TE.matmul(o, lhsT=aqT[r0:r0 + 64, p, qs], rhs=kbar_hi[r0:r0 + 64, p, :], start=True, stop=False)
                    ins = TE.matmul(o, lhsT=aqT[r0:r0 + 64, p, qs], rhs=kbar_lo[r0:r0 + 64, p, :], start=False,
                                    stop=True)
                return ins
            PE.op(gate, reads=B_aq + [B_kbar], writes=B_pA)
            obm = C("OBM", sub=((ob - 4) * 64, 64)).rearrange("p (a two j) -> p a two j", two=2, j=8)
            gmv = gm[:].rearrange("p (a two) j -> p a two j", two=2)
            DVE.op(lambda: V.tensor_tensor(out=gmv[:, :, 0, :],
                                           in0=pA[0][:, 256:288].rearrange("p (a j) -> p a j", a=4),
                                           in1=obm[:, :, 0, :], op=ALU.add), reads=[B_pA[0]], writes=[B_gm])
            DVE.op(lambda: V.tensor_tensor(out=gmv[:, :, 1, :],
                                           in0=pA[1][:, 256:288].rearrange("p (a j) -> p a j", a=4),
                                           in1=obm[:, :, 1, :], op=ALU.add), reads=[B_pA[1]], writes=[B_gm])
            for hd in range(8):
                DVE.op(lambda hd=hd: V.max(out=top8[:, hd, :], in_=gm[:, hd, :]), reads=[B_gm], writes=[B_gm])
            DVE.op(lambda: V.tensor_tensor(out=gm[:], in0=gm[:], in1=bc(top8[:, :, 2:3], [128, 8, 8]),
                                           op=ALU.is_ge), reads=[B_gm], writes=[B_gm])
            DVE.op(lambda: V.tensor_scalar(out=selb[:, :, 0:8], in0=gm[:], scalar1=-NEGA, scalar2=NEGA, op0=ALU.mult,
                                           op1=ALU.add), reads=[B_gm], writes=[B_gm])
            DVE.op(lambda: V.tensor_copy(out=selb[:, :, 64:72], in_=selb[:, :, 0:8]), reads=[B_gm], writes=[B_gm])

            def trsel():
                for hd in range(8):
                    ins = TE.transpose(out=pT[0:72, hd * 128:(hd + 1) * 128], in_=selb[:, hd, :], identity=ident_bf)
                return ins
            yield
            PE.op(trsel, reads=[B_gm], writes=[B_pT])
            DVE.op(lambda: V.tensor_copy(out=selT[:, :, qs], in_=pT[0:72, :].rearrange("p (h q) -> p h q", h=8)),
                   reads=[B_pT], writes=[B_selT])
            if s == 0 and b == 4 and tt == 0:
                dump("selb", gm[:].rearrange("p h j -> p (h j)"), [B_gm])

        def out_block(s, b):
            for tt in range(TPB):
                gt = b * TPB + tt
                sl = state["xo"] % 2
                state["xo"] += 1
                dma(SP, ch_xo[sl], xo[sl][:], x[s, gt * 128:(gt + 1) * 128, :], writes=[B_xo[sl]])

                def tr(tt=tt):
                    for kc in range(8):
                        ins = TE.transpose(out=pT[:, kc * 128:(kc + 1) * 128], in_=mixed[:, tt, kc * 128:(kc + 1) * 128],
                                           identity=ident_bf)
                    return ins
                PE.op(tr, reads=B_mx[tt], writes=[B_pT])
                ACT.op(lambda: A.copy(out=mixT[:, 0:4, :], in_=pTh[:, 0:4, :]), reads=[B_pT], writes=[B_mixT])
                DVE.op(lambda: V.tensor_copy(out=mixT[:, 4:8, :], in_=pTh[:, 4:8, :]), reads=[B_pT, B_mixT],
                       writes=[B_mixT])

                def op_():
                    for hf in range(2):
                        for kc in range(8):
                            ins = TE.matmul(pA[hf][:, :], lhsT=mixT[:, kc, :], rhs=w_out_bf[:, kc, hf * 512:(hf + 1) * 512],
                                            start=(kc == 0), stop=(kc == 7))
                    return ins
                PE.op(op_, reads=[B_mixT], writes=B_pA)
                for hf in range(2):
                    ACT.op(lambda hf=hf: A.activation(out=junk[:, 0:512], in_=pA[hf][:, :], func=AF.Square,
                                                      accum_out=small[:, 8 + hf:9 + hf]),
                           reads=[B_pA[hf]], writes=[B_junk, B_small])
                DVE.op(lambda: V.tensor_tensor(out=small[:, 10:11], in0=small[:, 8:9], in1=small[:, 9:10], op=ALU.add),
                       reads=[B_small], writes=[B_small])
                ACT.op(lambda: A.activation(out=small[:, 11:12], in_=small[:, 10:11], func=AF.Ln, scale=1.0 / D,
                                            bias=EPS), reads=[B_small], writes=[B_small])
                ACT.op(lambda: A.activation(out=small[:, 12:13], in_=small[:, 11:12], func=AF.Exp, scale=-0.5),
                       reads=[B_small], writes=[B_small])
                for hf in range(2):
                    DVE.op(lambda hf=hf: V.scalar_tensor_tensor(
                        out=tmpf[hf][:], in0=pA[hf][:, :], scalar=small[:, 12:13], op0=ALU.mult,
                        in1=wpost_bc[:, hf * 512:(hf + 1) * 512], op1=ALU.mult),
                        reads=[B_pA[hf], B_small], writes=[B_tmpf[hf]])
                    POOL.op(lambda hf=hf, sl=sl: G.tensor_tensor(out=xo[sl][:, hf * 512:(hf + 1) * 512],
                                                                 in0=xo[sl][:, hf * 512:(hf + 1) * 512],
                                                                 in1=tmpf[hf][:], op=ALU.add),
                            reads=[B_tmpf[hf], B_xo[sl]], writes=[B_xo[sl]])
                dma(SP, ch_st[sl], y[s, gt * 128:(gt + 1) * 128, :], xo[sl][:], reads=[B_xo[sl]])

        for s in range(nseq):
            for b in range(nblk):
                if os.environ.get("K_STOP") == "w":
                    break
                state["uid"] += 1
                with contextlib.ExitStack() as ea:
                    inproj_block(s, b, ea)
                    barrier()
                with contextlib.ExitStack() as eb:
                    gens = []
                    if "gdn" in phases:
                        gens.append(gdn_block(s, b, eb))
                    if "attn" in phases:
                        gens.append(attn_block(s, b))
                    while gens:
                        for gq in list(gens):
                            try:
                                next(gq)
                            except StopIteration:
                                gens.remove(gq)
                    if "gdn" in phases:
                        barrier()
                if "out" in phases:
                    out_block(s, b)
        for ch in ch_st + dbg_chans:
            if ch.count:
                nc.sync.wait_ge(ch.sem, ch.count)
    return nc


def make_params(norm_pre_w, conv_w, a_log, dt_bias, gdn_norm_w, norm_post_w):
    params = np.zeros((128, PW), np.float32)
    params[:, 0:8] = np.asarray(norm_pre_w)[0].reshape(8, 128).T
    cw = np.asarray(conv_w)[0]
    params[:, 8:56] = cw.reshape(4, 12, 128).transpose(2, 0, 1).reshape(128, 48)
    params[:, 56:60] = np.asarray(a_log)[0][None, :]
    params[:, 60:64] = np.asarray(dt_bias)[0][None, :]
    params[:, 64:576] = np.tile(np.asarray(gdn_norm_w)[0], 4)[None, :]
    params[:, 576:1600] = np.asarray(norm_post_w)[0][None, :]
    return params


def kernel(x, norm_pre_w, w_in, conv_w, a_log, dt_bias, gdn_norm_w, w_out, norm_post_w):
    x = np.ascontiguousarray(np.asarray(x, dtype=np.float32))
    consts = make_consts()
    params = make_params(norm_pre_w, conv_w, a_log, dt_bias, gdn_norm_w, norm_post_w)
    w_in0 = np.ascontiguousarray(np.asarray(w_in, dtype=np.float32)[0])
    w_out0 = np.ascontiguousarray(np.asarray(w_out, dtype=np.float32)[0])
    nc = build()
    in_maps = []
    for c in range(NCORES):
        in_maps.append({"x": x[c * NSEQ:(c + 1) * NSEQ], "w_in": w_in0, "w_out": w_out0, "consts": consts,
                        "params": params})
    res = run_bass_kernel_spmd(nc, in_maps, core_ids=list(range(NCORES)))
    return np.concatenate([r["y"] for r in res.results], axis=0)
```

```python
import contextlib
import math
import os

import numpy as np

import concourse.bass as bass
import concourse.mybir as mybir
from concourse.bass_utils import run_bass_kernel_spmd

F32 = mybir.dt.float32
BF16 = mybir.dt.bfloat16
AF = mybir.ActivationFunctionType
ALU = mybir.AluOpType

T = 2048
D = 1024
NCOL = 4104
NSEQ = 4
NCORES = 8
EPS = 1e-6
NEGA = -240000.0
NEGD = -30000.0


class Tok:
    __slots__ = ("sem", "val", "eng")

    def __init__(self, sem, val, eng):
        self.sem, self.val, self.eng = sem, val, eng


class Buf:
    __slots__ = ("name", "w", "r", "excl")

    def __init__(self, name, excl=False):
        self.name = name
        self.w = []
        self.r = []
        self.excl = excl


class Eng:
    def __init__(self, e, sem, name, is_pe=False):
        self.e, self.sem, self.name, self.is_pe = e, sem, name, is_pe
        self.count = 0
        self.waited = {}

    def _wait(self, tok):
        if tok.eng is self and self.is_pe:
            return
        k = id(tok.sem)
        if self.waited.get(k, 0) >= tok.val:
            return
        self.e.wait_ge(tok.sem, tok.val)
        self.waited[k] = tok.val

    def deps(self, reads, writes):
        for b in reads:
            for t in b.w:
                self._wait(t)
            if b.excl:
                for t in b.r:
                    self._wait(t)
        for b in writes:
            for t in b.w:
                self._wait(t)
            for t in b.r:
                self._wait(t)

    def commit(self, tok, reads, writes):
        for b in writes:
            b.w = [tok]
            b.r = []
        for b in reads:
            if b in writes:
                continue
            if b.excl:
                b.w = [tok]
                b.r = []
                continue
            b.r = [t for t in b.r if t.sem is not tok.sem] + [tok]

    def op(self, fn, reads=(), writes=()):
        self.deps(reads, writes)
        ins = fn()
        self.count += 1
        ins.then_inc(self.sem, 1)
        tok = Tok(self.sem, self.count, self)
        self.commit(tok, reads, writes)
        return tok


class Chan:
    def __init__(self, sem):
        self.sem = sem
        self.count = 0


def dma(q, chan, out, in_, reads=(), writes=()):
    q.deps(reads, writes)
    chan.count += 16
    q.e.dma_start(out=out, in_=in_).then_inc(chan.sem, 16)
    tok = Tok(chan.sem, chan.count, None)
    q.commit(tok, reads, writes)
    return tok


class _Cols:
    def __init__(self):
        self.n = 0
        self.d = {}

    def add(self, name, w):
        self.d[name] = (self.n, w)
        self.n += w
        return self.d[name]


def _const_layout():
    c = _Cols()
    c.add("IDENT", 128)
    c.add("TRI", 128)
    c.add("ONES", 128)
    c.add("ALIBI", 8 * 16)
    c.add("OBM", 4 * 64)
    c.add("MASK_LS", 128)
    c.add("MASK_US", 128)
    c.add("MASK_UI", 128)
    c.add("CAUS", 128)
    c.add("SEL", 12 * 128)
    c.add("IND", 8 * 128)
    return c


CL = _const_layout()
SEL_A, SEL_AP, SEL_NB = 0, 1, 2


def make_consts():
    c = np.zeros((128, CL.n), np.float32)
    p = np.arange(128)[:, None]
    f = np.arange(128)[None, :]

    def put(name, arr):
        o, w = CL.d[name]
        c[:, o:o + w] = arr

    put("IDENT", (p == f).astype(np.float32))
    put("TRI", (p <= f).astype(np.float32))
    put("ONES", np.ones((128, 128), np.float32))
    put("MASK_LS", np.where(p > f, 0.0, NEGD))
    put("MASK_US", np.where(f > p, 0.0, NEGD))
    put("MASK_UI", np.where(f >= p, 0.0, NEGD))
    put("CAUS", np.where(f >= p, 0.0, NEGA))
    slopes = (2.0 ** (-8.0 / 8)) ** np.arange(1, 9)
    al = np.zeros((128, 8, 16), np.float32)
    for h in range(8):
        for d in range(16):
            al[:, h, d] = -slopes[h] * (128 * d + 127 - np.arange(128))
    put("ALIBI", al.reshape(128, 128))
    sel = np.zeros((128, 12, 128), np.float32)
    for h in range(4):
        for sp in range(3):
            sel[sp * 12 + 0 * 4 + h, SEL_A * 4 + h, :] = 1.0
            sel[sp * 12 + 2 * 4 + h, SEL_AP * 4 + h, :] = 1.0
            sel[sp * 12 + 1 * 4 + h, SEL_NB * 4 + h, :] = -1.0
    put("SEL", sel.reshape(128, 12 * 128))
    ind = np.zeros((128, 8, 128), np.float32)
    for kb in range(8):
        ind[kb, kb, :] = 1.0
        ind[64 + kb, kb, :] = 1.0
    put("IND", ind.reshape(128, 8 * 128))
    obm = np.zeros((128, 4, 8, 8), np.float32)
    for ob in range(4, 8):
        obm[:, ob - 4, :, ob:] = -1e30
    put("OBM", obm.reshape(128, 256))
    return c


PW = 8 + 48 + 4 + 4 + 512 + 1024
LN_QS = math.log(128.0 ** -0.5)
BT = 256
TPB = 2
NB = T // BT
CF32 = 768


class NS:
    pass


def build(nseq=NSEQ, nblk=NB, dbg=None, phases=("gdn", "attn", "out")):
    nc = bass.Bass("TRN2", target_bir_lowering=False)
    dbg = dbg or {}
    x = nc.dram_tensor("x", [nseq, T, D], F32, kind="ExternalInput").ap()
    w_in = nc.dram_tensor("w_in", [D, NCOL], F32, kind="ExternalInput").ap()
    w_out = nc.dram_tensor("w_out", [D, D], F32, kind="ExternalInput").ap()
    consts = nc.dram_tensor("consts", [128, CL.n], F32, kind="ExternalInput").ap()
    params = nc.dram_tensor("params", [128, PW], F32, kind="ExternalInput").ap()
    y = nc.dram_tensor("y", [nseq, T, D], F32, kind="ExternalOutput").ap()
    dbg_aps = {}
    for name, shape in dbg.items():
        dbg_aps[name] = nc.dram_tensor("dbg_" + name, list(shape), F32, kind="ExternalOutput").ap()
    AX = mybir.AxisListType.X

    with contextlib.ExitStack() as es:
        def sb(name, shape, dt=F32, st=None):
            return (st or es).enter_context(nc.sbuf_tensor(name, list(shape), dt))

        def ps(name, shape, dt=F32):
            return es.enter_context(nc.psum_tensor(name, list(shape), dt))

        def sem(name):
            return es.enter_context(nc.semaphore(name))

        PE = Eng(nc.tensor, sem("s_pe"), "pe", is_pe=True)
        ACT = Eng(nc.scalar, sem("s_act"), "act")
        DVE = Eng(nc.vector, sem("s_dve"), "dve")
        POOL = Eng(nc.gpsimd, sem("s_pool"), "pool")
        SP = Eng(nc.sync, sem("s_sp"), "sp")
        V, G, A, TE = nc.vector, nc.gpsimd, nc.scalar, nc.tensor

        def chan(name):
            return Chan(sem(name))

        pA = [ps(f"pA{i}", [128, 512]) for i in range(2)]
        B_pA = [Buf("pA0", True), Buf("pA1", True)]
        pB = [ps(f"pB{i}", [128, 512]) for i in range(4)]
        B_pB = [Buf(f"pB{i}", True) for i in range(4)]
        pT = ps("pT", [128, 1024], BF16)
        B_pT = Buf("pT", True)
        pS = ps("pS", [128, 512])
        B_pS = Buf("pS", True)
        scr = sb("scr", [128, 8])
        scrb = sb("scrb", [128, 8], BF16)
        dbg_chans = []

        B_scr = Buf("scr")

        def barrier():
            b = B_scr
            PE.op(lambda: TE.matmul(pS[0:8, 510:512], lhsT=scrb[0:8, 0:8], rhs=scrb[0:8, 0:2], start=True, stop=True),
                  reads=[b], writes=[B_pS])
            ACT.op(lambda: A.copy(out=scr[0:1, 0:1], in_=scr[0:1, 0:1]), reads=[b, B_pS], writes=[b])
            DVE.op(lambda: V.tensor_copy(out=scr[0:1, 1:2], in_=scr[0:1, 1:2]), reads=[b], writes=[b])
            POOL.op(lambda: G.tensor_copy(out=scr[0:1, 2:3], in_=scr[0:1, 2:3]), reads=[b], writes=[b])
            for e in (PE, ACT, DVE, SP):
                e.deps([b], [])
            for ch in dbg_chans:
                nc.sync.wait_ge(ch.sem, ch.count)

        POOL.op(lambda: G.memset(scrb[:], 0.0), writes=[B_scr])
        POOL.op(lambda: G.memset(scr[:], 0.0), writes=[B_scr])

        cf = sb("cf", [128, CF32])
        cbf = sb("cbf", [128, CL.n], BF16)
        prm = sb("prm", [128, PW])
        w_in_bf = sb("w_in_bf", [128, 8, NCOL], BF16)
        w_out_bf = sb("w_out_bf", [128, 8, D], BF16)
        HALF = NCOL // 2
        B_c = Buf("consts")
        ch_c = chan("c_c")
        dma(SP, ch_c, cf[:], consts[:, 0:CF32], writes=[B_c])
        dma(SP, ch_c, prm[:], params[:, :], writes=[B_c])
        B_c.w = [B_c.w[-1]]
        npw = prm[:, 0:8]
        convw = prm[:, 8:56]
        alog_bc = prm[:, 56:60]
        dtb_bc = prm[:, 60:64]
        gnw_bc = prm[:, 64:576]
        wpost_bc = prm[:, 576:1600]
        with contextlib.ExitStack() as es2:
            stg = [sb(f"stg{i}", [128, HALF], st=es2) for i in range(2)]
            B_stg = [Buf("stg0"), Buf("stg1")]
            ch_stg = [chan("c_stg0"), chan("c_stg1")]
            i = 0
            hh = CL.n // 2
            for hf in range(2):
                sl = i % 2
                dma(SP, ch_stg[sl], stg[sl][:, 0:hh], consts[:, hf * hh:(hf + 1) * hh], writes=[B_stg[sl]])
                DVE.op(lambda sl=sl, hf=hf: V.tensor_copy(out=cbf[:, hf * hh:(hf + 1) * hh], in_=stg[sl][:, 0:hh]),
                       reads=[B_stg[sl]], writes=[Buf("t")])
                i += 1
            for kc in range(8):
                for hf in range(2):
                    sl = i % 2
                    dma(SP, ch_stg[sl], stg[sl][:], w_in[kc * 128:(kc + 1) * 128, hf * HALF:(hf + 1) * HALF],
                        writes=[B_stg[sl]])
                    if sl == 0:
                        ACT.op(lambda sl=sl, kc=kc, hf=hf: A.activation(
                            out=w_in_bf[:, kc, hf * HALF:(hf + 1) * HALF], in_=stg[sl][:], func=AF.Copy,
                            scale=npw[:, kc:kc + 1]), reads=[B_stg[sl], B_c], writes=[Buf("t")])
                    else:
                        DVE.op(lambda sl=sl, kc=kc, hf=hf: V.tensor_scalar(
                            out=w_in_bf[:, kc, hf * HALF:(hf + 1) * HALF], in0=stg[sl][:],
                            scalar1=npw[:, kc:kc + 1], scalar2=None, op0=ALU.mult),
                            reads=[B_stg[sl], B_c], writes=[Buf("t")])
                    i += 1
            for kc in range(8):
                sl = i % 2
                dma(SP, ch_stg[sl], stg[sl][:, 0:D], w_out[kc * 128:(kc + 1) * 128, :], writes=[B_stg[sl]])
                if sl == 0:
                    ACT.op(lambda sl=sl, kc=kc: A.copy(out=w_out_bf[:, kc, :], in_=stg[sl][:, 0:D]),
                           reads=[B_stg[sl]], writes=[Buf("t")])
                else:
                    DVE.op(lambda sl=sl, kc=kc: V.tensor_copy(out=w_out_bf[:, kc, :], in_=stg[sl][:, 0:D]),
                           reads=[B_stg[sl]], writes=[Buf("t")])
                i += 1
            barrier()

        def C(name, bf=False, rows=128, sub=None):
            o, w = CL.d[name]
            t = cbf if bf else cf
            if not bf:
                assert o + w <= CF32
            if sub is not None:
                so, sw = sub
                return t[0:rows, o + so:o + so + sw]
            return t[0:rows, o:o + w]

        IND_O = CL.d["IND"][0]
        ident_bf = C("IDENT", bf=True)
        ident_f = C("IDENT")

        xo = [sb(f"xo{i}", [128, D]) for i in range(2)]
        B_xo = [Buf(f"xo{i}") for i in range(2)]
        ch_xo = [chan(f"c_xo{i}") for i in range(2)]
        ch_st = [chan(f"c_st{i}") for i in range(2)]
        ch_xs = chan("c_xs")
        aqT = sb("aqT", [128, 8, BT], BF16)
        B_aq = [Buf(f"aq{p}") for p in range(4)]
        akT = sb("akT", [128, 4, T], BF16)
        B_ak = [[Buf(f"ak{p}_{b}") for b in range(NB)] for p in range(4)]
        vtm = sb("vtm", [128, 16, 8, 65], BF16)
        B_v = [Buf(f"v{t}") for t in range(16)]
        siluz = sb("siluz", [128, TPB, 512], BF16)
        B_sz = [Buf(f"sz{t}") for t in range(TPB)]
        zw = sb("zw", [128, TPB, 512], BF16)
        B_zw = [Buf(f"zw{t}") for t in range(TPB)]
        sraw = sb("sraw", [128, TPB, 8])
        B_sraw = Buf("sraw")
        dT = [sb(f"dT{k}", [128, 4, BT], BF16) for k in range(3)]
        B_dT = [[Buf(f"dT{k}_{h}") for h in range(4)] for k in range(3)]
        halo = sb("halo", [128, 12, 4], BF16)
        B_halo = [Buf(f"halo{c}") for c in range(12)]
        mixed = sb("mixed", [128, TPB, D], BF16)
        B_mx = [[Buf(f"mx{t}_{i}") for i in range(2)] for t in range(TPB)]
        mixT = sb("mixT", [128, 8, 128], BF16)
        B_mixT = Buf("mixT")
        tmpf = [sb(f"tmpf{i}", [128, 512]) for i in range(2)]
        B_tmpf = [Buf("tmpf0"), Buf("tmpf1")]
        small = sb("small", [128, 64])
        B_small = Buf("small")
        junk = sb("junk", [128, D], BF16)
        B_junk = Buf("junk")
        nea = sb("nea", [128, 4])
        Sst = sb("Sst", [128, 4, 128])
        Sbf = sb("Sbf", [128, 4, 128], BF16)
        B_S, B_Sbf = Buf("S"), Buf("Sbf")
        kbar = sb("kbar", [128, 4, 8])
        kbar_hi = sb("kbar_hi", [128, 4, 8], BF16)
        kbar_lo = sb("kbar_lo", [128, 4, 8], BF16)
        kbar_t = sb("kbar_t", [128, 4, 8])
        B_kbar = Buf("kbar")
        gm = sb("gm", [128, 8, 8])
        top8 = sb("top8", [128, 8, 8])
        selb = sb("selb", [128, 8, 72], BF16)
        B_gm = Buf("gm")
        selT = sb("selT", [128, 8, BT], BF16)
        B_selT = Buf("selT")
        NPT = 4
        PTs = sb("PTs", [128, NPT, BT], BF16)
        B_PT = [Buf(f"PT{i}") for i in range(NPT)]
        B_pSreg = [[Buf(f"pSreg{i}_{j}") for j in range(4)] for i in range(2)]
        att_t = sb("att_t", [128, 4, 64])
        rden = sb("rden", [128, 4])
        B_att = Buf("att_t")

        pBh = [p[:, :].rearrange("p (h d) -> p h d", h=4) for p in pB]
        pO = [pB[3], pS]
        B_pO = [B_pB[3], B_pS]
        pOh = [p[:, :].rearrange("p (h d) -> p h d", h=4) for p in pO]
        pTh = pT[:, :].rearrange("p (h d) -> p h d", h=8)

        def bc(ap, shape):
            return ap.to_broadcast(list(shape))

        def merge(dst, srcs):
            for sbuf in srcs:
                dst.w = dst.w + sbuf.w
                dst.r = dst.r + sbuf.r

        POOL.op(lambda: G.memset(vtm[:, :, :, 64:65], 1.0), writes=B_v)
        POOL.op(lambda: G.memset(kbar[:], 0.0), writes=[B_kbar])
        POOL.op(lambda: G.memset(aqT[:], 0.0), writes=B_aq)
        POOL.op(lambda: G.memset(selT[:], 0.0), writes=[B_selT])
        POOL.op(lambda: G.memset(selb[:], 0.0), writes=[B_gm])
        ACT.op(lambda: A.activation(out=nea[:], in_=alog_bc, func=AF.Exp), reads=[B_c], writes=[B_small])
        DVE.op(lambda: V.tensor_scalar(out=nea[:], in0=nea[:], scalar1=-1.0, scalar2=None, op0=ALU.mult),
               reads=[B_small], writes=[B_small])
        barrier()

        dbg_toks = []

        def dump(name, ap, bufs):
            if name not in dbg_aps:
                return
            ch = chan("c_dbg_" + name)
            dbg_chans.append(ch)
            dbg_toks.append(dma(POOL, ch, dbg_aps[name], ap, reads=bufs))

        state = {"xo": 0, "pt": 0, "uid": 0}

        def inproj_block(s, b, ea):
            t0 = b * BT
            uid = state["uid"]
            xs = sb(f"xs_{uid}", [128, D], st=ea)
            xn = sb(f"xn_{uid}", [128, D], BF16, st=ea)
            hT = sb(f"hT_{uid}", [128, 8, BT], BF16, st=ea)
            pre = sb(f"pre_{uid}", [128, 2, BT + 8], BF16, st=ea)
            cdg = sb(f"cdg_{uid}", [128, 2, 4, 128], BF16, st=ea)
            B_xs, B_xn = Buf("xs"), Buf("xn")
            B_hT = [Buf(f"hT{t}") for t in range(TPB)]
            B_pre = [Buf("pre0"), Buf("pre1")]
            B_cdg = [Buf("cdg0"), Buf("cdg1")]
            for tt in range(TPB):
                gt = b * TPB + tt
                dma(SP, ch_xs, xs[:], x[s, gt * 128:(gt + 1) * 128, :], writes=[B_xs])
                ACT.op(lambda: A.activation(out=junk[:], in_=xs[:], func=AF.Square, accum_out=small[:, 0:1]),
                       reads=[B_xs], writes=[B_junk, B_small])
                ACT.op(lambda: A.activation(out=small[:, 1:2], in_=small[:, 0:1], func=AF.Ln,
                                            scale=1.0 / D, bias=EPS), reads=[B_small], writes=[B_small])
                ACT.op(lambda: A.activation(out=small[:, 2:3], in_=small[:, 1:2], func=AF.Exp, scale=-0.5),
                       reads=[B_small], writes=[B_small])
                DVE.op(lambda: V.tensor_scalar(out=xn[:], in0=xs[:], scalar1=small[:, 2:3], scalar2=None,
                                               op0=ALU.mult), reads=[B_xs, B_small], writes=[B_xn])

                if os.environ.get("K_STOP") == "p1a":
                    return

                def tr():
                    for kc in range(8):
                        ins = TE.transpose(out=pT[:, kc * 128:(kc + 1) * 128], in_=xn[:, kc * 128:(kc + 1) * 128],
                                           identity=ident_bf)
                    return ins
                yield
                PE.op(tr, reads=[B_xn], writes=[B_pT])
                if os.environ.get("K_STOP") == "p1b":
                    return
                ACT.op(lambda tt=tt: A.copy(out=hT[:, 0:4, tt * 128:(tt + 1) * 128], in_=pTh[:, 0:4, :]),
                       reads=[B_pT], writes=[Buf("t")])
                if os.environ.get("K_STOP") == "p1c":
                    return
                DVE.op(lambda tt=tt: V.tensor_copy(out=hT[:, 4:8, tt * 128:(tt + 1) * 128], in_=pTh[:, 4:8, :]),
                       reads=[B_pT], writes=[B_hT[tt]])
                if os.environ.get("K_STOP") == "p1d":
                    return
            if s == 0 and b == 0:
                dump("hT", hT[:, 0, :], B_hT)

            if os.environ.get("K_STOP") == "p1":
                return
            def fm_chunk(col0, bank):
                def f():
                    for kc in range(8):
                        ins = TE.matmul(pA[bank][:, 0:BT], lhsT=w_in_bf[:, kc, col0:col0 + 128], rhs=hT[:, kc, :],
                                        start=(kc == 0), stop=(kc == 7))
                    return ins
                PE.op(f, reads=B_hT, writes=[B_pA[bank]])

            ci_all = 0
            for p in range(4):
                bank = ci_all % 2
                ci_all += 1
                yield
                fm_chunk(0 + p * 128, bank)
                ACT.op(lambda p=p, bank=bank: A.copy(out=aqT[0:64, 2 * p, :], in_=pA[bank][0:64, 0:BT]),
                       reads=[B_pA[bank]], writes=[B_aq[p]])
                ACT.op(lambda p=p, bank=bank: A.copy(out=aqT[64:128, 2 * p + 1, :], in_=pA[bank][64:128, 0:BT]),
                       reads=[B_pA[bank]], writes=[B_aq[p]])
            for p in range(4):
                bank = ci_all % 2
                ci_all += 1
                yield
                fm_chunk(512 + p * 128, bank)
                DVE.op(lambda p=p, bank=bank: V.tensor_copy(out=akT[:, p, t0:t0 + BT], in_=pA[bank][:, 0:BT]),
                       reads=[B_pA[bank]], writes=[B_ak[p][b]])
            for kind in range(3):
                for hd in range(4):
                    ci = kind * 4 + hd
                    bank = ci_all % 2
                    ci_all += 1
                    yield
                    fm_chunk(2048 + ci * 128, bank)
                    sl = ci % 2
                    for tap in range(4):
                        DVE.op(lambda sl=sl, tap=tap, ci=ci: V.tensor_scalar(
                            out=cdg[:, sl, tap, :], in0=ident_f, scalar1=convw[:, tap * 12 + ci:tap * 12 + ci + 1],
                            scalar2=None, op0=ALU.mult), writes=[B_cdg[sl]])
                    if b == 0:
                        POOL.op(lambda sl=sl: G.memset(pre[:, sl, 0:4], 0.0), writes=[B_pre[sl]])
                    else:
                        POOL.op(lambda sl=sl, ci=ci: G.tensor_copy(out=pre[:, sl, 0:4], in_=halo[:, ci, :]),
                                reads=[B_halo[ci]], writes=[B_pre[sl]])
                    ACT.op(lambda sl=sl, bank=bank: A.copy(out=pre[:, sl, 4:4 + BT], in_=pA[bank][:, 0:BT]),
                           reads=[B_pA[bank]], writes=[B_pre[sl]])
                    POOL.op(lambda sl=sl, ci=ci: G.tensor_copy(out=halo[:, ci, :], in_=pre[:, sl, BT:BT + 4]),
                            reads=[B_pre[sl]], writes=[B_halo[ci]])
                    cb = ci % 2

                    def cv(sl=sl, cb=cb):
                        for tap in range(4):
                            ins = TE.matmul(pB[cb][:, 0:BT], lhsT=cdg[:, sl, tap, :],
                                            rhs=pre[:, sl, 1 + tap:1 + tap + BT], start=(tap == 0), stop=(tap == 3))
                        return ins
                    yield
                    PE.op(cv, reads=[B_pre[sl], B_cdg[sl]], writes=[B_pB[cb]])
                    ACT.op(lambda kind=kind, hd=hd, cb=cb: A.activation(out=dT[kind][:, hd, :], in_=pB[cb][:, 0:BT],
                                                                        func=AF.Silu),
                           reads=[B_pB[cb]], writes=[B_dT[kind][hd]])
            if s == 0 and b == 0:
                dump("aqT", aqT[:, 0, :], B_aq)
                dump("dqT", dT[0][:, 0, :], B_dT[0])
                dump("dkT", dT[1][:, 0, :], B_dT[1])

            if os.environ.get("K_STOP") == "p2":
                return
            for tt in range(TPB):
                gt = b * TPB + tt

                tmb = [(pB[1], B_pB[1]), (pB[2], B_pB[2]), (pB[3], B_pB[3])] if tt % 2 == 0 else \
                      [(pA[0], B_pA[0]), (pA[1], B_pA[1]), (pB[0], B_pB[0])]

                def tm(tt=tt, tmb=tmb):
                    for j, col0 in enumerate((1024, 1536, 3584)):
                        for kc in range(8):
                            ins = TE.matmul(tmb[j][0][:, :], lhsT=hT[:, kc, tt * 128:(tt + 1) * 128],
                                            rhs=w_in_bf[:, kc, col0:col0 + 512], start=(kc == 0), stop=(kc == 7))
                    for kc in range(8):
                        ins = TE.matmul(pS[:, 0:8], lhsT=hT[:, kc, tt * 128:(tt + 1) * 128],
                                        rhs=w_in_bf[:, kc, 4096:4104], start=(kc == 0), stop=(kc == 7))
                    return ins
                yield
                PE.op(tm, reads=B_hT, writes=[tmb[0][1], tmb[1][1], tmb[2][1], B_pS])
                ACT.op(lambda gt=gt, tmb=tmb: A.copy(out=vtm[:, gt, :, 0:64],
                                                     in_=tmb[0][0][:, :].rearrange("p (h d) -> p h d", h=8)),
                       reads=[tmb[0][1]], writes=[B_v[gt]])
                ACT.op(lambda tt=tt, tmb=tmb: A.activation(out=siluz[:, tt, :], in_=tmb[1][0][:, :], func=AF.Silu),
                       reads=[tmb[1][1]], writes=[B_sz[tt]])
                fsl = tt % 2
                ACT.op(lambda fsl=fsl, tmb=tmb: A.activation(out=tmpf[fsl][:], in_=tmb[2][0][:, :], func=AF.Silu),
                       reads=[tmb[2][1]], writes=[B_tmpf[fsl]])
                POOL.op(lambda tt=tt, fsl=fsl: G.tensor_tensor(out=zw[:, tt, :], in0=tmpf[fsl][:], in1=gnw_bc,
                                                               op=ALU.mult),
                        reads=[B_tmpf[fsl]], writes=[B_zw[tt]])
                DVE.op(lambda tt=tt: V.tensor_copy(out=sraw[:, tt, :], in_=pS[:, 0:8]), reads=[B_pS], writes=[B_sraw])
            if s == 0 and b == 0:
                dump("sraw", sraw[:, 0, :], [B_sraw])
                dump("vtm", vtm[:, 0, 0, :], B_v)
                dump("zw", zw[:, 0, :], B_zw)

        def gdn_block(s, b, eb):
            uid = state["uid"]
            g = NS()

            def gb(name, shape, dt=F32):
                return sb(f"{name}_{uid}", shape, dt, st=eb)
            g.st_lnr = gb("st_lnr", [128, TPB, 8])
            g.st_lnbn = gb("st_lnbn", [128, TPB, 4])
            g.st_g = gb("st_g", [128, TPB, 4])
            g.st_e = gb("st_e", [128, TPB, 8])
            g.ST = gb("ST", [128, TPB, 12])
            g.st_c = gb("st_c", [128, TPB, 4])
            g.ex_a = gb("ex_a", [128, TPB, 4])
            g.ex_c = gb("ex_c", [128, TPB, 4])
            g.ex_b = gb("ex_b", [128, TPB, 4])
            g.ex_gl = gb("ex_gl", [128, TPB, 4])
            g.STs = gb("STs", [128, TPB, 36], BF16)
            g.STr = gb("STr", [128, TPB, 12])
            g.STh = gb("STh", [128, TPB, 12])
            g.STT = gb("STT", [128, TPB, 128], BF16)
            g.sq = [gb(f"sq{i}", [128, 4, BT], BF16) for i in range(2)]
            g.Eb = [gb(f"Eb{i}", [128, 4, 128], BF16) for i in range(2)]
            g.Xb = [gb(f"Xb{i}", [128, 4, 128], BF16) for i in range(2)]
            g.Yb = [gb(f"Yb{i}", [128, 4, 128], BF16) for i in range(2)]
            g.Pb = [gb(f"Pb{i}", [128, 4, 128], BF16) for i in range(2)]
            g.intraT = gb("intraT", [128, 4, 128], BF16)
            g.kbg = gb("kbg", [128, 4, 128], BF16)
            g.kdec = gb("kdec", [128, 4, 128], BF16)
            g.vb = gb("vb", [128, 4, 128], BF16)
            g.qdT = gb("qdT", [128, 4, 128], BF16)
            g.u_sb = gb("u_sb", [128, 4, 128])
            g.wT = gb("wT", [128, 4, 128], BF16)
            g.vnew = gb("vnew", [128, 4, 128], BF16)
            g.osq = gb("osq", [128, 4, 128])
            g.ost = gb("ost", [128, 16])
            g.B_st, g.B_STT = Buf("stats"), Buf("STT")
            g.B_sq = [Buf("sqq"), Buf("sqk")]
            g.B_Eb = [Buf("Eb0"), Buf("Eb1")]
            g.B_X = [Buf("X0"), Buf("X1")]
            g.B_Y = [Buf("Y0"), Buf("Y1")]
            g.B_P = [Buf("P0"), Buf("P1")]
            g.B_iT, g.B_kbg, g.B_kdec, g.B_vb, g.B_qdT = Buf("iT"), Buf("kbg"), Buf("kdec"), Buf("vb"), Buf("qdT")
            g.B_u, g.B_wT, g.B_vn, g.B_osq, g.B_ost = Buf("u"), Buf("wT"), Buf("vn"), Buf("osq"), Buf("ost")
            B_st = g.B_st
            ST, st_lnr, st_lnbn, st_g, st_e, st_c = g.ST, g.st_lnr, g.st_lnbn, g.st_g, g.st_e, g.st_c
            STs, STr, STh, STT = g.STs, g.STr, g.STh, g.STT
            if b == 0:
                DVE.op(lambda: V.memset(Sst[:], 0.0), writes=[B_S])
                POOL.op(lambda: G.memset(Sbf[:], 0.0), writes=[B_Sbf])
            for k in range(2):
                POOL.op(lambda k=k: G.tensor_tensor(out=g.sq[k][:], in0=dT[k][:], in1=dT[k][:], op=ALU.mult),
                        reads=B_dT[k], writes=[g.B_sq[k]])
            ones_col = C("ONES", bf=True, sub=(0, 1))

            def ssq():
                for tt in range(TPB):
                    for k in range(2):
                        for hd in range(4):
                            c0 = 64 + tt * 8 + k * 4 + hd
                            ins = TE.matmul(pB[0][:, c0:c0 + 1], lhsT=g.sq[k][:, hd, tt * 128:(tt + 1) * 128],
                                            rhs=ones_col, start=True, stop=True)
                return ins
            yield
            PE.op(ssq, reads=g.B_sq, writes=[B_pB[0]])
            pS_ssq = pB[0][:, 64:64 + 8 * TPB].rearrange("p (t k) -> p t k", t=TPB)
            ACT.op(lambda: A.activation(out=st_lnr[:], in_=pS_ssq, func=AF.Ln, bias=EPS),
                   reads=[B_pB[0]], writes=[B_st])
            ACT.op(lambda: A.activation(out=st_e[:, :, 0:4], in_=sraw[:, :, 0:4], func=AF.Exp, scale=-1.0),
                   reads=[B_sraw, B_st], writes=[B_st])
            ACT.op(lambda: A.activation(out=st_lnbn[:], in_=st_e[:, :, 0:4], func=AF.Ln, bias=1.0),
                   reads=[B_st], writes=[B_st])
            DVE.op(lambda: V.tensor_tensor(out=st_e[:, :, 4:8], in0=sraw[:, :, 4:8],
                                           in1=bc(dtb_bc.unsqueeze(1), [128, TPB, 4]), op=ALU.add),
                   reads=[B_sraw, B_st], writes=[B_st])
            ACT.op(lambda: A.activation(out=st_e[:, :, 4:8], in_=st_e[:, :, 4:8], func=AF.Exp),
                   reads=[B_st], writes=[B_st])
            ACT.op(lambda: A.activation(out=st_e[:, :, 4:8], in_=st_e[:, :, 4:8], func=AF.Ln, bias=1.0),
                   reads=[B_st], writes=[B_st])
            DVE.op(lambda: V.tensor_tensor(out=st_g[:], in0=st_e[:, :, 4:8], in1=bc(nea[:].unsqueeze(1), [128, TPB, 4]),
                                           op=ALU.mult), reads=[B_st], writes=[B_st])

            def gcm():
                for tt in range(TPB):
                    TE.matmul(pB[0][:, 96 + tt * 4:100 + tt * 4], lhsT=C("TRI"), rhs=st_g[:, tt, :], start=True, stop=True)
                    ins = TE.matmul(pB[0][:, 112 + tt * 4:116 + tt * 4], lhsT=C("ONES"), rhs=st_g[:, tt, :],
                                    start=True, stop=True)
                return ins
            yield
            PE.op(gcm, reads=[B_st], writes=[B_pB[0]])
            gc = pB[0][:, 96:96 + 4 * TPB].rearrange("p (t k) -> p t k", t=TPB)
            gl = pB[0][:, 112:112 + 4 * TPB].rearrange("p (t k) -> p t k", t=TPB)
            lnrq = st_lnr[:, :, 0:4]
            lnrk = st_lnr[:, :, 4:8]
            DVE.op(lambda: V.scalar_tensor_tensor(out=ST[:, :, 4:8], in0=lnrk, scalar=0.5, op0=ALU.mult, in1=gc,
                                                  op1=ALU.add), reads=[B_st, B_pB[0]], writes=[B_st])
            DVE.op(lambda: V.scalar_tensor_tensor(out=ST[:, :, 0:4], in0=lnrk, scalar=-0.5, op0=ALU.mult, in1=gc,
                                                  op1=ALU.add), reads=[B_st, B_pB[0]], writes=[B_st])
            DVE.op(lambda: V.scalar_tensor_tensor(out=st_c[:], in0=ST[:, :, 4:8], scalar=-1.0, op0=ALU.mult, in1=gl,
                                                  op1=ALU.add), reads=[B_st, B_pB[0]], writes=[B_st])
            DVE.op(lambda: V.tensor_tensor(out=ST[:, :, 0:4], in0=ST[:, :, 0:4], in1=st_lnbn[:], op=ALU.subtract),
                   reads=[B_st], writes=[B_st])
            DVE.op(lambda: V.scalar_tensor_tensor(out=ST[:, :, 8:12], in0=lnrq, scalar=-0.5, op0=ALU.mult, in1=gc,
                                                  op1=ALU.add), reads=[B_st, B_pB[0]], writes=[B_st])
            DVE.op(lambda: V.tensor_scalar(out=ST[:, :, 8:12], in0=ST[:, :, 8:12], scalar1=LN_QS, scalar2=None,
                                           op0=ALU.add), reads=[B_st], writes=[B_st])
            ACT.op(lambda: A.activation(out=g.ex_a[:], in_=ST[:, :, 0:4], func=AF.Exp), reads=[B_st], writes=[B_st])
            ACT.op(lambda: A.activation(out=g.ex_c[:], in_=st_c[:], func=AF.Exp), reads=[B_st], writes=[B_st])
            ACT.op(lambda: A.activation(out=g.ex_b[:], in_=st_lnbn[:], func=AF.Exp, scale=-1.0),
                   reads=[B_st], writes=[B_st])
            ACT.op(lambda: A.activation(out=g.ex_gl[:], in_=gl, func=AF.Exp), reads=[B_st, B_pB[0]], writes=[B_st])
            DVE.op(lambda: V.tensor_copy(out=STs[:, :, 0:12], in_=ST[:]), reads=[B_st], writes=[B_st])
            DVE.op(lambda: V.tensor_copy(out=STh[:], in_=STs[:, :, 0:12]), reads=[B_st], writes=[B_st])
            DVE.op(lambda: V.tensor_tensor(out=STr[:], in0=ST[:], in1=STh[:], op=ALU.subtract),
                   reads=[B_st], writes=[B_st])
            DVE.op(lambda: V.tensor_copy(out=STs[:, :, 12:24], in_=STr[:]), reads=[B_st], writes=[B_st])
            DVE.op(lambda: V.tensor_copy(out=STh[:], in_=STs[:, :, 12:24]), reads=[B_st], writes=[B_st])
            DVE.op(lambda: V.tensor_tensor(out=STr[:], in0=STr[:], in1=STh[:], op=ALU.subtract),
                   reads=[B_st], writes=[B_st])
            DVE.op(lambda: V.tensor_copy(out=STs[:, :, 24:36], in_=STr[:]), reads=[B_st], writes=[B_st])

            def trs():
                for tt in range(TPB):
                    ins = TE.transpose(out=pT[0:36, tt * 128:(tt + 1) * 128], in_=STs[:, tt, :], identity=ident_bf)
                return ins
            yield
            PE.op(trs, reads=[B_st], writes=[B_pT])
            POOL.op(lambda: G.memset(STT[:], 0.0), writes=[g.B_STT])
            ACT.op(lambda: A.copy(out=STT[0:36], in_=pT[0:36, 0:128 * TPB].rearrange("p (t k) -> p t k", t=TPB)),
                   reads=[B_pT], writes=[g.B_STT])
            if s == 0 and b == 0:
                dump("ST", ST[:, 0, :], [B_st])
                dump("exa", g.ex_a[:, 0, :], [B_st])
                dump("sg", st_g[:, 0, :], [B_st])
            for tt in range(TPB):
                yield from gdn_chunk(s, b, tt, g)

        def gdn_chunk(s, b, tt, g):
            cs = slice(tt * 128, (tt + 1) * 128)
            first = (s == 0 and b == 0 and tt == 0)
            qT, kT, vT = dT[0], dT[1], dT[2]
            STc = g.STT[:, tt, :]
            B_st, B_STT = g.B_st, g.B_STT
            Eb, Xb, Yb, Pb = g.Eb, g.Xb, g.Yb, g.Pb
            B_Eb, B_X, B_Y, B_P = g.B_Eb, g.B_X, g.B_Y, g.B_P

            def sel(which, hd):
                return C("SEL", bf=True, rows=128, sub=((which * 4 + hd) * 128, 128))

            def trkv():
                for hd in range(4):
                    TE.transpose(out=pT[:, hd * 128:(hd + 1) * 128], in_=kT[:, hd, cs], identity=ident_bf)
                for hd in range(4):
                    ins = TE.transpose(out=pT[:, 512 + hd * 128:512 + (hd + 1) * 128], in_=vT[:, hd, cs],
                                       identity=ident_bf)
                return ins
            yield
            PE.op(trkv, reads=B_dT[1] + B_dT[2], writes=[B_pT])
            DVE.op(lambda: V.tensor_tensor(out=g.kbg[:], in0=pTh[:, 0:4, :],
                                           in1=bc(g.ex_a[:, tt, :].unsqueeze(2), [128, 4, 128]), op=ALU.mult),
                   reads=[B_pT, B_st], writes=[g.B_kbg])
            DVE.op(lambda: V.tensor_tensor(out=g.kdec[:], in0=pTh[:, 0:4, :],
                                           in1=bc(g.ex_c[:, tt, :].unsqueeze(2), [128, 4, 128]), op=ALU.mult),
                   reads=[B_pT, B_st], writes=[g.B_kdec])
            DVE.op(lambda: V.tensor_tensor(out=g.vb[:], in0=pTh[:, 4:8, :],
                                           in1=bc(g.ex_b[:, tt, :].unsqueeze(2), [128, 4, 128]), op=ALU.mult),
                   reads=[B_pT, B_st], writes=[g.B_vb])

            def kkqk():
                for hd in range(4):
                    TE.matmul(pB[0][:, hd * 128:(hd + 1) * 128], lhsT=kT[:, hd, cs], rhs=kT[:, hd, cs],
                              start=True, stop=True)
                for hd in range(4):
                    ins = TE.matmul(pB[1][:, hd * 128:(hd + 1) * 128], lhsT=kT[:, hd, cs], rhs=qT[:, hd, cs],
                                    start=True, stop=True)
                return ins
            yield
            PE.op(kkqk, reads=B_dT[0] + B_dT[1], writes=[B_pB[0], B_pB[1]])

            def dmat(bank, which, mask, lower):
                def f():
                    for hd in range(4):
                        o = pB[bank][:, hd * 128:(hd + 1) * 128]
                        if lower:
                            TE.matmul(o, lhsT=STc, rhs=sel(which, hd), start=True, stop=False)
                            TE.matmul(o, lhsT=sel(SEL_NB, hd), rhs=STc, start=False, stop=False)
                        else:
                            TE.matmul(o, lhsT=sel(which, hd), rhs=STc, start=True, stop=False)
                            TE.matmul(o, lhsT=STc, rhs=sel(SEL_NB, hd), start=False, stop=False)
                        ins = TE.matmul(o, lhsT=ident_bf, rhs=C(mask, bf=True), start=False, stop=True)
                    return ins
                PE.op(f, reads=[B_STT], writes=[B_pB[bank]])

            yield
            dmat(2, SEL_A, "MASK_LS", True)
            ACT.op(lambda: A.activation(out=Eb[0][:], in_=pBh[2], func=AF.Exp), reads=[B_pB[2]], writes=[B_Eb[0]])
            DVE.op(lambda: V.scalar_tensor_tensor(out=Xb[0][:], in0=pBh[0], scalar=-1.0, op0=ALU.mult, in1=Eb[0][:],
                                                  op1=ALU.mult), reads=[B_pB[0], B_Eb[0]], writes=[B_X[0]])
            yield
            dmat(2, SEL_A, "MASK_US", False)
            ACT.op(lambda: A.activation(out=Eb[1][:], in_=pBh[2], func=AF.Exp), reads=[B_pB[2]], writes=[B_Eb[1]])
            DVE.op(lambda: V.scalar_tensor_tensor(out=Yb[0][:], in0=pBh[0], scalar=-1.0, op0=ALU.mult, in1=Eb[1][:],
                                                  op1=ALU.mult), reads=[B_pB[0], B_Eb[1]], writes=[B_Y[0]])
            yield
            dmat(2, SEL_AP, "MASK_UI", False)
            ACT.op(lambda: A.activation(out=Eb[0][:], in_=pBh[2], func=AF.Exp), reads=[B_pB[2]], writes=[B_Eb[0]])
            DVE.op(lambda: V.tensor_tensor(out=g.intraT[:], in0=pBh[1], in1=Eb[0][:], op=ALU.mult),
                   reads=[B_pB[1], B_Eb[0]], writes=[g.B_iT])

            def fb():
                for hd in range(4):
                    ins = TE.matmul(pB[2][:, hd * 128:(hd + 1) * 128], lhsT=sel(SEL_AP, hd), rhs=STc,
                                    start=True, stop=True)
                return ins
            yield
            PE.op(fb, reads=[B_STT], writes=[B_pB[2]])
            ACT.op(lambda: A.activation(out=Eb[1][:], in_=pBh[2], func=AF.Exp), reads=[B_pB[2]], writes=[B_Eb[1]])
            POOL.op(lambda: G.tensor_tensor(out=g.qdT[:], in0=qT[:, :, cs], in1=Eb[1][:], op=ALU.mult),
                    reads=B_dT[0] + [B_Eb[1]], writes=[g.B_qdT])
            if first:
                dump("X0", Xb[0][:, 0, :], [B_X[0]])
                dump("Y0", Yb[0][:, 0, :], [B_Y[0]])
                dump("intraT", g.intraT[:, 0, :], [g.B_iT])
                dump("qdT", g.qdT[:, 0, :], [g.B_qdT])
                dump("kbg", g.kbg[:, 0, :], [g.B_kbg])

            POOL.op(lambda: G.tensor_tensor(out=Pb[0][:], in0=Yb[0][:],
                                            in1=bc(ident_bf.unsqueeze(1), [128, 4, 128]), op=ALU.add),
                    reads=[B_Y[0]], writes=[B_P[0]])
            cur = 0
            pc = 0
            for lvl in range(1, 7):
                nxt = 1 - cur

                def sqx(cur=cur):
                    for hd in range(4):
                        ins = TE.matmul(pB[0][:, hd * 128:(hd + 1) * 128], lhsT=Yb[cur][:, hd, :], rhs=Xb[cur][:, hd, :],
                                        start=True, stop=True)
                    return ins
                yield
                PE.op(sqx, reads=[B_X[cur], B_Y[cur]], writes=[B_pB[0]])
                if lvl < 6:
                    def sqy(cur=cur):
                        for hd in range(4):
                            ins = TE.matmul(pB[1][:, hd * 128:(hd + 1) * 128], lhsT=Xb[cur][:, hd, :],
                                            rhs=Yb[cur][:, hd, :], start=True, stop=True)
                        return ins
                    PE.op(sqy, reads=[B_X[cur], B_Y[cur]], writes=[B_pB[1]])
                ACT.op(lambda nxt=nxt: A.copy(out=Xb[nxt][:], in_=pBh[0]), reads=[B_pB[0]], writes=[B_X[nxt]])
                if lvl < 6:
                    DVE.op(lambda nxt=nxt: V.tensor_copy(out=Yb[nxt][:], in_=pBh[1]), reads=[B_pB[1]],
                           writes=[B_Y[nxt]])
                pn = 1 - pc

                def pm(nxt=nxt, pc=pc):
                    for hd in range(4):
                        ins = TE.matmul(pB[2][:, hd * 128:(hd + 1) * 128], lhsT=Xb[nxt][:, hd, :], rhs=Pb[pc][:, hd, :],
                                        start=True, stop=True)
                    return ins
                yield
                PE.op(pm, reads=[B_X[nxt], B_P[pc]], writes=[B_pB[2]])
                DVE.op(lambda pn=pn, pc=pc: V.tensor_tensor(out=Pb[pn][:], in0=pBh[2], in1=Pb[pc][:], op=ALU.add),
                       reads=[B_pB[2], B_P[pc]], writes=[B_P[pn]])
                cur = nxt
                pc = pn
            TT = Pb[pc]
            B_TT = B_P[pc]
            if first:
                dump("TT", TT[:, 0, :], [B_TT])

            def uw():
                for hd in range(4):
                    TE.matmul(pB[0][:, hd * 128:(hd + 1) * 128], lhsT=TT[:, hd, :], rhs=g.vb[:, hd, :],
                              start=True, stop=True)
                for hd in range(4):
                    ins = TE.matmul(pB[1][:, hd * 128:(hd + 1) * 128], lhsT=g.kbg[:, hd, :], rhs=TT[:, hd, :],
                                    start=True, stop=True)
                return ins
            yield
            PE.op(uw, reads=[B_TT, g.B_vb, g.B_kbg], writes=[B_pB[0], B_pB[1]])
            ACT.op(lambda: A.copy(out=g.u_sb[:], in_=pBh[0]), reads=[B_pB[0]], writes=[g.B_u])
            DVE.op(lambda: V.tensor_copy(out=g.wT[:], in_=pBh[1]), reads=[B_pB[1]], writes=[g.B_wT])

            def ws():
                for hd in range(4):
                    ins = TE.matmul(pB[2][:, hd * 128:(hd + 1) * 128], lhsT=g.wT[:, hd, :], rhs=Sbf[:, hd, :],
                                    start=True, stop=True)
                return ins
            yield
            PE.op(ws, reads=[g.B_wT, B_Sbf], writes=[B_pB[2]])
            DVE.op(lambda: V.scalar_tensor_tensor(out=g.vnew[:], in0=pBh[2], scalar=-1.0, op0=ALU.mult, in1=g.u_sb[:],
                                                  op1=ALU.add), reads=[B_pB[2], g.B_u], writes=[g.B_vn])

            def oo():
                for hd in range(4):
                    TE.matmul(pB[2][:, hd * 128:(hd + 1) * 128], lhsT=g.qdT[:, hd, :], rhs=Sbf[:, hd, :],
                              start=True, stop=False)
                    TE.matmul(pB[2][:, hd * 128:(hd + 1) * 128], lhsT=g.intraT[:, hd, :], rhs=g.vnew[:, hd, :],
                              start=False, stop=True)
                for hd in range(4):
                    ins = TE.matmul(pB[0][:, hd * 128:(hd + 1) * 128], lhsT=g.kdec[:, hd, :], rhs=g.vnew[:, hd, :],
                                    start=True, stop=True)
                return ins
            yield
            PE.op(oo, reads=[g.B_qdT, B_Sbf, g.B_iT, g.B_vn, g.B_kdec], writes=[B_pB[2], B_pB[0]])
            DVE.op(lambda: V.tensor_tensor(out=Sst[:], in0=Sst[:],
                                           in1=bc(g.ex_gl[:, tt, :].unsqueeze(2), [128, 4, 128]), op=ALU.mult),
                   reads=[B_st], writes=[B_S])
            DVE.op(lambda: V.tensor_tensor(out=Sst[:], in0=pBh[0], in1=Sst[:], op=ALU.add),
                   reads=[B_pB[0]], writes=[B_S])
            POOL.op(lambda: G.tensor_copy(out=Sbf[:], in_=Sst[:]), reads=[B_S], writes=[B_Sbf])
            ACT.op(lambda: A.activation(out=g.osq[:], in_=pBh[2], func=AF.Square), reads=[B_pB[2]], writes=[g.B_osq])
            DVE.op(lambda: V.tensor_reduce(out=g.ost[:, 0:4], in_=g.osq[:], op=ALU.add, axis=AX),
                   reads=[g.B_osq], writes=[g.B_ost])
            ACT.op(lambda: A.activation(out=g.ost[:, 4:8], in_=g.ost[:, 0:4], func=AF.Ln, scale=1.0 / 128, bias=EPS),
                   reads=[g.B_ost], writes=[g.B_ost])
            ACT.op(lambda: A.activation(out=g.ost[:, 8:12], in_=g.ost[:, 4:8], func=AF.Exp, scale=-0.5),
                   reads=[g.B_ost], writes=[g.B_ost])
            DVE.op(lambda: V.tensor_tensor(out=g.osq[:], in0=pBh[2],
                                           in1=bc(g.ost[:, 8:12].unsqueeze(2), [128, 4, 128]), op=ALU.mult),
                   reads=[B_pB[2], g.B_ost], writes=[g.B_osq])
            POOL.op(lambda: G.tensor_tensor(out=mixed[:, tt, 512:1024], in0=g.osq[:].rearrange("p h d -> p (h d)"),
                                            in1=zw[:, tt, :], op=ALU.mult),
                    reads=[g.B_osq, B_zw[tt]], writes=[B_mx[tt][1]])
            if first:
                dump("u", g.u_sb[:, 0, :], [g.B_u])
                dump("gdn_out", mixed[:, 0, 512:1024], [B_mx[0][1]])

        def attn_block(s, b):
            t0 = b * BT
            ob = b
            use_sel = ob >= 4
            DVE.op(lambda: V.tensor_reduce(out=kbar[:, :, b:b + 1], in_=akT[:, :, t0:t0 + BT].unsqueeze(2),
                                           op=ALU.add, axis=AX),
                   reads=[B_ak[p][b] for p in range(4)], writes=[B_kbar])
            DVE.op(lambda: V.tensor_scalar(out=kbar[:, :, b:b + 1], in0=kbar[:, :, b:b + 1],
                                           scalar1=1.0 / 256, scalar2=None, op0=ALU.mult),
                   reads=[B_kbar], writes=[B_kbar])
            DVE.op(lambda: V.tensor_copy(out=kbar_hi[:], in_=kbar[:]), reads=[B_kbar], writes=[B_kbar])
            DVE.op(lambda: V.tensor_copy(out=kbar_t[:], in_=kbar_hi[:]), reads=[B_kbar], writes=[B_kbar])
            DVE.op(lambda: V.tensor_tensor(out=kbar_t[:], in0=kbar[:], in1=kbar_t[:], op=ALU.subtract),
                   reads=[B_kbar], writes=[B_kbar])
            DVE.op(lambda: V.tensor_copy(out=kbar_lo[:], in_=kbar_t[:]), reads=[B_kbar], writes=[B_kbar])
            if use_sel:
                for tt in range(TPB):
                    yield from attn_select(s, b, tt)
            nkt = 2 * b + 2
            for half in range(2):
                tiles = [(h4, kt) for h4 in range(4) for kt in range(nkt)]

                def emit_qk(h4, kt):
                    hd = half * 4 + h4
                    p, r0 = hd // 2, 64 * (hd % 2)
                    kb = kt // 2
                    sl = state["pt"] % NPT
                    state["pt"] += 1
                    sb_i = sl % 2
                    B_s = B_pA[sb_i]
                    lastk = (kt == nkt - 1)
                    q0 = 128 if lastk else 0
                    sreg = pA[sb_i][:, q0:256]
                    selm = use_sel and kb < ob

                    def qk():
                        ins = TE.matmul(sreg, lhsT=akT[:, p, kt * 128:(kt + 1) * 128],
                                        rhs=aqT[:, hd, q0:256], start=True, stop=not (selm or kb == ob))
                        if kb == ob:
                            ins = TE.matmul(pA[sb_i][:, q0:q0 + 128], lhsT=ident_bf, rhs=C("CAUS", bf=True),
                                            start=False, stop=True)
                        elif selm:
                            ins = TE.matmul(sreg, lhsT=cbf[:, IND_O + kb * 128:IND_O + (kb + 1) * 128],
                                            rhs=selT[:, hd, :], start=False, stop=True)
                        return ins
                    PE.op(qk, reads=[B_ak[p][kb], B_aq[p]] + ([B_selT] if selm else []), writes=[B_s])
                    dref = (2 * b + 1) - kt
                    if hd == 0 and not lastk:
                        ACT.op(lambda: A.activation(out=PTs[:, sl, 0:128], in_=pA[sb_i][:, 0:128], func=AF.Exp, scale=0.125,
                                                    bias=C("ALIBI", sub=(hd * 16 + dref - 1, 1))),
                               reads=[B_s], writes=[B_PT[sl]])
                        ACT.op(lambda: A.activation(out=PTs[:, sl, 128:256], in_=pA[sb_i][:, 128:256], func=AF.Exp,
                                                    scale=0.125, bias=C("ALIBI", sub=(hd * 16 + dref, 1))),
                               reads=[B_s], writes=[B_PT[sl]])
                    else:
                        ACT.op(lambda: A.activation(out=PTs[:, sl, q0:256], in_=sreg, func=AF.Exp, scale=0.125,
                                                    bias=C("ALIBI", sub=(hd * 16 + dref, 1))),
                               reads=[B_s], writes=[B_PT[sl]])
                    return sl

                def emit_pv(h4, kt, sl):
                    hd = half * 4 + h4

                    def pv():
                        ins = None
                        for tq in range(TPB):
                            gt = 2 * b + tq
                            if kt > gt:
                                continue
                            ins = TE.matmul(pO[tq][:, h4 * 128:h4 * 128 + 65], lhsT=PTs[:, sl, tq * 128:(tq + 1) * 128],
                                            rhs=vtm[:, kt, hd, :], start=(kt == 0), stop=(kt == gt))
                        return ins
                    PE.op(pv, reads=[B_PT[sl], B_v[kt]], writes=B_pO)

                pend = []
                for (h4, kt) in tiles:
                    yield
                    sl = emit_qk(h4, kt)
                    pend.append((h4, kt, sl))
                    if len(pend) > 1:
                        emit_pv(*pend.pop(0))
                while pend:
                    emit_pv(*pend.pop(0))
                for tq in range(TPB):
                    DVE.op(lambda tq=tq: V.reciprocal(out=rden[:], in_=pOh[tq][:, :, 64]),
                           reads=[B_pO[tq]], writes=[B_att])
                    DVE.op(lambda tq=tq: V.tensor_tensor(out=att_t[:], in0=pOh[tq][:, :, 0:64],
                                                         in1=bc(rden[:].unsqueeze(2), [128, 4, 64]),
                                                         op=ALU.mult),
                           reads=[B_pO[tq], B_att], writes=[B_att])
                    POOL.op(lambda half=half, tq=tq: G.tensor_tensor(
                        out=mixed[:, tq, half * 256:(half + 1) * 256], in0=att_t[:].rearrange("p h d -> p (h d)"),
                        in1=siluz[:, tq, half * 256:(half + 1) * 256], op=ALU.mult),
                        reads=[B_att, B_sz[tq]], writes=[B_mx[tq][0]])
            if s == 0 and b in (0, 4):
                dump(f"attn{2 * b}", mixed[:, 0, 0:512], [B_mx[0][0]])

        def attn_select(s, b, tt):
            ob = b
            qs = slice(tt * 128, (tt + 1) * 128)

            def gate():
                for hd in range(8):
                    p, r0 = hd // 2, 64 * (hd % 2)
                    o = pA[hd % 2][:, 256 + p * 8:264 + p * 8]
                    TE.matmul(o, lhsT=aqT[r0:r0 + 64, hd, qs], rhs=kbar_hi[r0:r0 + 64, p, :], start=True, stop=False)
                    ins = TE.matmul(o, lhsT=aqT[r0:r0 + 64, hd, qs], rhs=kbar_lo[r0:r0 + 64, p, :], start=False,
                                    stop=True)
                return ins
            PE.op(gate, reads=B_aq + [B_kbar], writes=B_pA)
            obm = C("OBM", sub=((ob - 4) * 64, 64)).rearrange("p (a two j) -> p a two j", two=2, j=8)
            gmv = gm[:].rearrange("p (a two) j -> p a two j", two=2)
            DVE.op(lambda: V.tensor_tensor(out=gmv[:, :, 0, :],
                                           in0=pA[0][:, 256:288].rearrange("p (a j) -> p a j", a=4),
                                           in1=obm[:, :, 0, :], op=ALU.add), reads=[B_pA[0]], writes=[B_gm])
            DVE.op(lambda: V.tensor_tensor(out=gmv[:, :, 1, :],
                                           in0=pA[1][:, 256:288].rearrange("p (a j) -> p a j", a=4),
                                           in1=obm[:, :, 1, :], op=ALU.add), reads=[B_pA[1]], writes=[B_gm])
            for hd in range(8):
                DVE.op(lambda hd=hd: V.max(out=top8[:, hd, :], in_=gm[:, hd, :]), reads=[B_gm], writes=[B_gm])
            DVE.op(lambda: V.tensor_tensor(out=gm[:], in0=gm[:], in1=bc(top8[:, :, 2:3], [128, 8, 8]),
                                           op=ALU.is_ge), reads=[B_gm], writes=[B_gm])
            DVE.op(lambda: V.tensor_scalar(out=selb[:, :, 0:8], in0=gm[:], scalar1=-NEGA, scalar2=NEGA, op0=ALU.mult,
                                           op1=ALU.add), reads=[B_gm], writes=[B_gm])
            DVE.op(lambda: V.tensor_copy(out=selb[:, :, 64:72], in_=selb[:, :, 0:8]), reads=[B_gm], writes=[B_gm])

            def trsel():
                for hd in range(8):
                    ins = TE.transpose(out=pT[0:72, hd * 128:(hd + 1) * 128], in_=selb[:, hd, :], identity=ident_bf)
                return ins
            yield
            PE.op(trsel, reads=[B_gm], writes=[B_pT])
            DVE.op(lambda: V.tensor_copy(out=selT[0:72, :, qs], in_=pT[0:72, :].rearrange("p (h q) -> p h q", h=8)),
                   reads=[B_pT], writes=[B_selT])
            if s == 0 and b == 4 and tt == 0:
                dump("selb", gm[:].rearrange("p h j -> p (h j)"), [B_gm])

        def out_block(s, b):
            for tt in range(TPB):
                gt = b * TPB + tt
                sl = state["xo"] % 2
                state["xo"] += 1
                dma(SP, ch_xo[sl], xo[sl][:], x[s, gt * 128:(gt + 1) * 128, :], writes=[B_xo[sl]])

                def tr(tt=tt):
                    for kc in range(8):
                        ins = TE.transpose(out=pT[:, kc * 128:(kc + 1) * 128], in_=mixed[:, tt, kc * 128:(kc + 1) * 128],
                                           identity=ident_bf)
                    return ins
                yield
                PE.op(tr, reads=B_mx[tt], writes=[B_pT])
                ACT.op(lambda: A.copy(out=mixT[:, 0:4, :], in_=pTh[:, 0:4, :]), reads=[B_pT], writes=[B_mixT])
                DVE.op(lambda: V.tensor_copy(out=mixT[:, 4:8, :], in_=pTh[:, 4:8, :]), reads=[B_pT, B_mixT],
                       writes=[B_mixT])

                def op_():
                    for hf in range(2):
                        for kc in range(8):
                            ins = TE.matmul(pA[hf][:, :], lhsT=mixT[:, kc, :], rhs=w_out_bf[:, kc, hf * 512:(hf + 1) * 512],
                                            start=(kc == 0), stop=(kc == 7))
                    return ins
                yield
                PE.op(op_, reads=[B_mixT], writes=B_pA)
                for hf in range(2):
                    ACT.op(lambda hf=hf: A.activation(out=junk[:, 0:512], in_=pA[hf][:, :], func=AF.Square,
                                                      accum_out=small[:, 8 + hf:9 + hf]),
                           reads=[B_pA[hf]], writes=[B_junk, B_small])
                DVE.op(lambda: V.tensor_tensor(out=small[:, 10:11], in0=small[:, 8:9], in1=small[:, 9:10], op=ALU.add),
                       reads=[B_small], writes=[B_small])
                ACT.op(lambda: A.activation(out=small[:, 11:12], in_=small[:, 10:11], func=AF.Ln, scale=1.0 / D,
                                            bias=EPS), reads=[B_small], writes=[B_small])
                ACT.op(lambda: A.activation(out=small[:, 12:13], in_=small[:, 11:12], func=AF.Exp, scale=-0.5),
                       reads=[B_small], writes=[B_small])
                for hf in range(2):
                    DVE.op(lambda hf=hf: V.scalar_tensor_tensor(
                        out=tmpf[hf][:], in0=pA[hf][:, :], scalar=small[:, 12:13], op0=ALU.mult,
                        in1=wpost_bc[:, hf * 512:(hf + 1) * 512], op1=ALU.mult),
                        reads=[B_pA[hf], B_small], writes=[B_tmpf[hf]])
                    POOL.op(lambda hf=hf, sl=sl: G.tensor_tensor(out=xo[sl][:, hf * 512:(hf + 1) * 512],
                                                                 in0=xo[sl][:, hf * 512:(hf + 1) * 512],
                                                                 in1=tmpf[hf][:], op=ALU.add),
                            reads=[B_tmpf[hf], B_xo[sl]], writes=[B_xo[sl]])
                dma(SP, ch_st[sl], y[s, gt * 128:(gt + 1) * 128, :], xo[sl][:], reads=[B_xo[sl]])

        def run_streams(gens):
            gens = list(gens)
            while gens:
                for gq in list(gens):
                    try:
                        next(gq)
                    except StopIteration:
                        gens.remove(gq)

        blocks = [(s, b) for s in range(nseq) for b in range(nblk)]
        prev = None
        for (s, b) in blocks:
            if os.environ.get("K_STOP") == "w":
                break
            state["uid"] += 1
            with contextlib.ExitStack() as ea:
                gens = [inproj_block(s, b, ea)]
                if prev is not None and "out" in phases:
                    gens.append(out_block(*prev))
                run_streams(gens)
                barrier()
            with contextlib.ExitStack() as eb:
                gens = []
                if "gdn" in phases:
                    gens.append(gdn_block(s, b, eb))
                if "attn" in phases:
                    gens.append(attn_block(s, b))
                run_streams(gens)
                if "gdn" in phases:
                    barrier()
            prev = (s, b)
        if prev is not None and "out" in phases:
            run_streams([out_block(*prev)])
        for ch in ch_st + dbg_chans:
            if ch.count:
                nc.sync.wait_ge(ch.sem, ch.count)
    return nc


def make_params(norm_pre_w, conv_w, a_log, dt_bias, gdn_norm_w, norm_post_w):
    params = np.zeros((128, PW), np.float32)
    params[:, 0:8] = np.asarray(norm_pre_w)[0].reshape(8, 128).T
    cw = np.asarray(conv_w)[0]
    params[:, 8:56] = cw.reshape(4, 12, 128).transpose(2, 0, 1).reshape(128, 48)
    params[:, 56:60] = np.asarray(a_log)[0][None, :]
    params[:, 60:64] = np.asarray(dt_bias)[0][None, :]
    params[:, 64:576] = np.tile(np.asarray(gdn_norm_w)[0], 4)[None, :]
    params[:, 576:1600] = np.asarray(norm_post_w)[0][None, :]
    return params


def kernel(x, norm_pre_w, w_in, conv_w, a_log, dt_bias, gdn_norm_w, w_out, norm_post_w):
    x = np.ascontiguousarray(np.asarray(x, dtype=np.float32))
    consts = make_consts()
    params = make_params(norm_pre_w, conv_w, a_log, dt_bias, gdn_norm_w, norm_post_w)
    w_in0 = np.ascontiguousarray(np.asarray(w_in, dtype=np.float32)[0])
    w_out0 = np.ascontiguousarray(np.asarray(w_out, dtype=np.float32)[0])
    nc = build()
    in_maps = []
    for c in range(NCORES):
        in_maps.append({"x": x[c * NSEQ:(c + 1) * NSEQ], "w_in": w_in0, "w_out": w_out0, "consts": consts,
                        "params": params})
    res = run_bass_kernel_spmd(nc, in_maps, core_ids=list(range(NCORES)))
    return np.concatenate([r["y"] for r in res.results], axis=0)
```

```python
import contextlib
import math
import os

import numpy as np

import concourse.bass as bass
import concourse.mybir as mybir
from concourse.bass_utils import run_bass_kernel_spmd

F32 = mybir.dt.float32
BF16 = mybir.dt.bfloat16
AF = mybir.ActivationFunctionType
ALU = mybir.AluOpType

T = 2048
D = 1024
NCOL = 4104
NSEQ = 4
NCORES = 8
EPS = 1e-6
NEGA = -240000.0
NEGD = -30000.0


class Tok:
    __slots__ = ("sem", "val", "eng")

    def __init__(self, sem, val, eng):
        self.sem, self.val, self.eng = sem, val, eng


class Buf:
    __slots__ = ("name", "w", "r", "excl")

    def __init__(self, name, excl=False):
        self.name = name
        self.w = []
        self.r = []
        self.excl = excl


class Eng:
    def __init__(self, e, sem, name, is_pe=False):
        self.e, self.sem, self.name, self.is_pe = e, sem, name, is_pe
        self.count = 0
        self.waited = {}

    def _wait(self, tok):
        if tok.eng is self and self.is_pe:
            return
        k = id(tok.sem)
        if self.waited.get(k, 0) >= tok.val:
            return
        self.e.wait_ge(tok.sem, tok.val)
        self.waited[k] = tok.val

    def deps(self, reads, writes):
        for b in reads:
            for t in b.w:
                self._wait(t)
            if b.excl:
                for t in b.r:
                    self._wait(t)
        for b in writes:
            for t in b.w:
                self._wait(t)
            for t in b.r:
                self._wait(t)

    def commit(self, tok, reads, writes):
        for b in writes:
            b.w = [tok]
            b.r = []
        for b in reads:
            if b in writes:
                continue
            if b.excl:
                b.w = [tok]
                b.r = []
                continue
            b.r = [t for t in b.r if t.sem is not tok.sem] + [tok]

    def op(self, fn, reads=(), writes=()):
        self.deps(reads, writes)
        ins = fn()
        self.count += 1
        ins.then_inc(self.sem, 1)
        tok = Tok(self.sem, self.count, self)
        self.commit(tok, reads, writes)
        return tok


class Chan:
    def __init__(self, sem):
        self.sem = sem
        self.count = 0


def dma(q, chan, out, in_, reads=(), writes=()):
    q.deps(reads, writes)
    chan.count += 16
    q.e.dma_start(out=out, in_=in_).then_inc(chan.sem, 16)
    tok = Tok(chan.sem, chan.count, None)
    q.commit(tok, reads, writes)
    return tok


class _Cols:
    def __init__(self):
        self.n = 0
        self.d = {}

    def add(self, name, w):
        self.d[name] = (self.n, w)
        self.n += w
        return self.d[name]


def _const_layout():
    c = _Cols()
    c.add("IDENT", 128)
    c.add("TRI", 128)
    c.add("ONES", 128)
    c.add("ALIBI", 8 * 16)
    c.add("OBM", 4 * 64)
    c.add("MASK_LS", 128)
    c.add("MASK_US", 128)
    c.add("MASK_UI", 128)
    c.add("CAUS", 128)
    c.add("SEL", 12 * 128)
    c.add("IND", 8 * 128)
    return c


CL = _const_layout()
SEL_A, SEL_AP, SEL_NB = 0, 1, 2


def make_consts():
    c = np.zeros((128, CL.n), np.float32)
    p = np.arange(128)[:, None]
    f = np.arange(128)[None, :]

    def put(name, arr):
        o, w = CL.d[name]
        c[:, o:o + w] = arr

    put("IDENT", (p == f).astype(np.float32))
    put("TRI", (p <= f).astype(np.float32))
    put("ONES", np.ones((128, 128), np.float32))
    put("MASK_LS", np.where(p > f, 0.0, NEGD))
    put("MASK_US", np.where(f > p, 0.0, NEGD))
    put("MASK_UI", np.where(f >= p, 0.0, NEGD))
    put("CAUS", np.where(f >= p, 0.0, NEGA))
    slopes = (2.0 ** (-8.0 / 8)) ** np.arange(1, 9)
    al = np.zeros((128, 8, 16), np.float32)
    for h in range(8):
        for d in range(16):
            al[:, h, d] = -slopes[h] * (128 * d + 127 - np.arange(128))
    put("ALIBI", al.reshape(128, 128))
    sel = np.zeros((128, 12, 128), np.float32)
    for h in range(4):
        for sp in range(3):
            sel[sp * 12 + 0 * 4 + h, SEL_A * 4 + h, :] = 1.0
            sel[sp * 12 + 2 * 4 + h, SEL_AP * 4 + h, :] = 1.0
            sel[sp * 12 + 1 * 4 + h, SEL_NB * 4 + h, :] = -1.0
    put("SEL", sel.reshape(128, 12 * 128))
    ind = np.zeros((128, 8, 128), np.float32)
    for kb in range(8):
        ind[kb, kb, :] = 1.0
        ind[64 + kb, kb, :] = 1.0
    put("IND", ind.reshape(128, 8 * 128))
    obm = np.zeros((128, 4, 8, 8), np.float32)
    for ob in range(4, 8):
        obm[:, ob - 4, :, ob:] = -1e30
    put("OBM", obm.reshape(128, 256))
    return c


PW = 8 + 48 + 4 + 4 + 512 + 1024
LN_QS = math.log(128.0 ** -0.5)
BT = 256
TPB = 2
NB = T // BT
CF32 = 768


class NS:
    pass


def build(nseq=NSEQ, nblk=NB, dbg=None, phases=("gdn", "attn", "out")):
    nc = bass.Bass("TRN2", target_bir_lowering=False)
    dbg = dbg or {}
    x = nc.dram_tensor("x", [nseq, T, D], F32, kind="ExternalInput").ap()
    w_in = nc.dram_tensor("w_in", [D, NCOL], F32, kind="ExternalInput").ap()
    w_out = nc.dram_tensor("w_out", [D, D], F32, kind="ExternalInput").ap()
    consts = nc.dram_tensor("consts", [128, CL.n], F32, kind="ExternalInput").ap()
    params = nc.dram_tensor("params", [128, PW], F32, kind="ExternalInput").ap()
    y = nc.dram_tensor("y", [nseq, T, D], F32, kind="ExternalOutput").ap()
    dbg_aps = {}
    for name, shape in dbg.items():
        dbg_aps[name] = nc.dram_tensor("dbg_" + name, list(shape), F32, kind="ExternalOutput").ap()
    AX = mybir.AxisListType.X

    with contextlib.ExitStack() as es:
        def sb(name, shape, dt=F32, st=None):
            return (st or es).enter_context(nc.sbuf_tensor(name, list(shape), dt))

        def ps(name, shape, dt=F32):
            return es.enter_context(nc.psum_tensor(name, list(shape), dt))

        def sem(name):
            return es.enter_context(nc.semaphore(name))

        PE = Eng(nc.tensor, sem("s_pe"), "pe", is_pe=True)
        ACT = Eng(nc.scalar, sem("s_act"), "act")
        DVE = Eng(nc.vector, sem("s_dve"), "dve")
        POOL = Eng(nc.gpsimd, sem("s_pool"), "pool")
        SP = Eng(nc.sync, sem("s_sp"), "sp")
        V, G, A, TE = nc.vector, nc.gpsimd, nc.scalar, nc.tensor

        def chan(name):
            return Chan(sem(name))

        pA = [ps(f"pA{i}", [128, 512]) for i in range(2)]
        B_pA = [Buf("pA0", True), Buf("pA1", True)]
        pB = [ps(f"pB{i}", [128, 512]) for i in range(4)]
        B_pB = [Buf(f"pB{i}", True) for i in range(4)]
        pT = ps("pT", [128, 1024], BF16)
        B_pT = Buf("pT", True)
        pS = ps("pS", [128, 512])
        B_pS = Buf("pS", True)
        scr = sb("scr", [128, 8])
        scrb = sb("scrb", [128, 8], BF16)
        dbg_chans = []

        B_scr = Buf("scr")

        def barrier():
            b = B_scr
            PE.op(lambda: TE.matmul(pS[0:8, 510:512], lhsT=scrb[0:8, 0:8], rhs=scrb[0:8, 0:2], start=True, stop=True),
                  reads=[b], writes=[B_pS])
            ACT.op(lambda: A.copy(out=scr[0:1, 0:1], in_=scr[0:1, 0:1]), reads=[b, B_pS], writes=[b])
            DVE.op(lambda: V.tensor_copy(out=scr[0:1, 1:2], in_=scr[0:1, 1:2]), reads=[b], writes=[b])
            POOL.op(lambda: G.tensor_copy(out=scr[0:1, 2:3], in_=scr[0:1, 2:3]), reads=[b], writes=[b])
            for e in (PE, ACT, DVE, SP):
                e.deps([b], [])
            for ch in dbg_chans:
                nc.sync.wait_ge(ch.sem, ch.count)

        POOL.op(lambda: G.memset(scrb[:], 0.0), writes=[B_scr])
        POOL.op(lambda: G.memset(scr[:], 0.0), writes=[B_scr])

        cf = sb("cf", [128, CF32])
        cbf = sb("cbf", [128, CL.n], BF16)
        prm = sb("prm", [128, PW])
        w_in_bf = sb("w_in_bf", [128, 8, NCOL], BF16)
        w_out_bf = sb("w_out_bf", [128, 8, D], BF16)
        HALF = NCOL // 2
        B_c = Buf("consts")
        ch_c = chan("c_c")
        dma(SP, ch_c, cf[:], consts[:, 0:CF32], writes=[B_c])
        dma(SP, ch_c, prm[:], params[:, :], writes=[B_c])
        B_c.w = [B_c.w[-1]]
        npw = prm[:, 0:8]
        convw = prm[:, 8:56]
        alog_bc = prm[:, 56:60]
        dtb_bc = prm[:, 60:64]
        gnw_bc = prm[:, 64:576]
        wpost_bc = prm[:, 576:1600]
        with contextlib.ExitStack() as es2:
            stg = [sb(f"stg{i}", [128, HALF], st=es2) for i in range(2)]
            B_stg = [Buf("stg0"), Buf("stg1")]
            ch_stg = [chan("c_stg0"), chan("c_stg1")]
            i = 0
            hh = CL.n // 2
            for hf in range(2):
                sl = i % 2
                dma(SP, ch_stg[sl], stg[sl][:, 0:hh], consts[:, hf * hh:(hf + 1) * hh], writes=[B_stg[sl]])
                DVE.op(lambda sl=sl, hf=hf: V.tensor_copy(out=cbf[:, hf * hh:(hf + 1) * hh], in_=stg[sl][:, 0:hh]),
                       reads=[B_stg[sl]], writes=[Buf("t")])
                i += 1
            for kc in range(8):
                for hf in range(2):
                    sl = i % 2
                    dma(SP, ch_stg[sl], stg[sl][:], w_in[kc * 128:(kc + 1) * 128, hf * HALF:(hf + 1) * HALF],
                        writes=[B_stg[sl]])
                    if sl == 0:
                        ACT.op(lambda sl=sl, kc=kc, hf=hf: A.activation(
                            out=w_in_bf[:, kc, hf * HALF:(hf + 1) * HALF], in_=stg[sl][:], func=AF.Copy,
                            scale=npw[:, kc:kc + 1]), reads=[B_stg[sl], B_c], writes=[Buf("t")])
                    else:
                        DVE.op(lambda sl=sl, kc=kc, hf=hf: V.tensor_scalar(
                            out=w_in_bf[:, kc, hf * HALF:(hf + 1) * HALF], in0=stg[sl][:],
                            scalar1=npw[:, kc:kc + 1], scalar2=None, op0=ALU.mult),
                            reads=[B_stg[sl], B_c], writes=[Buf("t")])
                    i += 1
            for kc in range(8):
                sl = i % 2
                dma(SP, ch_stg[sl], stg[sl][:, 0:D], w_out[kc * 128:(kc + 1) * 128, :], writes=[B_stg[sl]])
                if sl == 0:
                    ACT.op(lambda sl=sl, kc=kc: A.copy(out=w_out_bf[:, kc, :], in_=stg[sl][:, 0:D]),
                           reads=[B_stg[sl]], writes=[Buf("t")])
                else:
                    DVE.op(lambda sl=sl, kc=kc: V.tensor_copy(out=w_out_bf[:, kc, :], in_=stg[sl][:, 0:D]),
                           reads=[B_stg[sl]], writes=[Buf("t")])
                i += 1
            barrier()

        def C(name, bf=False, rows=128, sub=None):
            o, w = CL.d[name]
            t = cbf if bf else cf
            if not bf:
                assert o + w <= CF32
            if sub is not None:
                so, sw = sub
                return t[0:rows, o + so:o + so + sw]
            return t[0:rows, o:o + w]

        IND_O = CL.d["IND"][0]
        ident_bf = C("IDENT", bf=True)
        ident_f = C("IDENT")

        xo = [sb(f"xo{i}", [128, D]) for i in range(2)]
        B_xo = [Buf(f"xo{i}") for i in range(2)]
        ch_xo = [chan(f"c_xo{i}") for i in range(2)]
        ch_st = [chan(f"c_st{i}") for i in range(2)]
        ch_xs2 = [chan("c_xs0"), chan("c_xs1")]
        aqT = sb("aqT", [128, 8, BT], BF16)
        B_aq = [Buf(f"aq{p}") for p in range(4)]
        akT = sb("akT", [128, 4, T], BF16)
        B_ak = [[Buf(f"ak{p}_{b}") for b in range(NB)] for p in range(4)]
        vtm = sb("vtm", [128, 16, 8, 65], BF16)
        B_v = [Buf(f"v{t}") for t in range(16)]
        siluz = sb("siluz", [128, TPB, 512], BF16)
        B_sz = [Buf(f"sz{t}") for t in range(TPB)]
        zw = sb("zw", [128, TPB, 512], BF16)
        B_zw = [Buf(f"zw{t}") for t in range(TPB)]
        sraw = sb("sraw", [128, TPB, 8])
        B_sraw = Buf("sraw")
        dT = [sb(f"dT{k}", [128, 4, BT], BF16) for k in range(3)]
        B_dT = [[Buf(f"dT{k}_{h}") for h in range(4)] for k in range(3)]
        halo = sb("halo", [128, 12, 4], BF16)
        B_halo = [Buf(f"halo{c}") for c in range(12)]
        mixed = sb("mixed", [128, TPB, D], BF16)
        B_mx = [[Buf(f"mx{t}_{i}") for i in range(2)] for t in range(TPB)]
        mixT = sb("mixT", [128, 8, 128], BF16)
        B_mixT = Buf("mixT")
        tmpf = [sb(f"tmpf{i}", [128, 512]) for i in range(2)]
        B_tmpf = [Buf("tmpf0"), Buf("tmpf1")]
        small = sb("small", [128, 64])
        B_small = Buf("small")
        junk = sb("junk", [128, D], BF16)
        B_junk = Buf("junk")
        nea = sb("nea", [128, 4])
        Sst = sb("Sst", [128, 4, 128])
        Sbf = sb("Sbf", [128, 4, 128], BF16)
        B_S, B_Sbf = Buf("S"), Buf("Sbf")
        kbar = sb("kbar", [128, 4, 8])
        kbar_hi = sb("kbar_hi", [128, 4, 8], BF16)
        kbar_lo = sb("kbar_lo", [128, 4, 8], BF16)
        kbar_t = sb("kbar_t", [128, 4, 8])
        B_kbar = Buf("kbar")
        gm = sb("gm", [128, 8, 8])
        top8 = sb("top8", [128, 8, 8])
        selb = sb("selb", [128, 8, 72], BF16)
        B_gm = Buf("gm")
        selT = sb("selT", [128, 8, BT], BF16)
        B_selT = Buf("selT")
        NPT = 4
        PTs = sb("PTs", [128, NPT, BT], BF16)
        B_PT = [Buf(f"PT{i}") for i in range(NPT)]
        B_pSreg = [[Buf(f"pSreg{i}_{j}") for j in range(4)] for i in range(2)]
        att_t = sb("att_t", [128, 4, 64])
        rden = sb("rden", [128, 4])
        B_att = Buf("att_t")

        pBh = [p[:, :].rearrange("p (h d) -> p h d", h=4) for p in pB]
        pO = [pB[3], pS]
        B_pO = [B_pB[3], B_pS]
        pOh = [p[:, :].rearrange("p (h d) -> p h d", h=4) for p in pO]
        pTh = pT[:, :].rearrange("p (h d) -> p h d", h=8)

        def bc(ap, shape):
            return ap.to_broadcast(list(shape))

        def merge(dst, srcs):
            for sbuf in srcs:
                dst.w = dst.w + sbuf.w
                dst.r = dst.r + sbuf.r

        POOL.op(lambda: G.memset(vtm[:, :, :, 64:65], 1.0), writes=B_v)
        POOL.op(lambda: G.memset(kbar[:], 0.0), writes=[B_kbar])
        POOL.op(lambda: G.memset(aqT[:], 0.0), writes=B_aq)
        POOL.op(lambda: G.memset(selT[:], 0.0), writes=[B_selT])
        POOL.op(lambda: G.memset(selb[:], 0.0), writes=[B_gm])
        ACT.op(lambda: A.activation(out=nea[:], in_=alog_bc, func=AF.Exp), reads=[B_c], writes=[B_small])
        DVE.op(lambda: V.tensor_scalar(out=nea[:], in0=nea[:], scalar1=-1.0, scalar2=None, op0=ALU.mult),
               reads=[B_small], writes=[B_small])
        barrier()

        dbg_toks = []

        def dump(name, ap, bufs):
            if name not in dbg_aps:
                return
            ch = chan("c_dbg_" + name)
            dbg_chans.append(ch)
            dbg_toks.append(dma(POOL, ch, dbg_aps[name], ap, reads=bufs))

        state = {"xo": 0, "pt": 0, "uid": 0}

        def inproj_block(s, b, ea):
            t0 = b * BT
            uid = state["uid"]
            xs2 = [sb(f"xs{i}_{uid}", [128, D], st=ea) for i in range(2)]
            xn = sb(f"xn_{uid}", [128, D], BF16, st=ea)
            hT = sb(f"hT_{uid}", [128, 8, BT], BF16, st=ea)
            pre = sb(f"pre_{uid}", [128, 2, BT + 8], BF16, st=ea)
            cdg = sb(f"cdg_{uid}", [128, 2, 4, 128], BF16, st=ea)
            B_xs2, B_xn = [Buf("xs0"), Buf("xs1")], Buf("xn")
            B_hT = [Buf(f"hT{t}") for t in range(TPB)]
            B_pre = [Buf("pre0"), Buf("pre1")]
            B_cdg = [Buf("cdg0"), Buf("cdg1")]
            for tt in range(TPB):
                gt = b * TPB + tt
                dma(SP, ch_xs2[tt], xs2[tt][:], x[s, gt * 128:(gt + 1) * 128, :], writes=[B_xs2[tt]])
            for tt in range(TPB):
                gt = b * TPB + tt
                xs, B_xs = xs2[tt], B_xs2[tt]
                ACT.op(lambda xs=xs: A.activation(out=junk[:], in_=xs[:], func=AF.Square, accum_out=small[:, 0:1]),
                       reads=[B_xs], writes=[B_junk, B_small])
                ACT.op(lambda: A.activation(out=small[:, 1:2], in_=small[:, 0:1], func=AF.Ln,
                                            scale=1.0 / D, bias=EPS), reads=[B_small], writes=[B_small])
                ACT.op(lambda: A.activation(out=small[:, 2:3], in_=small[:, 1:2], func=AF.Exp, scale=-0.5),
                       reads=[B_small], writes=[B_small])
                DVE.op(lambda xs=xs: V.tensor_scalar(out=xn[:], in0=xs[:], scalar1=small[:, 2:3], scalar2=None,
                                                     op0=ALU.mult), reads=[B_xs, B_small], writes=[B_xn])
                if os.environ.get("K_STOP") == "p1a":
                    return

                def tr():
                    for kc in range(8):
                        ins = TE.transpose(out=pT[:, kc * 128:(kc + 1) * 128], in_=xn[:, kc * 128:(kc + 1) * 128],
                                           identity=ident_bf)
                    return ins
                yield
                PE.op(tr, reads=[B_xn], writes=[B_pT])
                if os.environ.get("K_STOP") == "p1b":
                    return
                ACT.op(lambda tt=tt: A.copy(out=hT[:, 0:4, tt * 128:(tt + 1) * 128], in_=pTh[:, 0:4, :]),
                       reads=[B_pT], writes=[Buf("t")])
                if os.environ.get("K_STOP") == "p1c":
                    return
                DVE.op(lambda tt=tt: V.tensor_copy(out=hT[:, 4:8, tt * 128:(tt + 1) * 128], in_=pTh[:, 4:8, :]),
                       reads=[B_pT], writes=[B_hT[tt]])
                if os.environ.get("K_STOP") == "p1d":
                    return
            if s == 0 and b == 0:
                dump("hT", hT[:, 0, :], B_hT)

            if os.environ.get("K_STOP") == "p1":
                return
            def fm_chunk(col0, bank):
                def f():
                    for kc in range(8):
                        ins = TE.matmul(pA[bank][:, 0:BT], lhsT=w_in_bf[:, kc, col0:col0 + 128], rhs=hT[:, kc, :],
                                        start=(kc == 0), stop=(kc == 7))
                    return ins
                PE.op(f, reads=B_hT, writes=[B_pA[bank]])

            ci_all = 0
            for p in range(4):
                bank = ci_all % 2
                ci_all += 1
                yield
                fm_chunk(0 + p * 128, bank)
                ACT.op(lambda p=p, bank=bank: A.copy(out=aqT[0:64, 2 * p, :], in_=pA[bank][0:64, 0:BT]),
                       reads=[B_pA[bank]], writes=[B_aq[p]])
                ACT.op(lambda p=p, bank=bank: A.copy(out=aqT[64:128, 2 * p + 1, :], in_=pA[bank][64:128, 0:BT]),
                       reads=[B_pA[bank]], writes=[B_aq[p]])
            for p in range(4):
                bank = ci_all % 2
                ci_all += 1
                yield
                fm_chunk(512 + p * 128, bank)
                DVE.op(lambda p=p, bank=bank: V.tensor_copy(out=akT[:, p, t0:t0 + BT], in_=pA[bank][:, 0:BT]),
                       reads=[B_pA[bank]], writes=[B_ak[p][b]])
            for kind in range(3):
                for hd in range(4):
                    ci = kind * 4 + hd
                    bank = ci_all % 2
                    ci_all += 1
                    yield
                    fm_chunk(2048 + ci * 128, bank)
                    sl = ci % 2
                    for tap in range(4):
                        DVE.op(lambda sl=sl, tap=tap, ci=ci: V.tensor_scalar(
                            out=cdg[:, sl, tap, :], in0=ident_f, scalar1=convw[:, tap * 12 + ci:tap * 12 + ci + 1],
                            scalar2=None, op0=ALU.mult), writes=[B_cdg[sl]])
                    if b == 0:
                        POOL.op(lambda sl=sl: G.memset(pre[:, sl, 0:4], 0.0), writes=[B_pre[sl]])
                    else:
                        POOL.op(lambda sl=sl, ci=ci: G.tensor_copy(out=pre[:, sl, 0:4], in_=halo[:, ci, :]),
                                reads=[B_halo[ci]], writes=[B_pre[sl]])
                    ACT.op(lambda sl=sl, bank=bank: A.copy(out=pre[:, sl, 4:4 + BT], in_=pA[bank][:, 0:BT]),
                           reads=[B_pA[bank]], writes=[B_pre[sl]])
                    POOL.op(lambda sl=sl, ci=ci: G.tensor_copy(out=halo[:, ci, :], in_=pre[:, sl, BT:BT + 4]),
                            reads=[B_pre[sl]], writes=[B_halo[ci]])
                    cb = ci % 2

                    def cv(sl=sl, cb=cb):
                        for tap in range(4):
                            ins = TE.matmul(pB[cb][:, 0:BT], lhsT=cdg[:, sl, tap, :],
                                            rhs=pre[:, sl, 1 + tap:1 + tap + BT], start=(tap == 0), stop=(tap == 3))
                        return ins
                    yield
                    PE.op(cv, reads=[B_pre[sl], B_cdg[sl]], writes=[B_pB[cb]])
                    ACT.op(lambda kind=kind, hd=hd, cb=cb: A.activation(out=dT[kind][:, hd, :], in_=pB[cb][:, 0:BT],
                                                                        func=AF.Silu),
                           reads=[B_pB[cb]], writes=[B_dT[kind][hd]])
            if s == 0 and b == 0:
                dump("aqT", aqT[:, 0, :], B_aq)
                dump("dqT", dT[0][:, 0, :], B_dT[0])
                dump("dkT", dT[1][:, 0, :], B_dT[1])

            if os.environ.get("K_STOP") == "p2":
                return
            for tt in range(TPB):
                gt = b * TPB + tt

                tmb = [(pB[1], B_pB[1]), (pB[2], B_pB[2]), (pB[3], B_pB[3])] if tt % 2 == 0 else \
                      [(pA[0], B_pA[0]), (pA[1], B_pA[1]), (pB[0], B_pB[0])]

                def tm(tt=tt, tmb=tmb):
                    for j, col0 in enumerate((1024, 1536, 3584)):
                        for kc in range(8):
                            ins = TE.matmul(tmb[j][0][:, :], lhsT=hT[:, kc, tt * 128:(tt + 1) * 128],
                                            rhs=w_in_bf[:, kc, col0:col0 + 512], start=(kc == 0), stop=(kc == 7))
                    for kc in range(8):
                        ins = TE.matmul(pS[:, 0:8], lhsT=hT[:, kc, tt * 128:(tt + 1) * 128],
                                        rhs=w_in_bf[:, kc, 4096:4104], start=(kc == 0), stop=(kc == 7))
                    return ins
                yield
                PE.op(tm, reads=B_hT, writes=[tmb[0][1], tmb[1][1], tmb[2][1], B_pS])
                ACT.op(lambda gt=gt, tmb=tmb: A.copy(out=vtm[:, gt, :, 0:64],
                                                     in_=tmb[0][0][:, :].rearrange("p (h d) -> p h d", h=8)),
                       reads=[tmb[0][1]], writes=[B_v[gt]])
                ACT.op(lambda tt=tt, tmb=tmb: A.activation(out=siluz[:, tt, :], in_=tmb[1][0][:, :], func=AF.Silu),
                       reads=[tmb[1][1]], writes=[B_sz[tt]])
                fsl = tt % 2
                ACT.op(lambda fsl=fsl, tmb=tmb: A.activation(out=tmpf[fsl][:], in_=tmb[2][0][:, :], func=AF.Silu),
                       reads=[tmb[2][1]], writes=[B_tmpf[fsl]])
                POOL.op(lambda tt=tt, fsl=fsl: G.tensor_tensor(out=zw[:, tt, :], in0=tmpf[fsl][:], in1=gnw_bc,
                                                               op=ALU.mult),
                        reads=[B_tmpf[fsl]], writes=[B_zw[tt]])
                DVE.op(lambda tt=tt: V.tensor_copy(out=sraw[:, tt, :], in_=pS[:, 0:8]), reads=[B_pS], writes=[B_sraw])
            if s == 0 and b == 0:
                dump("sraw", sraw[:, 0, :], [B_sraw])
                dump("vtm", vtm[:, 0, 0, :], B_v)
                dump("zw", zw[:, 0, :], B_zw)

        def gdn_block(s, b, eb):
            uid = state["uid"]
            g = NS()

            def gb(name, shape, dt=F32):
                return sb(f"{name}_{uid}", shape, dt, st=eb)
            g.st_lnr = gb("st_lnr", [128, TPB, 8])
            g.st_lnbn = gb("st_lnbn", [128, TPB, 4])
            g.st_g = gb("st_g", [128, TPB, 4])
            g.st_e = gb("st_e", [128, TPB, 8])
            g.ST = gb("ST", [128, TPB, 12])
            g.st_c = gb("st_c", [128, TPB, 4])
            g.ex_a = gb("ex_a", [128, TPB, 4])
            g.ex_c = gb("ex_c", [128, TPB, 4])
            g.ex_b = gb("ex_b", [128, TPB, 4])
            g.ex_gl = gb("ex_gl", [128, TPB, 4])
            g.STs = gb("STs", [128, TPB, 36], BF16)
            g.STr = gb("STr", [128, TPB, 12])
            g.STh = gb("STh", [128, TPB, 12])
            g.STT = gb("STT", [128, TPB, 128], BF16)
            g.sq = [gb(f"sq{i}", [128, 4, BT], BF16) for i in range(2)]
            g.Eb = [gb(f"Eb{i}", [128, 4, 128], BF16) for i in range(2)]
            g.Xb = [gb(f"Xb{i}", [128, 4, 128], BF16) for i in range(2)]
            g.Yb = [gb(f"Yb{i}", [128, 4, 128], BF16) for i in range(2)]
            g.Pb = [gb(f"Pb{i}", [128, 4, 128], BF16) for i in range(2)]
            g.intraT = gb("intraT", [128, 4, 128], BF16)
            g.kbg = gb("kbg", [128, 4, 128], BF16)
            g.kdec = gb("kdec", [128, 4, 128], BF16)
            g.vb = gb("vb", [128, 4, 128], BF16)
            g.qdT = gb("qdT", [128, 4, 128], BF16)
            g.u_sb = gb("u_sb", [128, 4, 128])
            g.wT = gb("wT", [128, 4, 128], BF16)
            g.vnew = gb("vnew", [128, 4, 128], BF16)
            g.osq = gb("osq", [128, 4, 128])
            g.ost = gb("ost", [128, 16])
            g.B_st, g.B_STT = Buf("stats"), Buf("STT")
            g.B_sq = [Buf("sqq"), Buf("sqk")]
            g.B_Eb = [Buf("Eb0"), Buf("Eb1")]
            g.B_X = [Buf("X0"), Buf("X1")]
            g.B_Y = [Buf("Y0"), Buf("Y1")]
            g.B_P = [Buf("P0"), Buf("P1")]
            g.B_iT, g.B_kbg, g.B_kdec, g.B_vb, g.B_qdT = Buf("iT"), Buf("kbg"), Buf("kdec"), Buf("vb"), Buf("qdT")
            g.B_u, g.B_wT, g.B_vn, g.B_osq, g.B_ost = Buf("u"), Buf("wT"), Buf("vn"), Buf("osq"), Buf("ost")
            B_st = g.B_st
            ST, st_lnr, st_lnbn, st_g, st_e, st_c = g.ST, g.st_lnr, g.st_lnbn, g.st_g, g.st_e, g.st_c
            STs, STr, STh, STT = g.STs, g.STr, g.STh, g.STT
            if b == 0:
                DVE.op(lambda: V.memset(Sst[:], 0.0), writes=[B_S])
                POOL.op(lambda: G.memset(Sbf[:], 0.0), writes=[B_Sbf])
            for k in range(2):
                POOL.op(lambda k=k: G.tensor_tensor(out=g.sq[k][:], in0=dT[k][:], in1=dT[k][:], op=ALU.mult),
                        reads=B_dT[k], writes=[g.B_sq[k]])
            ones_col = C("ONES", bf=True, sub=(0, 1))

            def ssq():
                for tt in range(TPB):
                    for k in range(2):
                        for hd in range(4):
                            c0 = 64 + tt * 8 + k * 4 + hd
                            ins = TE.matmul(pB[0][:, c0:c0 + 1], lhsT=g.sq[k][:, hd, tt * 128:(tt + 1) * 128],
                                            rhs=ones_col, start=True, stop=True)
                return ins
            yield
            PE.op(ssq, reads=g.B_sq, writes=[B_pB[0]])
            pS_ssq = pB[0][:, 64:64 + 8 * TPB].rearrange("p (t k) -> p t k", t=TPB)
            ACT.op(lambda: A.activation(out=st_lnr[:], in_=pS_ssq, func=AF.Ln, bias=EPS),
                   reads=[B_pB[0]], writes=[B_st])
            ACT.op(lambda: A.activation(out=st_e[:, :, 0:4], in_=sraw[:, :, 0:4], func=AF.Exp, scale=-1.0),
                   reads=[B_sraw, B_st], writes=[B_st])
            ACT.op(lambda: A.activation(out=st_lnbn[:], in_=st_e[:, :, 0:4], func=AF.Ln, bias=1.0),
                   reads=[B_st], writes=[B_st])
            DVE.op(lambda: V.tensor_tensor(out=st_e[:, :, 4:8], in0=sraw[:, :, 4:8],
                                           in1=bc(dtb_bc.unsqueeze(1), [128, TPB, 4]), op=ALU.add),
                   reads=[B_sraw, B_st], writes=[B_st])
            ACT.op(lambda: A.activation(out=st_e[:, :, 4:8], in_=st_e[:, :, 4:8], func=AF.Exp),
                   reads=[B_st], writes=[B_st])
            ACT.op(lambda: A.activation(out=st_e[:, :, 4:8], in_=st_e[:, :, 4:8], func=AF.Ln, bias=1.0),
                   reads=[B_st], writes=[B_st])
            DVE.op(lambda: V.tensor_tensor(out=st_g[:], in0=st_e[:, :, 4:8], in1=bc(nea[:].unsqueeze(1), [128, TPB, 4]),
                                           op=ALU.mult), reads=[B_st], writes=[B_st])

            def gcm():
                for tt in range(TPB):
                    TE.matmul(pB[0][:, 96 + tt * 4:100 + tt * 4], lhsT=C("TRI"), rhs=st_g[:, tt, :], start=True, stop=True)
                    ins = TE.matmul(pB[0][:, 112 + tt * 4:116 + tt * 4], lhsT=C("ONES"), rhs=st_g[:, tt, :],
                                    start=True, stop=True)
                return ins
            yield
            PE.op(gcm, reads=[B_st], writes=[B_pB[0]])
            gc = pB[0][:, 96:96 + 4 * TPB].rearrange("p (t k) -> p t k", t=TPB)
            gl = pB[0][:, 112:112 + 4 * TPB].rearrange("p (t k) -> p t k", t=TPB)
            lnrq = st_lnr[:, :, 0:4]
            lnrk = st_lnr[:, :, 4:8]
            DVE.op(lambda: V.scalar_tensor_tensor(out=ST[:, :, 4:8], in0=lnrk, scalar=0.5, op0=ALU.mult, in1=gc,
                                                  op1=ALU.add), reads=[B_st, B_pB[0]], writes=[B_st])
            DVE.op(lambda: V.scalar_tensor_tensor(out=ST[:, :, 0:4], in0=lnrk, scalar=-0.5, op0=ALU.mult, in1=gc,
                                                  op1=ALU.add), reads=[B_st, B_pB[0]], writes=[B_st])
            DVE.op(lambda: V.scalar_tensor_tensor(out=st_c[:], in0=ST[:, :, 4:8], scalar=-1.0, op0=ALU.mult, in1=gl,
                                                  op1=ALU.add), reads=[B_st, B_pB[0]], writes=[B_st])
            DVE.op(lambda: V.tensor_tensor(out=ST[:, :, 0:4], in0=ST[:, :, 0:4], in1=st_lnbn[:], op=ALU.subtract),
                   reads=[B_st], writes=[B_st])
            DVE.op(lambda: V.scalar_tensor_tensor(out=ST[:, :, 8:12], in0=lnrq, scalar=-0.5, op0=ALU.mult, in1=gc,
                                                  op1=ALU.add), reads=[B_st, B_pB[0]], writes=[B_st])
            DVE.op(lambda: V.tensor_scalar(out=ST[:, :, 8:12], in0=ST[:, :, 8:12], scalar1=LN_QS, scalar2=None,
                                           op0=ALU.add), reads=[B_st], writes=[B_st])
            ACT.op(lambda: A.activation(out=g.ex_a[:], in_=ST[:, :, 0:4], func=AF.Exp), reads=[B_st], writes=[B_st])
            ACT.op(lambda: A.activation(out=g.ex_c[:], in_=st_c[:], func=AF.Exp), reads=[B_st], writes=[B_st])
            ACT.op(lambda: A.activation(out=g.ex_b[:], in_=st_lnbn[:], func=AF.Exp, scale=-1.0),
                   reads=[B_st], writes=[B_st])
            ACT.op(lambda: A.activation(out=g.ex_gl[:], in_=gl, func=AF.Exp), reads=[B_st, B_pB[0]], writes=[B_st])
            DVE.op(lambda: V.tensor_copy(out=STs[:, :, 0:12], in_=ST[:]), reads=[B_st], writes=[B_st])
            DVE.op(lambda: V.tensor_copy(out=STh[:], in_=STs[:, :, 0:12]), reads=[B_st], writes=[B_st])
            DVE.op(lambda: V.tensor_tensor(out=STr[:], in0=ST[:], in1=STh[:], op=ALU.subtract),
                   reads=[B_st], writes=[B_st])
            DVE.op(lambda: V.tensor_copy(out=STs[:, :, 12:24], in_=STr[:]), reads=[B_st], writes=[B_st])
            DVE.op(lambda: V.tensor_copy(out=STh[:], in_=STs[:, :, 12:24]), reads=[B_st], writes=[B_st])
            DVE.op(lambda: V.tensor_tensor(out=STr[:], in0=STr[:], in1=STh[:], op=ALU.subtract),
                   reads=[B_st], writes=[B_st])
            DVE.op(lambda: V.tensor_copy(out=STs[:, :, 24:36], in_=STr[:]), reads=[B_st], writes=[B_st])

            def trs():
                for tt in range(TPB):
                    ins = TE.transpose(out=pT[0:36, tt * 128:(tt + 1) * 128], in_=STs[:, tt, :], identity=ident_bf)
                return ins
            yield
            PE.op(trs, reads=[B_st], writes=[B_pT])
            POOL.op(lambda: G.memset(STT[:], 0.0), writes=[g.B_STT])
            ACT.op(lambda: A.copy(out=STT[0:36], in_=pT[0:36, 0:128 * TPB].rearrange("p (t k) -> p t k", t=TPB)),
                   reads=[B_pT], writes=[g.B_STT])
            if s == 0 and b == 0:
                dump("ST", ST[:, 0, :], [B_st])
                dump("exa", g.ex_a[:, 0, :], [B_st])
                dump("sg", st_g[:, 0, :], [B_st])
            for tt in range(TPB):
                yield from gdn_chunk(s, b, tt, g)

        def gdn_chunk(s, b, tt, g):
            cs = slice(tt * 128, (tt + 1) * 128)
            first = (s == 0 and b == 0 and tt == 0)
            qT, kT, vT = dT[0], dT[1], dT[2]
            STc = g.STT[:, tt, :]
            B_st, B_STT = g.B_st, g.B_STT
            Eb, Xb, Yb, Pb = g.Eb, g.Xb, g.Yb, g.Pb
            B_Eb, B_X, B_Y, B_P = g.B_Eb, g.B_X, g.B_Y, g.B_P

            def sel(which, hd):
                return C("SEL", bf=True, rows=128, sub=((which * 4 + hd) * 128, 128))

            def trkv():
                for hd in range(4):
                    TE.transpose(out=pT[:, hd * 128:(hd + 1) * 128], in_=kT[:, hd, cs], identity=ident_bf)
                for hd in range(4):
                    ins = TE.transpose(out=pT[:, 512 + hd * 128:512 + (hd + 1) * 128], in_=vT[:, hd, cs],
                                       identity=ident_bf)
                return ins
            yield
            PE.op(trkv, reads=B_dT[1] + B_dT[2], writes=[B_pT])
            DVE.op(lambda: V.tensor_tensor(out=g.kbg[:], in0=pTh[:, 0:4, :],
                                           in1=bc(g.ex_a[:, tt, :].unsqueeze(2), [128, 4, 128]), op=ALU.mult),
                   reads=[B_pT, B_st], writes=[g.B_kbg])
            DVE.op(lambda: V.tensor_tensor(out=g.kdec[:], in0=pTh[:, 0:4, :],
                                           in1=bc(g.ex_c[:, tt, :].unsqueeze(2), [128, 4, 128]), op=ALU.mult),
                   reads=[B_pT, B_st], writes=[g.B_kdec])
            DVE.op(lambda: V.tensor_tensor(out=g.vb[:], in0=pTh[:, 4:8, :],
                                           in1=bc(g.ex_b[:, tt, :].unsqueeze(2), [128, 4, 128]), op=ALU.mult),
                   reads=[B_pT, B_st], writes=[g.B_vb])

            def kkqk():
                for hd in range(4):
                    TE.matmul(pB[0][:, hd * 128:(hd + 1) * 128], lhsT=kT[:, hd, cs], rhs=kT[:, hd, cs],
                              start=True, stop=True)
                for hd in range(4):
                    ins = TE.matmul(pB[1][:, hd * 128:(hd + 1) * 128], lhsT=kT[:, hd, cs], rhs=qT[:, hd, cs],
                                    start=True, stop=True)
                return ins
            yield
            PE.op(kkqk, reads=B_dT[0] + B_dT[1], writes=[B_pB[0], B_pB[1]])

            def dmat(bank, which, mask, lower):
                def f():
                    for hd in range(4):
                        o = pB[bank][:, hd * 128:(hd + 1) * 128]
                        if lower:
                            TE.matmul(o, lhsT=STc, rhs=sel(which, hd), start=True, stop=False)
                            TE.matmul(o, lhsT=sel(SEL_NB, hd), rhs=STc, start=False, stop=False)
                        else:
                            TE.matmul(o, lhsT=sel(which, hd), rhs=STc, start=True, stop=False)
                            TE.matmul(o, lhsT=STc, rhs=sel(SEL_NB, hd), start=False, stop=False)
                        ins = TE.matmul(o, lhsT=ident_bf, rhs=C(mask, bf=True), start=False, stop=True)
                    return ins
                PE.op(f, reads=[B_STT], writes=[B_pB[bank]])

            yield
            dmat(2, SEL_A, "MASK_LS", True)
            ACT.op(lambda: A.activation(out=Eb[0][:], in_=pBh[2], func=AF.Exp), reads=[B_pB[2]], writes=[B_Eb[0]])
            DVE.op(lambda: V.scalar_tensor_tensor(out=Xb[0][:], in0=pBh[0], scalar=-1.0, op0=ALU.mult, in1=Eb[0][:],
                                                  op1=ALU.mult), reads=[B_pB[0], B_Eb[0]], writes=[B_X[0]])
            yield
            dmat(2, SEL_A, "MASK_US", False)
            ACT.op(lambda: A.activation(out=Eb[1][:], in_=pBh[2], func=AF.Exp), reads=[B_pB[2]], writes=[B_Eb[1]])
            DVE.op(lambda: V.scalar_tensor_tensor(out=Yb[0][:], in0=pBh[0], scalar=-1.0, op0=ALU.mult, in1=Eb[1][:],
                                                  op1=ALU.mult), reads=[B_pB[0], B_Eb[1]], writes=[B_Y[0]])
            yield
            dmat(2, SEL_AP, "MASK_UI", False)
            ACT.op(lambda: A.activation(out=Eb[0][:], in_=pBh[2], func=AF.Exp), reads=[B_pB[2]], writes=[B_Eb[0]])
            DVE.op(lambda: V.tensor_tensor(out=g.intraT[:], in0=pBh[1], in1=Eb[0][:], op=ALU.mult),
                   reads=[B_pB[1], B_Eb[0]], writes=[g.B_iT])

            def fb():
                for hd in range(4):
                    ins = TE.matmul(pB[2][:, hd * 128:(hd + 1) * 128], lhsT=sel(SEL_AP, hd), rhs=STc,
                                    start=True, stop=True)
                return ins
            yield
            PE.op(fb, reads=[B_STT], writes=[B_pB[2]])
            ACT.op(lambda: A.activation(out=Eb[1][:], in_=pBh[2], func=AF.Exp), reads=[B_pB[2]], writes=[B_Eb[1]])
            POOL.op(lambda: G.tensor_tensor(out=g.qdT[:], in0=qT[:, :, cs], in1=Eb[1][:], op=ALU.mult),
                    reads=B_dT[0] + [B_Eb[1]], writes=[g.B_qdT])
            if first:
                dump("X0", Xb[0][:, 0, :], [B_X[0]])
                dump("Y0", Yb[0][:, 0, :], [B_Y[0]])
                dump("intraT", g.intraT[:, 0, :], [g.B_iT])
                dump("qdT", g.qdT[:, 0, :], [g.B_qdT])
                dump("kbg", g.kbg[:, 0, :], [g.B_kbg])

            POOL.op(lambda: G.tensor_tensor(out=Pb[0][:], in0=Yb[0][:],
                                            in1=bc(ident_bf.unsqueeze(1), [128, 4, 128]), op=ALU.add),
                    reads=[B_Y[0]], writes=[B_P[0]])
            cur = 0
            pc = 0
            for lvl in range(1, 7):
                nxt = 1 - cur

                def sqx(cur=cur):
                    for hd in range(4):
                        ins = TE.matmul(pB[0][:, hd * 128:(hd + 1) * 128], lhsT=Yb[cur][:, hd, :], rhs=Xb[cur][:, hd, :],
                                        start=True, stop=True)
                    return ins
                yield
                PE.op(sqx, reads=[B_X[cur], B_Y[cur]], writes=[B_pB[0]])
                if lvl < 6:
                    def sqy(cur=cur):
                        for hd in range(4):
                            ins = TE.matmul(pB[1][:, hd * 128:(hd + 1) * 128], lhsT=Xb[cur][:, hd, :],
                                            rhs=Yb[cur][:, hd, :], start=True, stop=True)
                        return ins
                    PE.op(sqy, reads=[B_X[cur], B_Y[cur]], writes=[B_pB[1]])
                ACT.op(lambda nxt=nxt: A.copy(out=Xb[nxt][:], in_=pBh[0]), reads=[B_pB[0]], writes=[B_X[nxt]])
                if lvl < 6:
                    DVE.op(lambda nxt=nxt: V.tensor_copy(out=Yb[nxt][:], in_=pBh[1]), reads=[B_pB[1]],
                           writes=[B_Y[nxt]])
                pn = 1 - pc

                def pm(nxt=nxt, pc=pc):
                    for hd in range(4):
                        ins = TE.matmul(pB[2][:, hd * 128:(hd + 1) * 128], lhsT=Xb[nxt][:, hd, :], rhs=Pb[pc][:, hd, :],
                                        start=True, stop=True)
                    return ins
                yield
                PE.op(pm, reads=[B_X[nxt], B_P[pc]], writes=[B_pB[2]])
                DVE.op(lambda pn=pn, pc=pc: V.tensor_tensor(out=Pb[pn][:], in0=pBh[2], in1=Pb[pc][:], op=ALU.add),
                       reads=[B_pB[2], B_P[pc]], writes=[B_P[pn]])
                cur = nxt
                pc = pn
            TT = Pb[pc]
            B_TT = B_P[pc]
            if first:
                dump("TT", TT[:, 0, :], [B_TT])

            def uw():
                for hd in range(4):
                    TE.matmul(pB[0][:, hd * 128:(hd + 1) * 128], lhsT=TT[:, hd, :], rhs=g.vb[:, hd, :],
                              start=True, stop=True)
                for hd in range(4):
                    ins = TE.matmul(pB[1][:, hd * 128:(hd + 1) * 128], lhsT=g.kbg[:, hd, :], rhs=TT[:, hd, :],
                                    start=True, stop=True)
                return ins
            yield
            PE.op(uw, reads=[B_TT, g.B_vb, g.B_kbg], writes=[B_pB[0], B_pB[1]])
            ACT.op(lambda: A.copy(out=g.u_sb[:], in_=pBh[0]), reads=[B_pB[0]], writes=[g.B_u])
            DVE.op(lambda: V.tensor_copy(out=g.wT[:], in_=pBh[1]), reads=[B_pB[1]], writes=[g.B_wT])

            def ws():
                for hd in range(4):
                    ins = TE.matmul(pB[2][:, hd * 128:(hd + 1) * 128], lhsT=g.wT[:, hd, :], rhs=Sbf[:, hd, :],
                                    start=True, stop=True)
                return ins
            yield
            PE.op(ws, reads=[g.B_wT, B_Sbf], writes=[B_pB[2]])
            DVE.op(lambda: V.scalar_tensor_tensor(out=g.vnew[:], in0=pBh[2], scalar=-1.0, op0=ALU.mult, in1=g.u_sb[:],
                                                  op1=ALU.add), reads=[B_pB[2], g.B_u], writes=[g.B_vn])

            def oo():
                for hd in range(4):
                    TE.matmul(pB[2][:, hd * 128:(hd + 1) * 128], lhsT=g.qdT[:, hd, :], rhs=Sbf[:, hd, :],
                              start=True, stop=False)
                    TE.matmul(pB[2][:, hd * 128:(hd + 1) * 128], lhsT=g.intraT[:, hd, :], rhs=g.vnew[:, hd, :],
                              start=False, stop=True)
                for hd in range(4):
                    ins = TE.matmul(pB[0][:, hd * 128:(hd + 1) * 128], lhsT=g.kdec[:, hd, :], rhs=g.vnew[:, hd, :],
                                    start=True, stop=True)
                return ins
            yield
            PE.op(oo, reads=[g.B_qdT, B_Sbf, g.B_iT, g.B_vn, g.B_kdec], writes=[B_pB[2], B_pB[0]])
            DVE.op(lambda: V.tensor_tensor(out=Sst[:], in0=Sst[:],
                                           in1=bc(g.ex_gl[:, tt, :].unsqueeze(2), [128, 4, 128]), op=ALU.mult),
                   reads=[B_st], writes=[B_S])
            DVE.op(lambda: V.tensor_tensor(out=Sst[:], in0=pBh[0], in1=Sst[:], op=ALU.add),
                   reads=[B_pB[0]], writes=[B_S])
            POOL.op(lambda: G.tensor_copy(out=Sbf[:], in_=Sst[:]), reads=[B_S], writes=[B_Sbf])
            ACT.op(lambda: A.activation(out=g.osq[:], in_=pBh[2], func=AF.Square), reads=[B_pB[2]], writes=[g.B_osq])
            DVE.op(lambda: V.tensor_reduce(out=g.ost[:, 0:4], in_=g.osq[:], op=ALU.add, axis=AX),
                   reads=[g.B_osq], writes=[g.B_ost])
            ACT.op(lambda: A.activation(out=g.ost[:, 4:8], in_=g.ost[:, 0:4], func=AF.Ln, scale=1.0 / 128, bias=EPS),
                   reads=[g.B_ost], writes=[g.B_ost])
            ACT.op(lambda: A.activation(out=g.ost[:, 8:12], in_=g.ost[:, 4:8], func=AF.Exp, scale=-0.5),
                   reads=[g.B_ost], writes=[g.B_ost])
            DVE.op(lambda: V.tensor_tensor(out=g.osq[:], in0=pBh[2],
                                           in1=bc(g.ost[:, 8:12].unsqueeze(2), [128, 4, 128]), op=ALU.mult),
                   reads=[B_pB[2], g.B_ost], writes=[g.B_osq])
            POOL.op(lambda: G.tensor_tensor(out=mixed[:, tt, 512:1024], in0=g.osq[:].rearrange("p h d -> p (h d)"),
                                            in1=zw[:, tt, :], op=ALU.mult),
                    reads=[g.B_osq, B_zw[tt]], writes=[B_mx[tt][1]])
            if first:
                dump("u", g.u_sb[:, 0, :], [g.B_u])
                dump("gdn_out", mixed[:, 0, 512:1024], [B_mx[0][1]])

        def attn_block(s, b):
            t0 = b * BT
            ob = b
            use_sel = ob >= 4
            DVE.op(lambda: V.tensor_reduce(out=kbar[:, :, b:b + 1], in_=akT[:, :, t0:t0 + BT].unsqueeze(2),
                                           op=ALU.add, axis=AX),
                   reads=[B_ak[p][b] for p in range(4)], writes=[B_kbar])
            DVE.op(lambda: V.tensor_scalar(out=kbar[:, :, b:b + 1], in0=kbar[:, :, b:b + 1],
                                           scalar1=1.0 / 256, scalar2=None, op0=ALU.mult),
                   reads=[B_kbar], writes=[B_kbar])
            DVE.op(lambda: V.tensor_copy(out=kbar_hi[:], in_=kbar[:]), reads=[B_kbar], writes=[B_kbar])
            DVE.op(lambda: V.tensor_copy(out=kbar_t[:], in_=kbar_hi[:]), reads=[B_kbar], writes=[B_kbar])
            DVE.op(lambda: V.tensor_tensor(out=kbar_t[:], in0=kbar[:], in1=kbar_t[:], op=ALU.subtract),
                   reads=[B_kbar], writes=[B_kbar])
            DVE.op(lambda: V.tensor_copy(out=kbar_lo[:], in_=kbar_t[:]), reads=[B_kbar], writes=[B_kbar])
            if use_sel:
                for tt in range(TPB):
                    yield from attn_select(s, b, tt)
            nkt = 2 * b + 2
            for half in range(2):
                tiles = [(h4, kt) for h4 in range(4) for kt in range(nkt)]

                def emit_qk(h4, kt):
                    hd = half * 4 + h4
                    p, r0 = hd // 2, 64 * (hd % 2)
                    kb = kt // 2
                    sl = state["pt"] % NPT
                    state["pt"] += 1
                    sb_i = sl % 2
                    B_s = B_pA[sb_i]
                    lastk = (kt == nkt - 1)
                    q0 = 128 if lastk else 0
                    sreg = pA[sb_i][:, q0:256]
                    selm = use_sel and kb < ob

                    def qk():
                        ins = TE.matmul(sreg, lhsT=akT[:, p, kt * 128:(kt + 1) * 128],
                                        rhs=aqT[:, hd, q0:256], start=True, stop=not (selm or kb == ob))
                        if kb == ob:
                            ins = TE.matmul(pA[sb_i][:, q0:q0 + 128], lhsT=ident_bf, rhs=C("CAUS", bf=True),
                                            start=False, stop=True)
                        elif selm:
                            ins = TE.matmul(sreg, lhsT=cbf[:, IND_O + kb * 128:IND_O + (kb + 1) * 128],
                                            rhs=selT[:, hd, :], start=False, stop=True)
                        return ins
                    PE.op(qk, reads=[B_ak[p][kb], B_aq[p]] + ([B_selT] if selm else []), writes=[B_s])
                    dref = (2 * b + 1) - kt
                    if hd == 0 and not lastk:
                        ACT.op(lambda: A.activation(out=PTs[:, sl, 0:128], in_=pA[sb_i][:, 0:128], func=AF.Exp, scale=0.125,
                                                    bias=C("ALIBI", sub=(hd * 16 + dref - 1, 1))),
                               reads=[B_s], writes=[B_PT[sl]])
                        ACT.op(lambda: A.activation(out=PTs[:, sl, 128:256], in_=pA[sb_i][:, 128:256], func=AF.Exp,
                                                    scale=0.125, bias=C("ALIBI", sub=(hd * 16 + dref, 1))),
                               reads=[B_s], writes=[B_PT[sl]])
                    else:
                        ACT.op(lambda: A.activation(out=PTs[:, sl, q0:256], in_=sreg, func=AF.Exp, scale=0.125,
                                                    bias=C("ALIBI", sub=(hd * 16 + dref, 1))),
                               reads=[B_s], writes=[B_PT[sl]])
                    return sl

                def emit_pv(h4, kt, sl):
                    hd = half * 4 + h4

                    def pv():
                        ins = None
                        for tq in range(TPB):
                            gt = 2 * b + tq
                            if kt > gt:
                                continue
                            ins = TE.matmul(pO[tq][:, h4 * 128:h4 * 128 + 65], lhsT=PTs[:, sl, tq * 128:(tq + 1) * 128],
                                            rhs=vtm[:, kt, hd, :], start=(kt == 0), stop=(kt == gt))
                        return ins
                    PE.op(pv, reads=[B_PT[sl], B_v[kt]], writes=B_pO)

                pend = []
                for (h4, kt) in tiles:
                    yield
                    sl = emit_qk(h4, kt)
                    pend.append((h4, kt, sl))
                    if len(pend) > 2:
                        emit_pv(*pend.pop(0))
                while pend:
                    emit_pv(*pend.pop(0))
                for tq in range(TPB):
                    DVE.op(lambda tq=tq: V.reciprocal(out=rden[:], in_=pOh[tq][:, :, 64]),
                           reads=[B_pO[tq]], writes=[B_att])
                    DVE.op(lambda tq=tq: V.tensor_tensor(out=att_t[:], in0=pOh[tq][:, :, 0:64],
                                                         in1=bc(rden[:].unsqueeze(2), [128, 4, 64]),
                                                         op=ALU.mult),
                           reads=[B_pO[tq], B_att], writes=[B_att])
                    POOL.op(lambda half=half, tq=tq: G.tensor_tensor(
                        out=mixed[:, tq, half * 256:(half + 1) * 256], in0=att_t[:].rearrange("p h d -> p (h d)"),
                        in1=siluz[:, tq, half * 256:(half + 1) * 256], op=ALU.mult),
                        reads=[B_att, B_sz[tq]], writes=[B_mx[tq][0]])
            if s == 0 and b in (0, 4):
                dump(f"attn{2 * b}", mixed[:, 0, 0:512], [B_mx[0][0]])

        def attn_select(s, b, tt):
            ob = b
            qs = slice(tt * 128, (tt + 1) * 128)

            def gate():
                for hd in range(8):
                    p, r0 = hd // 2, 64 * (hd % 2)
                    o = pA[hd % 2][:, 256 + p * 8:264 + p * 8]
                    TE.matmul(o, lhsT=aqT[r0:r0 + 64, hd, qs], rhs=kbar_hi[r0:r0 + 64, p, :], start=True, stop=False)
                    ins = TE.matmul(o, lhsT=aqT[r0:r0 + 64, hd, qs], rhs=kbar_lo[r0:r0 + 64, p, :], start=False,
                                    stop=True)
                return ins
            PE.op(gate, reads=B_aq + [B_kbar], writes=B_pA)
            obm = C("OBM", sub=((ob - 4) * 64, 64)).rearrange("p (a two j) -> p a two j", two=2, j=8)
            gmv = gm[:].rearrange("p (a two) j -> p a two j", two=2)
            DVE.op(lambda: V.tensor_tensor(out=gmv[:, :, 0, :],
                                           in0=pA[0][:, 256:288].rearrange("p (a j) -> p a j", a=4),
                                           in1=obm[:, :, 0, :], op=ALU.add), reads=[B_pA[0]], writes=[B_gm])
            DVE.op(lambda: V.tensor_tensor(out=gmv[:, :, 1, :],
                                           in0=pA[1][:, 256:288].rearrange("p (a j) -> p a j", a=4),
                                           in1=obm[:, :, 1, :], op=ALU.add), reads=[B_pA[1]], writes=[B_gm])
            for hd in range(8):
                DVE.op(lambda hd=hd: V.max(out=top8[:, hd, :], in_=gm[:, hd, :]), reads=[B_gm], writes=[B_gm])
            DVE.op(lambda: V.tensor_tensor(out=gm[:], in0=gm[:], in1=bc(top8[:, :, 2:3], [128, 8, 8]),
                                           op=ALU.is_ge), reads=[B_gm], writes=[B_gm])
            DVE.op(lambda: V.tensor_scalar(out=selb[:, :, 0:8], in0=gm[:], scalar1=-NEGA, scalar2=NEGA, op0=ALU.mult,
                                           op1=ALU.add), reads=[B_gm], writes=[B_gm])
            DVE.op(lambda: V.tensor_copy(out=selb[:, :, 64:72], in_=selb[:, :, 0:8]), reads=[B_gm], writes=[B_gm])

            def trsel():
                for hd in range(8):
                    ins = TE.transpose(out=pT[0:72, hd * 128:(hd + 1) * 128], in_=selb[:, hd, :], identity=ident_bf)
                return ins
            yield
            PE.op(trsel, reads=[B_gm], writes=[B_pT])
            DVE.op(lambda: V.tensor_copy(out=selT[0:72, :, qs], in_=pT[0:72, :].rearrange("p (h q) -> p h q", h=8)),
                   reads=[B_pT], writes=[B_selT])
            if s == 0 and b == 4 and tt == 0:
                dump("selb", gm[:].rearrange("p h j -> p (h j)"), [B_gm])

        def out_block(s, b):
            for tt in range(TPB):
                gt = b * TPB + tt
                sl = state["xo"] % 2
                state["xo"] += 1
                dma(SP, ch_xo[sl], xo[sl][:], x[s, gt * 128:(gt + 1) * 128, :], writes=[B_xo[sl]])

                def tr(tt=tt):
                    for kc in range(8):
                        ins = TE.transpose(out=pT[:, kc * 128:(kc + 1) * 128], in_=mixed[:, tt, kc * 128:(kc + 1) * 128],
                                           identity=ident_bf)
                    return ins
                yield
                PE.op(tr, reads=B_mx[tt], writes=[B_pT])
                ACT.op(lambda: A.copy(out=mixT[:, 0:4, :], in_=pTh[:, 0:4, :]), reads=[B_pT], writes=[B_mixT])
                DVE.op(lambda: V.tensor_copy(out=mixT[:, 4:8, :], in_=pTh[:, 4:8, :]), reads=[B_pT, B_mixT],
                       writes=[B_mixT])

                def op_():
                    for hf in range(2):
                        for kc in range(8):
                            ins = TE.matmul(pA[hf][:, :], lhsT=mixT[:, kc, :], rhs=w_out_bf[:, kc, hf * 512:(hf + 1) * 512],
                                            start=(kc == 0), stop=(kc == 7))
                    return ins
                yield
                PE.op(op_, reads=[B_mixT], writes=B_pA)
                for hf in range(2):
                    ACT.op(lambda hf=hf: A.activation(out=junk[:, 0:512], in_=pA[hf][:, :], func=AF.Square,
                                                      accum_out=small[:, 8 + hf:9 + hf]),
                           reads=[B_pA[hf]], writes=[B_junk, B_small])
                DVE.op(lambda: V.tensor_tensor(out=small[:, 10:11], in0=small[:, 8:9], in1=small[:, 9:10], op=ALU.add),
                       reads=[B_small], writes=[B_small])
                ACT.op(lambda: A.activation(out=small[:, 11:12], in_=small[:, 10:11], func=AF.Ln, scale=1.0 / D,
                                            bias=EPS), reads=[B_small], writes=[B_small])
                ACT.op(lambda: A.activation(out=small[:, 12:13], in_=small[:, 11:12], func=AF.Exp, scale=-0.5),
                       reads=[B_small], writes=[B_small])
                for hf in range(2):
                    DVE.op(lambda hf=hf: V.scalar_tensor_tensor(
                        out=tmpf[hf][:], in0=pA[hf][:, :], scalar=small[:, 12:13], op0=ALU.mult,
                        in1=wpost_bc[:, hf * 512:(hf + 1) * 512], op1=ALU.mult),
                        reads=[B_pA[hf], B_small], writes=[B_tmpf[hf]])
                    POOL.op(lambda hf=hf, sl=sl: G.tensor_tensor(out=xo[sl][:, hf * 512:(hf + 1) * 512],
                                                                 in0=xo[sl][:, hf * 512:(hf + 1) * 512],
                                                                 in1=tmpf[hf][:], op=ALU.add),
                            reads=[B_tmpf[hf], B_xo[sl]], writes=[B_xo[sl]])
                dma(SP, ch_st[sl], y[s, gt * 128:(gt + 1) * 128, :], xo[sl][:], reads=[B_xo[sl]])

        def run_streams(gens):
            gens = list(gens)
            while gens:
                for gq in list(gens):
                    try:
                        next(gq)
                    except StopIteration:
                        gens.remove(gq)

        blocks = [(s, b) for s in range(nseq) for b in range(nblk)]
        prev = None
        for (s, b) in blocks:
            if os.environ.get("K_STOP") == "w":
                break
            state["uid"] += 1
            with contextlib.ExitStack() as ea:
                gens = [inproj_block(s, b, ea)]
                if prev is not None and "out" in phases:
                    gens.append(out_block(*prev))
                run_streams(gens)
                barrier()
            with contextlib.ExitStack() as eb:
                gens = []
                if "gdn" in phases:
                    gens.append(gdn_block(s, b, eb))
                if "attn" in phases:
                    gens.append(attn_block(s, b))
                run_streams(gens)
                if "gdn" in phases:
                    barrier()
            prev = (s, b)
        if prev is not None and "out" in phases:
            run_streams([out_block(*prev)])
        for ch in ch_st + dbg_chans:
            if ch.count:
                nc.sync.wait_ge(ch.sem, ch.count)
    return nc


def make_params(norm_pre_w, conv_w, a_log, dt_bias, gdn_norm_w, norm_post_w):
    params = np.zeros((128, PW), np.float32)
    params[:, 0:8] = np.asarray(norm_pre_w)[0].reshape(8, 128).T
    cw = np.asarray(conv_w)[0]
    params[:, 8:56] = cw.reshape(4, 12, 128).transpose(2, 0, 1).reshape(128, 48)
    params[:, 56:60] = np.asarray(a_log)[0][None, :]
    params[:, 60:64] = np.asarray(dt_bias)[0][None, :]
    params[:, 64:576] = np.tile(np.asarray(gdn_norm_w)[0], 4)[None, :]
    params[:, 576:1600] = np.asarray(norm_post_w)[0][None, :]
    return params


def kernel(x, norm_pre_w, w_in, conv_w, a_log, dt_bias, gdn_norm_w, w_out, norm_post_w):
    x = np.ascontiguousarray(np.asarray(x, dtype=np.float32))
    consts = make_consts()
    params = make_params(norm_pre_w, conv_w, a_log, dt_bias, gdn_norm_w, norm_post_w)
    w_in0 = np.ascontiguousarray(np.asarray(w_in, dtype=np.float32)[0])
    w_out0 = np.ascontiguousarray(np.asarray(w_out, dtype=np.float32)[0])
    nc = build()
    in_maps = []
    for c in range(NCORES):
        in_maps.append({"x": x[c * NSEQ:(c + 1) * NSEQ], "w_in": w_in0, "w_out": w_out0, "consts": consts,
                        "params": params})
    res = run_bass_kernel_spmd(nc, in_maps, core_ids=list(range(NCORES)))
    return np.concatenate([r["y"] for r in res.results], axis=0)
```
